# Optimizing a Trainium2 kernel written in Bass

```python
import math
import jax
import jax.numpy as jnp
from jax import lax
import numpy as np

D_MODEL = 1024
BATCH = 16
SEQ = 4096
DEPTH = 1

GRID_W = 64
CTX_LEN = 256

ATTN_HEADS = 4
ATTN_HEAD_DIM = 64
ATTN_QK_WIDTH = ATTN_HEADS * 2 * ATTN_HEAD_DIM
ATTN_V_WIDTH = ATTN_HEADS * 2 * ATTN_HEAD_DIM
Q_BLOCK = 128
ROPE_BASE = 10000.0

SSM_WIDTH = D_MODEL - ATTN_V_WIDTH
SSM_GROUP = 16
SSM_GROUPS = SSM_WIDTH // SSM_GROUP
SSM_STATE = 64
DT_MIN = 1e-3
DT_MAX = 1e-1

IN_PROJ_WIDTH = 2 * ATTN_QK_WIDTH + ATTN_V_WIDTH + SSM_WIDTH

MOE_GROUPS = 4
EXPERTS_PER_GROUP = 8
N_EXPERTS = MOE_GROUPS * EXPERTS_PER_GROUP
TOP_K_IN_GROUP = 2
EXPERT_FF = D_MODEL // 2

RMS_EPS = 1e-6

kernel_name = 'hybrid_diffattn_s5_hmoe_dit_block'


def rms_norm(x, g):
    xf = x.astype(jnp.float32)
    y = xf * lax.rsqrt(jnp.mean(xf * xf, axis=-1, keepdims=True) + RMS_EPS)
    return (y * g.astype(jnp.float32)).astype(x.dtype)


def modulate(h, shift, scale):
    return h * (1.0 + scale) + shift


def axial_rope_tables(n_tokens):
    rows = n_tokens // GRID_W
    row = jnp.broadcast_to(jnp.arange(rows, dtype=jnp.float32)[:, None], (rows, GRID_W)).reshape(-1)
    col = jnp.broadcast_to(jnp.arange(GRID_W, dtype=jnp.float32)[None, :], (rows, GRID_W)).reshape(-1)
    half = ATTN_HEAD_DIM // 2
    inv = ROPE_BASE ** (-jnp.arange(0, half, 2, dtype=jnp.float32) / half)
    ang = jnp.stack([row[:, None] * inv, col[:, None] * inv], axis=1)
    return jnp.cos(ang), jnp.sin(ang)


def apply_axial_rope(x, cos, sin):
    xr = x.astype(jnp.float32).reshape(*x.shape[:-1], 2, 2, ATTN_HEAD_DIM // 4)
    x1, x2 = xr[..., 0, :], xr[..., 1, :]
    cs = cos[None, :, None, None]
    sn = sin[None, :, None, None]
    out = jnp.stack([x1 * cs - x2 * sn, x2 * cs + x1 * sn], axis=-2)
    return out.reshape(x.shape).astype(x.dtype)


def split_in_proj(p):
    b, n, _ = p.shape
    q = p[..., :ATTN_QK_WIDTH].reshape(b, n, ATTN_HEADS, 2, ATTN_HEAD_DIM)
    k = p[..., ATTN_QK_WIDTH:2 * ATTN_QK_WIDTH].reshape(b, n, ATTN_HEADS, 2, ATTN_HEAD_DIM)
    v = p[..., 2 * ATTN_QK_WIDTH:2 * ATTN_QK_WIDTH + ATTN_V_WIDTH].reshape(b, n, ATTN_HEADS, 2 * ATTN_HEAD_DIM)
    u = p[..., 2 * ATTN_QK_WIDTH + ATTN_V_WIDTH:]
    return q, k, v, u


def diff_lambda(lq1, lk1, lq2, lk2, lam_init):
    e1 = jnp.exp(jnp.sum(lq1.astype(jnp.float32) * lk1.astype(jnp.float32)))
    e2 = jnp.exp(jnp.sum(lq2.astype(jnp.float32) * lk2.astype(jnp.float32)))
    return e1 - e2 + lam_init


def diff_softmax_attend(q, k, v, lam):
    s = jnp.einsum('bqhcd,bkhcd->bhcqk', q, k, preferred_element_type=jnp.float32) * (ATTN_HEAD_DIM ** -0.5)
    p = jax.nn.softmax(s, axis=-1)
    a = p[:, :, 0] - lam * p[:, :, 1]
    return jnp.einsum('bhqk,bkhe->bqhe', a.astype(v.dtype), v)


def diff_attention_latent(q, k_lat, v_lat, k_ctx, v_ctx, lam):
    b, n = q.shape[:2]
    k_all = jnp.concatenate([k_lat, k_ctx], axis=1)
    v_all = jnp.concatenate([v_lat, v_ctx], axis=1)
    nblk = n // Q_BLOCK
    qb = q.reshape(b, nblk, Q_BLOCK, *q.shape[2:]).swapaxes(0, 1)
    ob = lax.map(lambda qi: diff_softmax_attend(qi, k_all, v_all, lam), qb)
    return ob.swapaxes(0, 1).reshape(b, n, ATTN_HEADS, 2 * ATTN_HEAD_DIM)


def diff_head_out(o, subln_g, lam_init):
    o = rms_norm(o, subln_g) * (1.0 - lam_init)
    return o.reshape(*o.shape[:2], ATTN_V_WIDTH)


def cplx_mul(ar, ai, br, bi):
    return ar * br - ai * bi, ar * bi + ai * br


def s5_combine(e_i, e_j):
    a_r, a_i, b_r, b_i = e_i
    c_r, c_i, d_r, d_i = e_j
    ar, ai = cplx_mul(c_r, c_i, a_r, a_i)
    br, bi = cplx_mul(c_r, c_i, b_r, b_i)
    return ar, ai, br + d_r, bi + d_i


def s5_discretise(a_re, a_im, log_dt, b_re, b_im):
    dt = jnp.exp(log_dt.astype(jnp.float32))[:, None]
    ar, ai = a_re.astype(jnp.float32), a_im.astype(jnp.float32)
    mag = jnp.exp(ar * dt)
    lr, li = mag * jnp.cos(ai * dt), mag * jnp.sin(ai * dt)
    den = ar * ar + ai * ai
    nr, ni = lr - 1.0, li
    cr = (nr * ar + ni * ai) / den
    ci = (ni * ar - nr * ai) / den
    bbr, bbi = cplx_mul(cr[..., None], ci[..., None], b_re.astype(jnp.float32), b_im.astype(jnp.float32))
    return lr, li, bbr, bbi


def s5_scan(u, lam_r, lam_i, bb_r, bb_i, h0):
    bu_r = jnp.einsum('gph,bngh->bngp', bb_r, u)
    bu_i = jnp.einsum('gph,bngh->bngp', bb_i, u)
    if h0 is not None:
        ir, ii = cplx_mul(lam_r, lam_i, h0[0], h0[1])
        bu_r = bu_r.at[:, 0].add(ir)
        bu_i = bu_i.at[:, 0].add(ii)
    n = u.shape[1]
    a_r = jnp.broadcast_to(lam_r, (1, n) + lam_r.shape)
    a_i = jnp.broadcast_to(lam_i, (1, n) + lam_i.shape)
    _, _, h_r, h_i = lax.associative_scan(s5_combine, (a_r, a_i, bu_r, bu_i), axis=1)
    return h_r, h_i


def s5_readout(c_re, c_im, h_r, h_i):
    return jnp.einsum('ghp,bngp->bngh', c_re, h_r) - jnp.einsum('ghp,bngp->bngh', c_im, h_i)


def s5_glu(y, w_glu, b_glu, dtype):
    y = jax.nn.gelu(y.reshape(*y.shape[:2], SSM_WIDTH)).astype(dtype)
    return y * jax.nn.sigmoid(y @ w_glu + b_glu)


def s5_mixer(u_lat, u_ctx, a_re, a_im, log_dt, b_re, b_im, c_re, c_im, d_skip, w_glu, b_glu, need_ctx):
    b, n, _ = u_lat.shape
    nc = u_ctx.shape[1]
    ul = u_lat.astype(jnp.float32).reshape(b, n, SSM_GROUPS, SSM_GROUP)
    uc = u_ctx.astype(jnp.float32).reshape(b, nc, SSM_GROUPS, SSM_GROUP)
    cr, ci = c_re.astype(jnp.float32), c_im.astype(jnp.float32)
    dd = d_skip.astype(jnp.float32)
    y_lat = dd * ul
    y_ctx = dd * uc if need_ctx else None
    for direction in range(2):
        lr, li, bbr, bbi = s5_discretise(a_re[direction], a_im[direction], log_dt[direction],
                                         b_re[direction], b_im[direction])
        ul_d = ul if direction == 0 else ul[:, ::-1]
        uc_d = uc if direction == 0 else uc[:, ::-1]
        hc_r, hc_i = s5_scan(uc_d, lr, li, bbr, bbi, None)
        hl_r, hl_i = s5_scan(ul_d, lr, li, bbr, bbi, (hc_r[:, -1], hc_i[:, -1]))
        yl = s5_readout(cr, ci, hl_r, hl_i)
        y_lat = y_lat + (yl if direction == 0 else yl[:, ::-1])
        if need_ctx:
            yc = s5_readout(cr, ci, hc_r, hc_i)
            y_ctx = y_ctx + (yc if direction == 0 else yc[:, ::-1])
    out_lat = s5_glu(y_lat, w_glu, b_glu, u_lat.dtype)
    out_ctx = s5_glu(y_ctx, w_glu, b_glu, u_ctx.dtype) if need_ctx else None
    return out_lat, out_ctx


def hier_moe(h, w_rg, b_rg, w_re, b_re, w_g, w_u, w_d):
    b, n, d = h.shape
    t = h.reshape(b * n, d)
    g_prob = jax.nn.softmax((t @ w_rg + b_rg).astype(jnp.float32), axis=-1)
    p_grp, g_idx = lax.top_k(g_prob, 1)
    e_logit = (t @ w_re + b_re).astype(jnp.float32).reshape(-1, MOE_GROUPS, EXPERTS_PER_GROUP)
    e_logit = jnp.take_along_axis(e_logit, g_idx[:, :, None], axis=1)[:, 0]
    top_v, top_i = lax.top_k(e_logit, TOP_K_IN_GROUP)
    w_slot = jax.nn.softmax(top_v, axis=-1) * p_grp
    e_id = g_idx * EXPERTS_PER_GROUP + top_i
    gates = jnp.einsum('tk,tke->te', w_slot,
                       jax.nn.one_hot(e_id, N_EXPERTS, dtype=jnp.float32)).astype(t.dtype)
    out = jnp.zeros_like(t)
    for e in range(N_EXPERTS):
        hid = jax.nn.silu(t @ w_g[e]) * (t @ w_u[e])
        out = out + gates[:, e:e + 1] * (hid @ w_d[e])
    return out.reshape(b, n, d)


def setup_inputs(seed: int = 0) -> dict:
    key = jax.random.key(seed)
    k = jax.random.split(key, 34)

    def nrm(i, shape, scale):
        return scale * jax.random.normal(k[i], shape, jnp.float32)

    D, F = D_MODEL, EXPERT_FF
    G, P, HS = SSM_GROUPS, SSM_STATE, SSM_GROUP
    d = ATTN_HEAD_DIM
    n_idx = jnp.arange(P, dtype=jnp.float32)
    return {
        'x': nrm(0, (BATCH, SEQ, D), 1.0),
        'c': nrm(1, (BATCH, D), 1.0),
        'ctx': nrm(2, (BATCH, CTX_LEN, D), 1.0),
        'c_ctx': nrm(3, (D,), 1.0),
        'w_ada': nrm(4, (DEPTH, D, 6 * D), 0.5 * D ** -0.5),
        'b_ada': nrm(5, (DEPTH, 6 * D), 0.01),
        'norm1_g': 1.0 + nrm(6, (DEPTH, D), 0.02),
        'w_in': nrm(7, (DEPTH, D, IN_PROJ_WIDTH), D ** -0.5),
        'q_norm_g': 1.0 + nrm(8, (DEPTH, d), 0.02),
        'k_norm_g': 1.0 + nrm(9, (DEPTH, d), 0.02),
        'lambda_q1': nrm(10, (DEPTH, d), 0.1),
        'lambda_k1': nrm(11, (DEPTH, d), 0.1),
        'lambda_q2': nrm(12, (DEPTH, d), 0.1),
        'lambda_k2': nrm(13, (DEPTH, d), 0.1),
        'subln_g': 1.0 + nrm(14, (DEPTH, 2 * d), 0.02),
        'ssm_a_re': -0.5 + nrm(15, (DEPTH, 2, G, P), 0.01),
        'ssm_a_im': math.pi * n_idx * (1.0 + nrm(16, (DEPTH, 2, G, P), 0.01)),
        'ssm_log_dt': jax.random.uniform(k[17], (DEPTH, 2, G), jnp.float32, math.log(DT_MIN), math.log(DT_MAX)),
        'ssm_b_re': nrm(18, (DEPTH, 2, G, P, HS), (2 * HS) ** -0.5),
        'ssm_b_im': nrm(19, (DEPTH, 2, G, P, HS), (2 * HS) ** -0.5),
        'ssm_c_re': nrm(20, (DEPTH, G, HS, P), P ** -0.5),
        'ssm_c_im': nrm(21, (DEPTH, G, HS, P), P ** -0.5),
        'ssm_d': nrm(22, (DEPTH, G, HS), 1.0),
        'w_glu': nrm(23, (DEPTH, SSM_WIDTH, SSM_WIDTH), SSM_WIDTH ** -0.5),
        'b_glu': nrm(24, (DEPTH, SSM_WIDTH), 0.01),
        'w_out': nrm(25, (DEPTH, D, D), D ** -0.5),
        'norm2_g': 1.0 + nrm(26, (DEPTH, D), 0.02),
        'w_route_group': nrm(27, (DEPTH, D, MOE_GROUPS), D ** -0.5),
        'b_route_group': nrm(28, (DEPTH, MOE_GROUPS), 0.01),
        'w_route_expert': nrm(29, (DEPTH, D, N_EXPERTS), D ** -0.5),
        'b_route_expert': nrm(30, (DEPTH, N_EXPERTS), 0.01),
        'w_exp_gate': nrm(31, (DEPTH, N_EXPERTS, D, F), D ** -0.5),
        'w_exp_up': nrm(32, (DEPTH, N_EXPERTS, D, F), D ** -0.5),
        'w_exp_down': nrm(33, (DEPTH, N_EXPERTS, F, D), F ** -0.5),
    }


def reference(x, c, ctx, c_ctx, w_ada, b_ada, norm1_g, w_in, q_norm_g, k_norm_g,
              lambda_q1, lambda_k1, lambda_q2, lambda_k2, subln_g,
              ssm_a_re, ssm_a_im, ssm_log_dt, ssm_b_re, ssm_b_im, ssm_c_re, ssm_c_im, ssm_d,
              w_glu, b_glu, w_out, norm2_g,
              w_route_group, b_route_group, w_route_expert, b_route_expert,
              w_exp_gate, w_exp_up, w_exp_down):
    n_lat = x.shape[1]
    cos, sin = axial_rope_tables(n_lat)
    h_ctx = ctx
    for l in range(DEPTH):
        last = l == DEPTH - 1
        lam_init = 0.8 - 0.6 * math.exp(-0.3 * l)
        mod = jax.nn.silu(c) @ w_ada[l] + b_ada[l]
        mod_c = jax.nn.silu(c_ctx) @ w_ada[l] + b_ada[l]
        sh1, sc1, g1, sh2, sc2, g2 = jnp.split(mod[:, None, :], 6, axis=-1)
        csh1, csc1, cg1, csh2, csc2, cg2 = jnp.split(mod_c, 6, axis=-1)

        px = modulate(rms_norm(x, norm1_g[l]), sh1, sc1) @ w_in[l]
        pc = modulate(rms_norm(h_ctx, norm1_g[l]), csh1, csc1) @ w_in[l]
        q_x, k_x, v_x, u_x = split_in_proj(px)
        q_c, k_c, v_c, u_c = split_in_proj(pc)
        q_x = apply_axial_rope(rms_norm(q_x, q_norm_g[l]), cos, sin)
        k_x = apply_axial_rope(rms_norm(k_x, k_norm_g[l]), cos, sin)
        k_c = rms_norm(k_c, k_norm_g[l])
        lam = diff_lambda(lambda_q1[l], lambda_k1[l], lambda_q2[l], lambda_k2[l], lam_init)
        a_x = diff_head_out(diff_attention_latent(q_x, k_x, v_x, k_c, v_c, lam), subln_g[l], lam_init)
        s_x, s_c = s5_mixer(u_x, u_c, ssm_a_re[l], ssm_a_im[l], ssm_log_dt[l], ssm_b_re[l], ssm_b_im[l],
                            ssm_c_re[l], ssm_c_im[l], ssm_d[l], w_glu[l], b_glu[l], not last)
        x = x + g1 * (jnp.concatenate([a_x, s_x], axis=-1) @ w_out[l])
        if not last:
            q_c = rms_norm(q_c, q_norm_g[l])
            a_c = diff_head_out(diff_softmax_attend(q_c, k_c, v_c, lam), subln_g[l], lam_init)
            h_ctx = h_ctx + cg1 * (jnp.concatenate([a_c, s_c], axis=-1) @ w_out[l])

        x = x + g2 * hier_moe(modulate(rms_norm(x, norm2_g[l]), sh2, sc2),
                              w_route_group[l], b_route_group[l], w_route_expert[l], b_route_expert[l],
                              w_exp_gate[l], w_exp_up[l], w_exp_down[l])
        if not last:
            h_ctx = h_ctx + cg2 * hier_moe(modulate(rms_norm(h_ctx, norm2_g[l]), csh2, csc2),
                                           w_route_group[l], b_route_group[l], w_route_expert[l],
                                           b_route_expert[l], w_exp_gate[l], w_exp_up[l], w_exp_down[l])
    return x
```

```python
import math
import numpy as np
import concourse.bass as bass
import concourse.mybir as mybir
from concourse.bass_utils import run_bass_kernel_spmd

F32 = mybir.dt.float32
BF16 = mybir.dt.bfloat16
I32 = mybir.dt.int32
ALU = mybir.AluOpType
AF = mybir.ActivationFunctionType
AX = mybir.AxisListType
ENGS = ("sync", "scalar", "vector", "gpsimd", "tensor")

NB = 2
SEQ = 4096
CTX = 256
D = 1024
NT = 34
TAU = 4608
EPS = 1e-6
PI = math.pi
LAM_INIT = 0.8 - 0.6 * math.exp(0.0)


class Prog:
    def __init__(self, nc):
        self.nc = nc
        self.ops = []
        self.last_w = {}
        self.readers = {}
        self.ctx = []
        self.last_eng = {}
        self.last_sem = {}

    def sb(self, name, shape, dt):
        self.uid = getattr(self, "uid", 0) + 1
        cm = self.nc.sbuf_tensor("s%d_%s" % (self.uid, name), shape, dt)
        t = cm.__enter__()
        self.ctx.append(cm)
        return t

    def ps(self, name, shape, dt):
        self.uid = getattr(self, "uid", 0) + 1
        cm = self.nc.psum_tensor("p%d_%s" % (self.uid, name), shape, dt)
        t = cm.__enter__()
        self.ctx.append(cm)
        return t

    def mark(self):
        return len(self.ctx)

    def release(self, mark):
        self.barrier()
        while len(self.ctx) > mark:
            self.ctx.pop().__exit__(None, None, None)

    def op(self, eng, fn, r=(), w=(), dma=False, semkey=None):
        i = len(self.ops)
        deps = set()
        for k in list(r) + list(w):
            if k in self.last_w:
                deps.add(self.last_w[k])
        for k in w:
            for q in self.readers.get(k, ()):
                deps.add(q)
        self.ops.append(dict(eng=eng, fn=fn, deps=deps, dma=dma, semkey=semkey))
        for k in w:
            self.last_w[k] = i
            self.readers[k] = []
        for k in r:
            self.readers.setdefault(k, []).append(i)
        if dma:
            self.last_sem[semkey] = i
        else:
            self.last_eng[eng] = i
        return i

    def call(self, eng, name, r=(), w=(), **kw):
        return self.op(eng, lambda e: getattr(e, name)(**kw), r=r, w=w)

    def dma(self, eng, out, in_, r=(), w=(), semkey=None, **kw):
        return self.op(eng, lambda e: e.dma_start(out=out, in_=in_, **kw), r=r, w=w, dma=True, semkey=semkey)

    def barrier(self):
        deps = set(self.last_eng.values()) | set(self.last_sem.values())
        for e in ENGS:
            i = len(self.ops)
            self.ops.append(dict(eng=e, fn=None, deps=set(deps), dma=False, semkey=None))
        self.last_w = {}
        self.readers = {}

    def emit(self, final_wait_ops=()):
        nc = self.nc
        ops = self.ops
        eng_sem = {}
        for e in ENGS:
            cm = nc.semaphore("p_" + e)
            eng_sem[e] = cm.__enter__()
            self.ctx.append(cm)
        needed = set(final_wait_ops)
        for o in ops:
            needed |= o["deps"]
        dma_sem = {}
        cnt = {e: 0 for e in ENGS}
        dcnt = {}
        for i, o in enumerate(ops):
            o["sem"] = None
            if o["fn"] is None:
                continue
            if o["dma"]:
                k = o["semkey"]
                if k not in dma_sem:
                    cm = nc.semaphore("d_%d" % len(dma_sem))
                    dma_sem[k] = cm.__enter__()
                    self.ctx.append(cm)
                    dcnt[k] = 0
                dcnt[k] += 16
                o["sem"] = dma_sem[k]
                o["ticket"] = dcnt[k]
                o["inc"] = 16
            elif i in needed:
                cnt[o["eng"]] += 1
                o["sem"] = eng_sem[o["eng"]]
                o["ticket"] = cnt[o["eng"]]
                o["inc"] = 1
        self.n_sems = len(dma_sem) + len(ENGS)
        per_eng = {e: [] for e in ENGS}
        for i, o in enumerate(ops):
            per_eng[o["eng"]].append(i)

        def run_engine(ename, eobj):
            waited = {}
            for i in per_eng[ename]:
                o = ops[i]
                for d in sorted(o["deps"]):
                    p = ops[d]
                    if p["fn"] is None:
                        continue
                    if p["eng"] == ename and not p["dma"] and ename == "tensor":
                        continue
                    sem = p["sem"]
                    key = id(sem)
                    if waited.get(key, 0) >= p["ticket"]:
                        continue
                    eobj.wait_ge(sem, p["ticket"])
                    waited[key] = p["ticket"]
                if o["fn"] is None:
                    continue
                ins = o["fn"](eobj)
                if o["sem"] is not None:
                    ins.then_inc(o["sem"], o["inc"])
            if ename == "sync":
                for d in final_wait_ops:
                    p = ops[d]
                    eobj.wait_ge(p["sem"], p["ticket"])

        with nc.Block() as block:
            @block.sync
            def _(e):
                run_engine("sync", e)

            @block.scalar
            def _(e):
                run_engine("scalar", e)

            @block.vector
            def _(e):
                run_engine("vector", e)

            @block.gpsimd
            def _(e):
                run_engine("gpsimd", e)

            @block.tensor
            def _(e):
                run_engine("tensor", e)

    def close(self):
        while self.ctx:
            self.ctx.pop().__exit__(None, None, None)


def build_program(dbg=None):
    nc = bass.Bass("TRN2", target_bir_lowering=False)
    T = {}

    def din(name, shape, dt=F32):
        T[name] = nc.dram_tensor(name, list(shape), dt, kind="ExternalInput").ap()
        return T[name]

    def dint(name, shape, dt):
        T[name] = nc.dram_tensor(name, list(shape), dt, kind="Internal").ap()
        return T[name]

    x_d = din("x", [NB * SEQ, D])
    ctx_d = din("ctx", [NB * CTX, D])
    cvT_d = din("cvT", [128, 8, 3])
    wada_d = din("w_ada", [D, 6 * D])
    bada_d = din("b_ada", [3, 6 * D])
    n1g_d = din("norm1_g", [128, D])
    n2g_d = din("norm2_g", [128, D])
    sel3_d = din("sel3", [3, 3 * 128])
    win_d = din("w_in", [D, 2048])
    qg_d = din("qg", [128, 512])
    kg_d = din("kg", [128, 512])
    lam_d = din("lamv", [128, 256])
    sub_d = din("subln", [128, 512])
    ropec_d = din("rope_cos", [SEQ, 512])
    ropes_d = din("rope_sin", [SEQ, 512])
    ident_d = din("ident", [128, 128])
    sA_re = din("sA_re", [128, 512]); sA_im = din("sA_im", [128, 512]); sA_dt = din("sA_dt", [128, 512])
    sB_re = din("sB_re", [128, 512]); sB_im = din("sB_im", [128, 512]); sMask = din("sMask", [128, 2])
    pA_re = din("pA_re", [128, 32]); pA_im = din("pA_im", [128, 32]); pA_dt = din("pA_dt", [128, 32])
    cC_re = din("cC_re", [128, 256]); cC_im = din("cC_im", [128, 256]); cMask = din("cMask", [128, 2])
    dsk_d = din("dskip", [128, 4]); eye32_d = din("eye32", [128, 32]); iota_d = din("iota1", [128, 512])
    wglu_d = din("w_glu", [512, 512]); bglu_d = din("b_gluT", [128, 4])
    wout_d = din("w_out", [D, D])
    wr_d = din("w_r", [D, 36]); br_d = din("b_r", [128, 36])
    weg_d = din("w_eg", [32, D, 512]); weu_d = din("w_eu", [32, D, 512]); wed_d = din("w_ed", [32, 512, D])
    out_d = nc.dram_tensor("out", [NB * SEQ, D], F32, kind="ExternalOutput").ap()
    mod_d = dint("mod_s", [3, 128, 6 * D], F32)
    qT_d = dint("qT_s", [NB, 4, 128, SEQ], BF16)
    uT_d = dint("uT_s", [NB, 4, 128, TAU], BF16)
    dint("tab_s", [32, 2, 128, 512], F32)
    x2T_d = dint("x2T_s", [NB * SEQ // 2048, 128, 8, 2048], BF16)
    dbg_t = {}
    if dbg:
        for name, shape in dbg.items():
            dbg_t[name] = nc.dram_tensor("dbg_" + name, list(shape), F32, kind="ExternalOutput").ap()

    P = Prog(nc)
    C = P.call
    fin = []

    ident = P.sb("ident", [128, 128], BF16)
    identf = P.sb("identf", [128, 128], F32)
    epst = P.sb("epst", [128, 1], F32)
    gates = P.sb("gates", [128, NB * 32, 32], F32)
    neglam = P.sb("neglam", [128, 1], F32)
    banks = [P.ps("bk%d" % i, [128, 512], F32) for i in range(6)]
    tbs = [P.ps("tb%d" % i, [128, 1024], BF16) for i in range(2)]
    P.dma("sync", identf[:], ident_d[:], w=["identf"], semkey="c0")
    C("vector", "tensor_copy", r=["identf"], w=["ident"], out=ident[:], in_=identf[:])
    C("vector", "memset", w=["epst"], ap=epst[:], constant=EPS)

    def rsqrt_rows(src, dst, scale, n, rk, wk):
        C("scalar", "activation", r=rk + ["epst"], w=wk, out=dst, in_=src, func=AF.Sqrt, bias=epst[:, 0:1], scale=scale)
        C("vector", "reciprocal", r=wk, w=wk, out=dst, in_=dst)

    m0 = P.mark()
    cvT = P.sb("cvT", [128, 8, 3], F32)
    cvS = P.sb("cvS", [128, 8, 3], BF16)
    wa = [P.sb("wa%d" % i, [128, 8, 512], BF16) for i in range(2)]
    modsb = P.sb("modsb", [3, 6 * D], F32)
    bad = P.sb("bad", [3, 6 * D], F32)
    lamt = P.sb("lamt", [128, 256], F32)
    lamw = P.sb("lamw", [128, 128], F32)
    lams = P.sb("lams", [128, 4], F32)
    P.dma("sync", cvT[:], cvT_d[:], w=["cvT"], semkey="c1")
    P.dma("sync", bad[:], bada_d[:], w=["bad"], semkey="c2")
    P.dma("sync", lamt[:], lam_d[:], w=["lamt"], semkey="c3")
    sel3f = P.sb("sel3f", [3, 384], F32); sel3 = P.sb("sel3", [3, 384], BF16)
    mhi = P.sb("mhi", [3, 6 * D], BF16); mlo = P.sb("mlo", [3, 6 * D], BF16)
    mstg = [P.sb("mstg%d" % i, [128, 512], F32) for i in range(2)]
    P.dma("sync", sel3f[:], sel3_d[:], w=["sel3f"], semkey="c2b")
    C("vector", "tensor_copy", r=["sel3f"], w=["sel3"], out=sel3[:], in_=sel3f[:])
    C("scalar", "activation", r=["cvT"], w=["cvS"], out=cvS[:], in_=cvT[:], func=AF.Silu)
    wada_v = wada_d.rearrange("(k p) n -> p k n", p=128)
    for cb in range(12):
        s = cb % 2
        P.dma("gpsimd", wa[s][:], wada_v[:, :, cb * 512:(cb + 1) * 512], w=["wa%d" % s], semkey="wa%d" % s)
        for k in range(8):
            C("tensor", "matmul", r=["wa%d" % s, "cvS"], w=["bk0"], out=banks[0][0:3, :], lhsT=cvS[:, k, :], rhs=wa[s][:, k, :], start=(k == 0), stop=(k == 7))
        C("vector", "tensor_tensor", r=["bk0", "bad"], w=["modsb"], out=modsb[:, cb * 512:(cb + 1) * 512], in0=banks[0][0:3, :], in1=bad[:, cb * 512:(cb + 1) * 512], op=ALU.add)
    C("vector", "tensor_copy", r=["modsb"], w=["mhi"], out=mhi[:], in_=modsb[:])
    C("vector", "tensor_tensor", r=["modsb", "mhi"], w=["mlo"], out=mlo[:], in0=modsb[:], in1=mhi[:], op=ALU.subtract)
    bc = 0
    for r_ in range(3):
        for cb in range(12):
            st = bc % 2
            bk = 1 + bc % 2
            bc += 1
            C("tensor", "matmul", r=["sel3", "mhi"], w=["bk%d" % bk], out=banks[bk][:], lhsT=sel3[:, r_ * 128:(r_ + 1) * 128], rhs=mhi[:, cb * 512:(cb + 1) * 512], start=True, stop=False)
            C("tensor", "matmul", r=["sel3", "mlo"], w=["bk%d" % bk], out=banks[bk][:], lhsT=sel3[:, r_ * 128:(r_ + 1) * 128], rhs=mlo[:, cb * 512:(cb + 1) * 512], start=False, stop=True)
            C("vector", "tensor_copy", r=["bk%d" % bk], w=["mstg%d" % st], out=mstg[st][:], in_=banks[bk][:])
            P.dma("sync", mod_d[r_, :, cb * 512:(cb + 1) * 512], mstg[st][:], r=["mstg%d" % st], w=["mod_d"], semkey="mstg%d" % st)
    C("vector", "tensor_tensor", r=["lamt"], w=["lamw"], out=lamw[:, 0:64], in0=lamt[:, 0:64], in1=lamt[:, 64:128], op=ALU.mult)
    C("vector", "tensor_tensor", r=["lamt"], w=["lamw"], out=lamw[:, 64:128], in0=lamt[:, 128:192], in1=lamt[:, 192:256], op=ALU.mult)
    C("vector", "tensor_reduce", r=["lamw"], w=["lams"], out=lams[:, 0:2], in_=lamw[:].rearrange("p (a b) -> p a b", b=64), axis=AX.X, op=ALU.add)
    C("scalar", "activation", r=["lams"], w=["lams"], out=lams[:, 2:4], in_=lams[:, 0:2], func=AF.Exp)
    C("vector", "tensor_tensor", r=["lams"], w=["lams"], out=lams[:, 0:1], in0=lams[:, 3:4], in1=lams[:, 2:3], op=ALU.subtract)
    C("vector", "tensor_scalar", r=["lams"], w=["neglam"], out=neglam[:], in0=lams[:, 0:1], scalar1=-LAM_INIT, scalar2=None, op0=ALU.add)
    P.release(m0)

    def load_mod_bcast(dst, key, row, chunk, semkey):
        P.dma("sync", dst[:], mod_d[row, :, chunk * D:(chunk + 1) * D], r=["mod_d"], w=[key], semkey=semkey)

    def norm_mod(xt, xk, A, Ak, B, Bk, outt, outk, W):
        sq, sqk, ss, ssk, t_, tk = W
        C("scalar", "activation", r=[xk], w=[sqk], out=sq[:], in_=xt, func=AF.Square)
        C("vector", "tensor_reduce", r=[sqk], w=[ssk], out=ss[:, 0:1], in_=sq[:], axis=AX.X, op=ALU.add)
        rsqrt_rows(ss[:, 0:1], ss[:, 1:2], 1.0 / D, 1, [ssk], [ssk])
        C("vector", "scalar_tensor_tensor", r=[xk, ssk, Ak], w=[tk], out=t_[:], in0=xt, scalar=ss[:, 1:2], in1=A[:], op0=ALU.mult, op1=ALU.mult)
        C("gpsimd", "tensor_tensor", r=[tk, Bk], w=[outk], out=outt, in0=t_[:], in1=B[:], op=ALU.add)

    nm_sq = P.sb("nm_sq", [128, D], F32)
    nm_ss = P.sb("nm_ss", [128, 2], F32)
    nm_ss2 = P.sb("nm_ss2", [128, 2], F32)
    nm_t = P.sb("nm_t", [128, D], F32)

    for b in range(NB):
        mb = P.mark()
        catA = P.sb("catA", [128, 4, SEQ], BF16)
        m1 = P.mark()
        KT = P.sb("KT", [128, 4, NT * 128], BF16)
        Vaug = P.sb("Vaug", [128, NT, 4, 130], BF16)
        mA = P.mark()
        winb = P.sb("winb", [128, 8, 2048], BF16)
        A1 = P.sb("A1", [128, D], F32); B1 = P.sb("B1", [128, D], F32)
        A1c = P.sb("A1c", [128, D], F32); B1c = P.sb("B1c", [128, D], F32)
        g1b = nm_sq
        Gq = P.sb("Gq", [128, 8, 64], F32); Gk = P.sb("Gk", [128, 8, 64], F32)
        xts = [P.sb("xt%d" % i, [128, D], F32) for i in range(2)]
        xmbs = [P.sb("xmb%d" % i, [128, D], BF16) for i in range(2)]
        NW = [(nm_sq, "nm_sq", nm_ss, "nm_ss", nm_t, "nm_t")] * 2
        xmT = [P.sb("xmT%d" % i, [128, 8, 128], BF16) for i in range(2)]
        sqt = P.sb("sqt", [128, 512], F32)
        ssq = P.sb("ssq", [128, 16], F32)
        qn = P.sb("qn", [128, 512], F32)
        qn2 = P.sb("qn2", [128, 512], F32)
        qr = P.sb("qr", [128, 512], BF16)
        rt = [P.sb("rt%d" % i, [128, 512], F32) for i in range(4)]
        rcs = [P.sb("rc0", [128, 512], F32)] * 2; rss = [P.sb("rs_0", [128, 512], F32)] * 2
        usb = P.sb("usb", [128, 4, 128], BF16)
        qTs = P.sb("qTs", [128, 4, 128], BF16)
        P.dma("gpsimd", winb[:], win_d.rearrange("(k p) n -> p k n", p=128), w=["winb"], semkey="winb")
        P.dma("sync", g1b[:], n1g_d[:], w=["nm_sq"], semkey="c5")
        load_mod_bcast(B1, "B1", b, 0, "c6"); load_mod_bcast(A1, "A1", b, 1, "c7")
        load_mod_bcast(B1c, "B1c", 2, 0, "c8"); load_mod_bcast(A1c, "A1c", 2, 1, "c9")
        for Ax, k_ in ((A1, "A1"), (A1c, "A1c")):
            C("vector", "scalar_tensor_tensor", r=[k_, "nm_sq"], w=[k_], out=Ax[:], in0=Ax[:], scalar=1.0, in1=g1b[:], op0=ALU.add, op1=ALU.mult)
        P.dma("sync", Gq[:].rearrange("p a b -> p (a b)"), qg_d[:], w=["Gq"], semkey="c10")
        P.dma("sync", Gk[:].rearrange("p a b -> p (a b)"), kg_d[:], w=["Gk"], semkey="c11")
        C("vector", "memset", w=["Vaug"], ap=Vaug[:, :, :, 128:130], constant=1.0)

        def qk_post(bank, bkey, G, Gkey, rope_lt, dst_is_q, tt):
            sqt_, sqk_ = sqt, "sqt"
            rc, rs_ = rcs[tt % 2], rss[tt % 2]
            rck, rsk = "rc0", "rs_0"
            C("scalar", "activation", r=[bkey], w=[sqk_], out=sqt_[:], in_=bank[:], func=AF.Square)
            C("vector", "tensor_reduce", r=[sqk_], w=["ssq"], out=ssq[:, 0:8], in_=sqt_[:].rearrange("p (a b) -> p a b", b=64), axis=AX.X, op=ALU.add)
            rsqrt_rows(ssq[:, 0:8], ssq[:, 8:16], 1.0 / 64, 8, ["ssq"], ["ssq"])
            C("vector", "tensor_tensor", r=[bkey, "ssq"], w=["qn"], out=qn[:].rearrange("p (a b) -> p a b", b=64), in0=bank[:].rearrange("p (a b) -> p a b", b=64), in1=ssq[:, 8:16].unsqueeze(2).to_broadcast([128, 8, 64]), op=ALU.mult)
            if rope_lt is None:
                C("gpsimd", "tensor_tensor", r=["qn", Gkey], w=["qr"], out=qr[:], in0=qn[:], in1=G[:].rearrange("p a b -> p (a b)"), op=ALU.mult)
            else:
                C("gpsimd", "tensor_tensor", r=["qn", Gkey], w=["qn2"], out=qn2[:], in0=qn[:], in1=G[:].rearrange("p a b -> p (a b)"), op=ALU.mult)
                v = lambda t, h: t[:].rearrange("p (a h f) -> p a h f", h=2, f=16)[:, :, h, :]
                C("vector", "tensor_tensor", r=["qn2", rck], w=["rt0"], out=v(rt[0], 0), in0=v(qn2, 0), in1=v(rc, 0), op=ALU.mult)
                C("vector", "tensor_tensor", r=["qn2", rsk], w=["rt1"], out=v(rt[1], 0), in0=v(qn2, 1), in1=v(rs_, 0), op=ALU.mult)
                C("vector", "tensor_tensor", r=["rt0", "rt1"], w=["qr"], out=v(qr, 0), in0=v(rt[0], 0), in1=v(rt[1], 0), op=ALU.subtract)
                C("gpsimd", "tensor_tensor", r=["qn2", rck], w=["rt2"], out=v(rt[2], 0), in0=v(qn2, 1), in1=v(rc, 0), op=ALU.mult)
                C("gpsimd", "tensor_tensor", r=["qn2", rsk], w=["rt3"], out=v(rt[3], 0), in0=v(qn2, 0), in1=v(rs_, 0), op=ALU.mult)
                C("gpsimd", "tensor_tensor", r=["rt2", "rt3"], w=["qr"], out=v(qr, 1), in0=v(rt[2], 0), in1=v(rt[3], 0), op=ALU.add)
            for h in range(4):
                C("tensor", "transpose", r=["qr", "ident"], w=["tb1"], out=tbs[1][:, h * 128:(h + 1) * 128], in_=qr[:, h * 128:(h + 1) * 128], identity=ident[:])
            if dst_is_q:
                C("scalar", "copy", r=["tb1"], w=["qTs"], out=qTs[:], in_=tbs[1][:, 0:512].rearrange("p (h t) -> p h t", t=128))
                P.dma("sync", qT_d[b].rearrange("h p n -> p h n")[:, :, rope_lt * 128:(rope_lt + 1) * 128], qTs[:], r=["qTs"], w=["qT_d"], semkey="qTs")
            else:
                C("scalar", "copy", r=["tb1"], w=["KT"], out=KT[:, :, tt * 128:(tt + 1) * 128], in_=tbs[1][:, 0:512].rearrange("p (h t) -> p h t", t=128))

        def p1_load_x(tt_):
            s_ = tt_ % 2
            lt_ = tt_ - 2
            src = ctx_d[b * CTX + tt_ * 128: b * CTX + (tt_ + 1) * 128, :] if tt_ < 2 else x_d[b * SEQ + lt_ * 128: b * SEQ + (lt_ + 1) * 128, :]
            P.dma("sync", xts[s_][:], src, w=["xt%d" % s_], semkey="xt%d" % s_)

        def p1_load_rope(tt_):
            lt_ = tt_ - 2
            if lt_ >= 0:
                P.dma("sync", rcs[0][:], ropec_d[lt_ * 128:(lt_ + 1) * 128, :], w=["rc0"], semkey="rc0")
                P.dma("sync", rss[0][:], ropes_d[lt_ * 128:(lt_ + 1) * 128, :], w=["rs_0"], semkey="rs_0")

        p1_load_x(0)
        for tt in range(NT):
            s = tt % 2
            lt = tt - 2
            isctx = tt < 2
            if tt + 1 < NT:
                p1_load_x(tt + 1)
            xmb = xmbs[s]
            if isctx:
                norm_mod(xts[s][:], "xt%d" % s, A1c, "A1c", B1c, "B1c", xmb[:], "xmb%d" % s, NW[s])
            else:
                norm_mod(xts[s][:], "xt%d" % s, A1, "A1", B1, "B1", xmb[:], "xmb%d" % s, NW[s])
            for k in range(8):
                C("tensor", "transpose", r=["xmb%d" % s, "ident"], w=["tb0"], out=tbs[0][:, k * 128:(k + 1) * 128], in_=xmb[:, k * 128:(k + 1) * 128], identity=ident[:])
            C("scalar", "copy", r=["tb0"], w=["xmT%d" % s], out=xmT[s][:].rearrange("p k t -> p (k t)"), in_=tbs[0][:])
            todo = [(512, 1), (1024, 2)] if isctx else [(0, 0), (512, 1), (1024, 2)]
            for col, bi in todo:
                for k in range(8):
                    C("tensor", "matmul", r=["xmT%d" % s, "winb"], w=["bk%d" % bi], out=banks[bi][:], lhsT=xmT[s][:, k, :], rhs=winb[:, k, col:col + 512], start=(k == 0), stop=(k == 7))
            for j in range(4):
                for k in range(8):
                    C("tensor", "matmul", r=["xmT%d" % s, "winb"], w=["bk3"], out=banks[3][:, j * 128:(j + 1) * 128], lhsT=winb[:, k, 1536 + 128 * j:1536 + 128 * (j + 1)], rhs=xmT[s][:, k, :], start=(k == 0), stop=(k == 7))
            C("scalar", "copy", r=["bk2"], w=["Vaug"], out=Vaug[:, tt, :, 0:128], in_=banks[2][:].rearrange("p (h e) -> p h e", e=128))
            C("vector", "tensor_copy", r=["bk3"], w=["usb"], out=usb[:].rearrange("p j t -> p (j t)"), in_=banks[3][:])
            uv = uT_d[b].rearrange("j p n -> p j n")
            if isctx:
                P.dma("sync", uv[:, :, tt * 128:(tt + 1) * 128], usb[:], r=["usb"], w=["uT_d"], semkey="usb")
                P.dma("sync", uv[:, :, 4352 + tt * 128:4352 + (tt + 1) * 128], usb[:], r=["usb"], w=["uT_d"], semkey="usb")
            else:
                P.dma("sync", uv[:, :, tt * 128:(tt + 1) * 128], usb[:], r=["usb"], w=["uT_d"], semkey="usb")
            qk_post(banks[1], "bk1", Gk, "Gk", None if isctx else lt, False, tt)
            if not isctx:
                qk_post(banks[0], "bk0", Gq, "Gq", lt, True, tt)
            if tt + 1 < NT:
                p1_load_rope(tt + 1)
        P.release(mA)

        m2 = P.mark()
        SG4 = P.sb("SG4", [128, 4, 128], F32)
        qblk = [P.sb("qblk%d" % i, [128, 512], BF16) for i in range(2)]
        pts = [P.sb("pt%d" % i, [128, 512], BF16) for i in range(4)]
        stv = [banks[0][:], banks[1][:], banks[2][:], tbs[1][:].bitcast(F32)]
        stk = ["bk0", "bk1", "bk2", "tb1"]
        osb = [P.sb("osb%d" % i, [128, 8, 129], F32) for i in range(2)]
        rr8 = P.sb("rr8", [128, 2, 8], F32)
        ss4 = P.sb("ss4", [128, 2, 4], F32)
        ept0 = P.sb("ept0", [128, 4, 128], F32)
        ept1 = P.sb("ept1", [128, 4, 128], F32)
        epoo = P.sb("epoo", [128, 4, 128], F32)
        ab4 = P.sb("ab4", [128, 4, 128], BF16)
        P.dma("sync", SG4[:].rearrange("p a b -> p (a b)"), sub_d[:], w=["SG4"], semkey="c12")
        C("vector", "tensor_scalar", r=["SG4"], w=["SG4"], out=SG4[:], in0=SG4[:], scalar1=1.0 - LAM_INIT, scalar2=None, op0=ALU.mult)
        obank = [banks[3], banks[4], banks[5]]

        def oreg(c, qt, lo, hi):
            r_ = c * 4 + qt
            return obank[r_ // 3][:, (r_ % 3) * 129 + lo:(r_ % 3) * 129 + hi], "bk%d" % (3 + r_ // 3)

        units = [(h, qb) for h in range(4) for qb in range(8)]
        steps = [(kt, c) for kt in range(NT) for c in range(2)]
        NS = len(steps)

        def load_q(ui):
            h, qb = units[ui]
            s = ui % 2
            P.dma("sync", qblk[s][:], qT_d[b, h, :, qb * 512:(qb + 1) * 512], r=["qT_d"], w=["qblk%d" % s], semkey="qblk%d" % s)

        def ep_stage1(ui):
            s = ui % 2
            for bi, (r0, r1) in enumerate(((0, 3), (3, 6), (6, 8))):
                nr = r1 - r0
                C("vector", "tensor_copy", r=["bk%d" % (3 + bi)], w=["osb%d" % s], out=osb[s][:, r0:r1, :], in_=obank[bi][:, 0:nr * 129].rearrange("p (a b) -> p a b", b=129))
            C("vector", "reciprocal", r=["osb%d" % s], w=["rr8"], out=rr8[:, s, :], in_=osb[s][:, :, 128])
            C("vector", "tensor_scalar", r=["rr8", "neglam"], w=["rr8"], out=rr8[:, s, 4:8], in0=rr8[:, s, 4:8], scalar1=neglam[:, 0:1], scalar2=None, op0=ALU.mult)
            C("vector", "tensor_tensor", r=["osb%d" % s, "rr8"], w=["ept0"], out=ept0[:], in0=osb[s][:, 0:4, 0:128], in1=rr8[:, s, 0:4].unsqueeze(2).to_broadcast([128, 4, 128]), op=ALU.mult)
            C("vector", "tensor_tensor", r=["osb%d" % s, "rr8"], w=["ept1"], out=ept1[:], in0=osb[s][:, 4:8, 0:128], in1=rr8[:, s, 4:8].unsqueeze(2).to_broadcast([128, 4, 128]), op=ALU.mult)
            C("gpsimd", "tensor_tensor", r=["ept0", "ept1"], w=["epoo"], out=epoo[:], in0=ept0[:], in1=ept1[:], op=ALU.add)
            C("gpsimd", "tensor_tensor", r=["epoo"], w=["ept0"], out=ept0[:], in0=epoo[:], in1=epoo[:], op=ALU.mult)
            C("vector", "tensor_reduce", r=["ept0"], w=["ss4"], out=ss4[:, s, :], in_=ept0[:], axis=AX.X, op=ALU.add)

        def ep_stage2(ui):
            s = ui % 2
            C("scalar", "activation", r=["ss4", "epst"], w=["ss4"], out=ss4[:, s, :], in_=ss4[:, s, :], func=AF.Sqrt, bias=epst[:, 0:1], scale=1.0 / 128)

        def ep_stage3(ui):
            s = ui % 2
            C("vector", "reciprocal", r=["ss4"], w=["ss4"], out=ss4[:, s, :], in_=ss4[:, s, :])
            C("vector", "tensor_tensor", r=["epoo", "ss4"], w=["ept1"], out=ept1[:], in0=epoo[:], in1=ss4[:, s, :].unsqueeze(2).to_broadcast([128, 4, 128]), op=ALU.mult)
            C("gpsimd", "tensor_tensor", r=["ept1", "SG4"], w=["ab4"], out=ab4[:], in0=ept1[:], in1=SG4[:], op=ALU.mult)
            for qt in range(4):
                C("tensor", "transpose", r=["ab4", "ident"], w=["tb0"], out=tbs[0][:, qt * 128:(qt + 1) * 128], in_=ab4[:, qt, :], identity=ident[:])

        def ep_stage4(ui):
            h, qb = units[ui]
            C("scalar", "copy", r=["tb0"], w=["catA"], out=catA[:, h, qb * 512:(qb + 1) * 512], in_=tbs[0][:, 0:512])

        gstep = 0
        load_q(0)
        load_q(1)
        for ui, (h, qb) in enumerate(units):
            s = ui % 2
            base = gstep

            def qk(i):
                kt, c = steps[i]
                si = (base + i) % 4
                C("tensor", "matmul", r=["KT", "qblk%d" % s], w=[stk[si]], out=stv[si], lhsT=KT[64 * c:64 * c + 64, h, kt * 128:(kt + 1) * 128], rhs=qblk[s][64 * c:64 * c + 64, :], start=True, stop=True)

            qk(0)
            qk(1)
            for i in range(NS):
                kt, c = steps[i]
                si = (base + i) % 4
                if i % 2 == 0 and i + 2 < NS:
                    qk(i + 2)
                    qk(i + 3)
                C("scalar", "activation", r=[stk[si]], w=["pt%d" % si], out=pts[si][:], in_=stv[si], func=AF.Exp, scale=0.125)
                for qt in range(4):
                    o_ap, o_key = oreg(c, qt, 0, 129)
                    C("tensor", "matmul", r=["pt%d" % si, "Vaug"], w=[o_key], out=o_ap, lhsT=pts[si][:, qt * 128:(qt + 1) * 128], rhs=Vaug[:, kt, h, 0:129], start=(kt == 0), stop=(kt == NT - 1), skip_group_check=True)
                if ui > 0:
                    if i == 12:
                        ep_stage2(ui - 1)
                    elif i == 18:
                        ep_stage3(ui - 1)
                    elif i == 30:
                        ep_stage4(ui - 1)
            gstep += NS
            ep_stage1(ui)
            if ui + 2 < len(units):
                load_q(ui + 2)
        ep_stage2(len(units) - 1)
        ep_stage3(len(units) - 1)
        ep_stage4(len(units) - 1)
        P.release(m1)

        catS = P.sb("catS", [128, 4, SEQ], BF16)
        m3 = P.mark()
        ssm_phase(P, C, T, b, banks, catS, epst, locals())
        P.release(m3)

        m4 = P.mark()
        woutb = P.sb("woutb", [128, 8, D], BF16)
        G1 = P.sb("G1", [128, D], F32); A2 = P.sb("A2", [128, D], F32); B2 = P.sb("B2", [128, D], F32)
        g2b = nm_sq
        xts = [P.sb("xq%d" % i, [128, D], F32) for i in range(2)]
        x1 = [P.sb("x1_%d" % i, [128, D], F32) for i in range(2)]
        nsq4 = P.sb("nsq4", [128, D], F32); nt4 = P.sb("nt4", [128, D], F32)
        NW4 = [(nm_sq, "nm_sq", nm_ss, "nm_ss", nm_t, "nm_t"), (nsq4, "nsq4", nm_ss2, "nm_ss2", nt4, "nt4")]
        gt4 = [P.sb("gt4_%d" % i, [128, D], F32) for i in range(2)]
        xm2s = [P.sb("xm2_%d" % i, [128, D], F32) for i in range(2)]
        x2Tfs = [P.sb("x2Tf%d" % i, [128, 8, 128], F32) for i in range(2)]
        x2Tbs = [P.sb("x2Tb%d" % i, [128, 8, 128], BF16) for i in range(2)]
        wrf = P.sb("wrf", [128, 8, 36], F32)
        brb = P.sb("brb", [128, 36], F32)
        lgs = [P.sb("lg%d" % i, [128, 36], F32) for i in range(2)]
        gws = [P.sb("gw%d" % i, [128, 16], F32) for i in range(2)]
        ohgs = [P.sb("ohg%d" % i, [128, 4], F32) for i in range(2)]
        msks = [P.sb("msk%d" % i, [128, 32], F32) for i in range(2)]
        msk2s = [P.sb("msk2%d" % i, [128, 32], F32) for i in range(2)]
        oh1s = [P.sb("oh1%d" % i, [128, 32], F32) for i in range(2)]
        oh2s = [P.sb("oh2%d" % i, [128, 32], F32) for i in range(2)]
        P.dma("gpsimd", woutb[:], wout_d.rearrange("(k p) n -> p k n", p=128), w=["woutb"], semkey="woutb")
        P.dma("sync", g2b[:], n2g_d[:], w=["nm_sq"], semkey="c13")
        load_mod_bcast(G1, "G1", b, 2, "c14"); load_mod_bcast(B2, "B2", b, 3, "c15"); load_mod_bcast(A2, "A2", b, 4, "c16")
        C("vector", "scalar_tensor_tensor", r=["A2", "nm_sq"], w=["A2"], out=A2[:], in0=A2[:], scalar=1.0, in1=g2b[:], op0=ALU.add, op1=ALU.mult)
        P.dma("sync", wrf[:], wr_d.rearrange("(k p) n -> p k n", p=128), w=["wrf"], semkey="c17")
        P.dma("sync", brb[:], br_d[:], w=["brb"], semkey="c18")
        for lt in range(32):
            s = lt % 2
            sx = "_%d" % s
            ob = (0, 1) if s == 0 else (4, 5)
            xm2, x2Tf, x2Tb = xm2s[s], x2Tfs[s], x2Tbs[s]
            lg, gw, ohg, msk, msk2, oh1, oh2 = lgs[s], gws[s], ohgs[s], msks[s], msk2s[s], oh1s[s], oh2s[s]
            K = lambda nme: nme + sx
            row0 = b * SEQ + lt * 128
            if lt == 0:
                P.dma("sync", xts[0][:], x_d[row0:row0 + 128, :], w=["xq0"], semkey="xq0")
            if lt + 1 < 32:
                P.dma("sync", xts[1 - s][:], x_d[row0 + 128:row0 + 256, :], w=["xq%d" % (1 - s)], semkey="xq%d" % (1 - s))
            for half in range(2):
                for k in range(8):
                    C("tensor", "matmul", r=["catA", "catS", "woutb"], w=["bk%d" % ob[half]], out=banks[ob[half]][:], lhsT=(catA if k < 4 else catS)[:, k % 4, lt * 128:(lt + 1) * 128], rhs=woutb[:, k, half * 512:(half + 1) * 512], start=(k == 0), stop=(k == 7))
            for half in range(2):
                sl = slice(half * 512, (half + 1) * 512)
                C("vector", "tensor_tensor", r=["bk%d" % ob[half], "G1"], w=[K("gt4")], out=gt4[s][:, sl], in0=banks[ob[half]][:], in1=G1[:, sl], op=ALU.mult)
            C("gpsimd", "tensor_tensor", r=[K("gt4"), "xq%d" % s], w=["x1_%d" % s], out=x1[s][:], in0=gt4[s][:], in1=xts[s][:], op=ALU.add)
            P.dma("sync", out_d[row0:row0 + 128, :], x1[s][:], r=["x1_%d" % s], w=["out_d%d" % (row0 // 128)], semkey="x1_%d" % s)
            norm_mod(x1[s][:], "x1_%d" % s, A2, "A2", B2, "B2", xm2[:], K("xm2"), NW4[s])
            for k in range(8):
                bi = 2 + k // 4
                C("tensor", "transpose", r=[K("xm2"), "identf"], w=["bk%d" % bi], out=banks[bi][:, (k % 4) * 128:(k % 4 + 1) * 128], in_=xm2[:, k * 128:(k + 1) * 128], identity=identf[:])
            for hh in range(2):
                C("scalar", "copy", r=["bk%d" % (2 + hh)], w=[K("x2Tf")], out=x2Tf[:, hh * 4:(hh + 1) * 4, :].rearrange("p k t -> p (k t)"), in_=banks[2 + hh][:])
            C("vector", "tensor_copy", r=[K("x2Tf")], w=[K("x2Tb")], out=x2Tb[:], in_=x2Tf[:])
            P.dma("sync", x2T_d[row0 // 2048, :, :, row0 % 2048:row0 % 2048 + 128], x2Tb[:], r=[K("x2Tb")], w=["x2T_d"], semkey="x2Tb%d" % s)
            rb = banks[ob[0]]
            rbk = "bk%d" % ob[0]
            for k in range(8):
                C("tensor", "matmul", r=[K("x2Tf"), "wrf"], w=[rbk], out=rb[:, 0:36], lhsT=x2Tf[:, k, :], rhs=wrf[:, k, :], start=(k == 0), stop=(k == 7))
            C("vector", "tensor_tensor", r=[rbk, "brb"], w=[K("lg")], out=lg[:], in0=rb[:, 0:36], in1=brb[:], op=ALU.add)
            gidx = b * 32 + lt
            gk, lk, ok_, mk, m2k, o1k, o2k = K("gw"), K("lg"), K("ohg"), K("msk"), K("msk2"), K("oh1"), K("oh2")
            C("vector", "tensor_reduce", r=[lk], w=[gk], out=gw[:, 0:1], in_=lg[:, 0:4], axis=AX.X, op=ALU.max)
            C("vector", "tensor_scalar", r=[lk, gk], w=[ok_], out=ohg[:], in0=lg[:, 0:4], scalar1=gw[:, 0:1], scalar2=None, op0=ALU.is_ge)
            C("vector", "tensor_scalar", r=[gk], w=[gk], out=gw[:, 1:2], in0=gw[:, 0:1], scalar1=-1.0, scalar2=None, op0=ALU.mult)
            C("scalar", "activation", r=[lk, gk], w=[gk], out=gw[:, 4:8], in_=lg[:, 0:4], func=AF.Exp, bias=gw[:, 1:2], scale=1.0)
            C("vector", "tensor_reduce", r=[gk], w=[gk], out=gw[:, 2:3], in_=gw[:, 4:8], axis=AX.X, op=ALU.add)
            C("vector", "reciprocal", r=[gk], w=[gk], out=gw[:, 3:4], in_=gw[:, 2:3])
            C("vector", "tensor_scalar", r=[ok_], w=[ok_], out=ohg[:], in0=ohg[:], scalar1=-1.0, scalar2=1e30, op0=ALU.add, op1=ALU.mult)
            C("vector", "tensor_tensor", r=[lk, ok_], w=[mk], out=msk[:].rearrange("p (g e) -> p g e", e=8), in0=lg[:, 4:36].rearrange("p (g e) -> p g e", e=8), in1=ohg[:].unsqueeze(2).to_broadcast([128, 4, 8]), op=ALU.add)
            C("vector", "tensor_reduce", r=[mk], w=[gk], out=gw[:, 8:9], in_=msk[:], axis=AX.X, op=ALU.max)
            C("vector", "tensor_scalar", r=[mk, gk], w=[o1k], out=oh1[:], in0=msk[:], scalar1=gw[:, 8:9], scalar2=None, op0=ALU.is_ge)
            C("vector", "scalar_tensor_tensor", r=[o1k, mk], w=[m2k], out=msk2[:], in0=oh1[:], scalar=-1e30, in1=msk[:], op0=ALU.mult, op1=ALU.add)
            C("vector", "tensor_reduce", r=[m2k], w=[gk], out=gw[:, 9:10], in_=msk2[:], axis=AX.X, op=ALU.max)
            C("vector", "tensor_scalar", r=[m2k, gk], w=[o2k], out=oh2[:], in0=msk2[:], scalar1=gw[:, 9:10], scalar2=None, op0=ALU.is_ge)
            C("vector", "tensor_tensor", r=[gk], w=[gk], out=gw[:, 10:11], in0=gw[:, 9:10], in1=gw[:, 8:9], op=ALU.subtract)
            C("scalar", "activation", r=[gk], w=[gk], out=gw[:, 11:12], in_=gw[:, 10:11], func=AF.Exp)
            C("vector", "tensor_scalar", r=[gk], w=[gk], out=gw[:, 12:13], in0=gw[:, 11:12], scalar1=1.0, scalar2=None, op0=ALU.add)
            C("vector", "reciprocal", r=[gk], w=[gk], out=gw[:, 12:13], in_=gw[:, 12:13])
            C("vector", "tensor_tensor", r=[gk], w=[gk], out=gw[:, 13:14], in0=gw[:, 12:13], in1=gw[:, 3:4], op=ALU.mult)
            C("vector", "tensor_tensor", r=[gk], w=[gk], out=gw[:, 14:15], in0=gw[:, 13:14], in1=gw[:, 11:12], op=ALU.mult)
            C("vector", "tensor_scalar", r=[o1k, gk], w=[o1k], out=oh1[:], in0=oh1[:], scalar1=gw[:, 13:14], scalar2=None, op0=ALU.mult)
            C("vector", "scalar_tensor_tensor", r=[o2k, gk, o1k], w=["gates"], out=gates[:, gidx, :], in0=oh2[:], scalar=gw[:, 14:15], in1=oh1[:], op0=ALU.mult, op1=ALU.add)
        P.release(mb)

    m5 = P.mark()
    SB = 2048
    acc = P.sb("acc", [128, 16, D], F32)
    x2Ts = [P.sb("x2T%d" % i, [128, 8, SB], BF16) for i in range(2)]
    wg = [P.sb("wg%d" % i, [128, 8, 512], BF16) for i in range(2)]
    wu = [P.sb("wu%d" % i, [128, 8, 512], BF16) for i in range(2)]
    wd = [P.sb("wd%d" % i, [128, 4, D], BF16) for i in range(2)]
    silb = P.sb("silb", [128, 2, 512], F32)
    sil = [silb[:, 0, :], silb[:, 1, :]]
    hid = [P.sb("hid%d" % i, [128, 4, 512], BF16) for i in range(2)]
    G2 = nm_t[:]
    xr = [nm_sq, nm_sq]
    ecnt = 0
    hcnt = 0
    NSB = NB * SEQ // SB

    def load_x2T(i):
        P.dma("sync", x2Ts[i % 2][:], x2T_d[i], r=["x2T_d"], w=["x2T%d" % (i % 2)], semkey="x2T%d" % (i % 2))

    load_x2T(0)
    for sbk in range(NSB):
        b = sbk // 2
        tok0 = sbk * SB
        x2T = x2Ts[sbk % 2]
        x2k = "x2T%d" % (sbk % 2)
        if sbk + 1 < NSB:
            load_x2T(sbk + 1)
        for e in range(32):
            s = ecnt % 2
            ecnt += 1
            P.dma("gpsimd", wg[s][:], weg_d[e].rearrange("(k p) f -> p k f", p=128), w=["wg%d" % s], semkey="wg%d" % s)
            P.dma("gpsimd", wu[s][:], weu_d[e].rearrange("(k p) f -> p k f", p=128), w=["wu%d" % s], semkey="wu%d" % s)
            P.dma("gpsimd", wd[s][:], wed_d[e].rearrange("(k p) f -> p k f", p=128), w=["wd%d" % s], semkey="wd%d" % s)
            for blk in range(SB // 512):
                hs = hcnt % 2
                hcnt += 1
                for f in range(4):
                    pg = (2 * f) % 4
                    pu = (2 * f + 1) % 4
                    for k in range(8):
                        C("tensor", "matmul", r=[x2k, "wg%d" % s], w=["bk%d" % pg], out=banks[pg][:], lhsT=wg[s][:, k, f * 128:(f + 1) * 128], rhs=x2T[:, k, blk * 512:(blk + 1) * 512], start=(k == 0), stop=(k == 7))
                    for k in range(8):
                        C("tensor", "matmul", r=[x2k, "wu%d" % s], w=["bk%d" % pu], out=banks[pu][:], lhsT=wu[s][:, k, f * 128:(f + 1) * 128], rhs=x2T[:, k, blk * 512:(blk + 1) * 512], start=(k == 0), stop=(k == 7))
                    C("scalar", "activation", r=["bk%d" % pg], w=["sil%d" % (f % 2)], out=sil[f % 2], in_=banks[pg][:], func=AF.Silu)
                    C("vector", "tensor_tensor", r=["bk%d" % pu, "sil%d" % (f % 2)], w=["hid%d" % hs], out=hid[hs][:, f, :], in0=banks[pu][:], in1=sil[f % 2], op=ALU.mult)
                for tl in range(4):
                    ti = blk * 4 + tl
                    for half in range(2):
                        bi = 4 + half
                        for f in range(4):
                            C("tensor", "matmul", r=["hid%d" % hs, "wd%d" % s], w=["bk%d" % bi], out=banks[bi][:], lhsT=hid[hs][:, f, tl * 128:(tl + 1) * 128], rhs=wd[s][:, f, half * 512:(half + 1) * 512], start=(f == 0), stop=(f == 3))
                        ak = "acc%d" % ti
                        if e == 0:
                            C("vector", "tensor_scalar", r=["bk%d" % bi, "gates"], w=[ak], out=acc[:, ti, half * 512:(half + 1) * 512], in0=banks[bi][:], scalar1=gates[:, sbk * 16 + ti, e:e + 1], scalar2=None, op0=ALU.mult)
                        else:
                            C("vector", "scalar_tensor_tensor", r=["bk%d" % bi, "gates", ak], w=[ak], out=acc[:, ti, half * 512:(half + 1) * 512], in0=banks[bi][:], scalar=gates[:, sbk * 16 + ti, e:e + 1], in1=acc[:, ti, half * 512:(half + 1) * 512], op0=ALU.mult, op1=ALU.add)
        P.dma("sync", G2, mod_d[b, :, 5 * D:6 * D], r=["mod_d"], w=["G2"], semkey="c19")
        for ti in range(16):
            s = ti % 2
            row0 = tok0 + ti * 128
            okey = "out_d%d" % (row0 // 128)
            P.dma("sync", xr[s][:], out_d[row0:row0 + 128, :], r=[okey], w=["xr0"], semkey="xr0")
            C("vector", "tensor_tensor", r=["acc%d" % ti, "G2"], w=["acc%d" % ti], out=acc[:, ti, :], in0=acc[:, ti, :], in1=G2, op=ALU.mult)
            C("vector", "tensor_tensor", r=["acc%d" % ti, "xr0"], w=["xr0"], out=xr[s][:], in0=acc[:, ti, :], in1=xr[s][:], op=ALU.add)
            fin.append(P.dma("sync", out_d[row0:row0 + 128, :], xr[s][:], r=["xr0"], w=[okey], semkey="xo0"))
    P.emit(final_wait_ops=fin[-1:])
    P.close()
    return nc


def ssm_phase(P, C, T, b, banks, catS, epst, env):
    TWO_PI = 2.0 * PI
    PIB = 3.141592
    ygT = P.sb("ygT", [128, 4, SEQ], BF16)
    hF = P.sb("hF", [128, 2, 2, 2050], BF16)
    WB1 = P.sb("WB1", [128, 8, 2, 2, 64], BF16)
    WCL = P.sb("WCL", [128, 2, 16, 2, 2, 16], BF16)
    WK = P.sb("WK", [128, 2, 4, 32], BF16)
    rho2_s = P.sb("rho2_s", [128, 32], F32)
    th2_s = P.sb("th2_s", [128, 32], F32)
    upg = P.sb("upg", [128, TAU], BF16)
    WB = P.sb("WB", [128, 8, 2, 2, 64], BF16)
    WC = P.sb("WC", [128, 16, 2, 2, 16], BF16)
    WD = P.sb("WD", [128, 4, 32], BF16)
    rho_s = P.sb("rho_s", [128, 32], F32)
    th_s = P.sb("th_s", [128, 32], F32)
    wglu = P.sb("wglu", [128, 4, 512], BF16)
    bglu = P.sb("bglu", [128, 4], F32)
    iot = P.sb("iot", [128, 512], F32)
    nm_sq_, nm_t_ = env["nm_sq"], env["nm_t"]
    tq = [nm_sq_[:, 0:512], nm_sq_[:, 512:1024], nm_t_[:, 0:512], nm_t_[:, 512:1024]]
    ki = P.sb("ki", [128, 512], I32)
    P.dma("gpsimd", wglu[:], T["w_glu"].rearrange("(k p) n -> p k n", p=128), w=["wglu"], semkey="wglu")
    P.dma("sync", bglu[:], T["b_gluT"][:], w=["bglu"], semkey="s0")
    P.dma("sync", iot[:], T["iota1"][:], w=["iot"], semkey="s1")

    def trig(src, sk, n, sin_out, sok, cos_out, cok, w0, w1):
        a, bq = tq[w0], tq[w1]
        C("vector", "tensor_scalar", r=[sk], w=["tq%d" % w0], out=a[:, 0:n], in0=src, scalar1=1.0 / TWO_PI, scalar2=None, op0=ALU.mult)
        C("vector", "tensor_copy", r=["tq%d" % w0], w=["ki"], out=ki[:, 0:n], in_=a[:, 0:n])
        C("vector", "tensor_copy", r=["ki"], w=["tq%d" % w0], out=a[:, 0:n], in_=ki[:, 0:n])
        C("vector", "scalar_tensor_tensor", r=["tq%d" % w0, sk], w=["tq%d" % w0], out=a[:, 0:n], in0=a[:, 0:n], scalar=-TWO_PI, in1=src, op0=ALU.mult, op1=ALU.add)
        C("vector", "tensor_scalar", r=["tq%d" % w0], w=["tq%d" % w1], out=bq[:, 0:n], in0=a[:, 0:n], scalar1=PI, scalar2=TWO_PI, op0=ALU.is_gt, op1=ALU.mult)
        C("vector", "tensor_tensor", r=["tq%d" % w0, "tq%d" % w1], w=[sok], out=sin_out, in0=a[:, 0:n], in1=bq[:, 0:n], op=ALU.subtract)
        C("vector", "tensor_scalar", r=[sok], w=["tq%d" % w1], out=bq[:, 0:n], in0=sin_out, scalar1=-PI, scalar2=TWO_PI, op0=ALU.is_lt, op1=ALU.mult)
        C("vector", "tensor_tensor", r=[sok, "tq%d" % w1], w=[sok], out=sin_out, in0=sin_out, in1=bq[:, 0:n], op=ALU.add)
        C("vector", "tensor_scalar", r=[sok], w=[sok], out=sin_out, in0=sin_out, scalar1=-PIB, scalar2=PIB, op0=ALU.max, op1=ALU.min)
        C("scalar", "activation", r=[sok], w=[sok], out=sin_out, in_=sin_out, func=AF.Sin)
        C("vector", "tensor_scalar", r=["tq%d" % w0], w=["tq%d" % w1], out=bq[:, 0:n], in0=a[:, 0:n], scalar1=PI / 2, scalar2=TWO_PI, op0=ALU.is_gt, op1=ALU.mult)
        C("vector", "scalar_tensor_tensor", r=["tq%d" % w0, "tq%d" % w1], w=[cok], out=cos_out, in0=a[:, 0:n], scalar=PI / 2, in1=bq[:, 0:n], op0=ALU.add, op1=ALU.subtract)
        C("vector", "tensor_scalar", r=[cok], w=["tq%d" % w1], out=bq[:, 0:n], in0=cos_out, scalar1=-PI, scalar2=TWO_PI, op0=ALU.is_lt, op1=ALU.mult)
        C("vector", "tensor_tensor", r=[cok, "tq%d" % w1], w=[cok], out=cos_out, in0=cos_out, in1=bq[:, 0:n], op=ALU.add)
        C("vector", "tensor_scalar", r=[cok], w=[cok], out=cos_out, in0=cos_out, scalar1=-PIB, scalar2=PIB, op0=ALU.max, op1=ALU.min)
        C("scalar", "activation", r=[cok], w=[cok], out=cos_out, in_=cos_out, func=AF.Sin)

    md = P.mark()
    L_ = {}
    for nm in ("sA_re", "sA_im", "sA_dt", "sB_re", "sB_im"):
        L_[nm] = P.sb("l_" + nm, [128, 512], F32)
        P.dma("sync", L_[nm][:], T[nm][:], w=[nm], semkey="s_" + nm)
    smask = P.sb("smask", [128, 2], F32); cmask = P.sb("cmask", [128, 2], F32)
    P.dma("sync", smask[:], T["sMask"][:], w=["smask"], semkey="s2")
    P.dma("sync", cmask[:], T["cMask"][:], w=["cmask"], semkey="s3")
    W = [P.sb("dw%d" % i, [128, 512], F32) for i in range(8)]
    wk = ["dw%d" % i for i in range(8)]
    VT = lambda *a, **k: C("vector", "tensor_tensor", *a, **k)
    dtv, mag, ang, sn, cs, lr, li, den = W
    C("scalar", "activation", r=["sA_dt"], w=[wk[0]], out=dtv[:], in_=L_["sA_dt"][:], func=AF.Exp)
    VT(r=["sA_re", wk[0]], w=[wk[1]], out=mag[:], in0=L_["sA_re"][:], in1=dtv[:], op=ALU.mult)
    C("scalar", "activation", r=[wk[1]], w=[wk[1]], out=mag[:], in_=mag[:], func=AF.Exp)
    VT(r=["sA_im", wk[0]], w=[wk[2]], out=ang[:], in0=L_["sA_im"][:], in1=dtv[:], op=ALU.mult)
    trig(ang[:], wk[2], 512, sn[:], wk[3], cs[:], wk[4], 0, 1)
    VT(r=[wk[1], wk[4]], w=[wk[5]], out=lr[:], in0=mag[:], in1=cs[:], op=ALU.mult)
    VT(r=[wk[1], wk[3]], w=[wk[6]], out=li[:], in0=mag[:], in1=sn[:], op=ALU.mult)
    C("vector", "tensor_scalar", r=[wk[5]], w=[wk[5]], out=lr[:], in0=lr[:], scalar1=-1.0, scalar2=None, op0=ALU.add)
    are, aim = L_["sA_re"], L_["sA_im"]
    VT(r=["sA_re"], w=[wk[7]], out=den[:], in0=are[:], in1=are[:], op=ALU.mult)
    VT(r=["sA_im"], w=[wk[0]], out=dtv[:], in0=aim[:], in1=aim[:], op=ALU.mult)
    VT(r=[wk[7], wk[0]], w=[wk[7]], out=den[:], in0=den[:], in1=dtv[:], op=ALU.add)
    C("vector", "reciprocal", r=[wk[7]], w=[wk[7]], out=den[:], in_=den[:])
    VT(r=[wk[5], "sA_re"], w=[wk[1]], out=mag[:], in0=lr[:], in1=are[:], op=ALU.mult)
    VT(r=[wk[6], "sA_im"], w=[wk[0]], out=dtv[:], in0=li[:], in1=aim[:], op=ALU.mult)
    VT(r=[wk[1], wk[0]], w=[wk[1]], out=mag[:], in0=mag[:], in1=dtv[:], op=ALU.add)
    VT(r=[wk[1], wk[7]], w=[wk[1]], out=mag[:], in0=mag[:], in1=den[:], op=ALU.mult)
    VT(r=[wk[6], "sA_re"], w=[wk[2]], out=ang[:], in0=li[:], in1=are[:], op=ALU.mult)
    VT(r=[wk[5], "sA_im"], w=[wk[0]], out=dtv[:], in0=lr[:], in1=aim[:], op=ALU.mult)
    VT(r=[wk[2], wk[0]], w=[wk[2]], out=ang[:], in0=ang[:], in1=dtv[:], op=ALU.subtract)
    VT(r=[wk[2], wk[7]], w=[wk[2]], out=ang[:], in0=ang[:], in1=den[:], op=ALU.mult)
    bre, bim = L_["sB_re"], L_["sB_im"]
    VT(r=[wk[1], "sB_re"], w=[wk[3]], out=sn[:], in0=mag[:], in1=bre[:], op=ALU.mult)
    VT(r=[wk[2], "sB_im"], w=[wk[0]], out=dtv[:], in0=ang[:], in1=bim[:], op=ALU.mult)
    VT(r=[wk[3], wk[0]], w=[wk[3]], out=sn[:], in0=sn[:], in1=dtv[:], op=ALU.subtract)
    VT(r=[wk[1], "sB_im"], w=[wk[4]], out=cs[:], in0=mag[:], in1=bim[:], op=ALU.mult)
    VT(r=[wk[2], "sB_re"], w=[wk[0]], out=dtv[:], in0=ang[:], in1=bre[:], op=ALU.mult)
    VT(r=[wk[4], wk[0]], w=[wk[4]], out=cs[:], in0=cs[:], in1=dtv[:], op=ALU.add)
    for ri, (src, sk) in enumerate(((sn, wk[3]), (cs, wk[4]))):
        for mp in range(2):
            C("vector", "tensor_scalar", r=[sk, "smask"], w=["WB"], out=WB[:, :, ri, mp, :], in0=src[:].rearrange("p (a c) -> p a c", c=64), scalar1=smask[:, mp:mp + 1], scalar2=None, op0=ALU.mult)
    VT(r=[wk[5], wk[3]], w=[wk[1]], out=mag[:], in0=lr[:], in1=sn[:], op=ALU.mult)
    VT(r=[wk[1], wk[3]], w=[wk[1]], out=mag[:], in0=mag[:], in1=sn[:], op=ALU.add)
    VT(r=[wk[6], wk[4]], w=[wk[0]], out=dtv[:], in0=li[:], in1=cs[:], op=ALU.mult)
    VT(r=[wk[1], wk[0]], w=[wk[1]], out=mag[:], in0=mag[:], in1=dtv[:], op=ALU.subtract)
    VT(r=[wk[5], wk[4]], w=[wk[2]], out=ang[:], in0=lr[:], in1=cs[:], op=ALU.mult)
    VT(r=[wk[2], wk[4]], w=[wk[2]], out=ang[:], in0=ang[:], in1=cs[:], op=ALU.add)
    VT(r=[wk[6], wk[3]], w=[wk[0]], out=dtv[:], in0=li[:], in1=sn[:], op=ALU.mult)
    VT(r=[wk[2], wk[0]], w=[wk[2]], out=ang[:], in0=ang[:], in1=dtv[:], op=ALU.add)
    for ri, (src, sk) in enumerate(((mag, wk[1]), (ang, wk[2]))):
        for mp in range(2):
            C("vector", "tensor_scalar", r=[sk, "smask"], w=["WB1"], out=WB1[:, :, ri, mp, :], in0=src[:].rearrange("p (a c) -> p a c", c=64), scalar1=smask[:, mp:mp + 1], scalar2=None, op0=ALU.mult)
    pre = P.sb("pre", [128, 32], F32); pim = P.sb("pim", [128, 32], F32); pdt = P.sb("pdt", [128, 32], F32)
    P.dma("sync", pre[:], T["pA_re"][:], w=["pre"], semkey="s4")
    P.dma("sync", pim[:], T["pA_im"][:], w=["pim"], semkey="s5")
    P.dma("sync", pdt[:], T["pA_dt"][:], w=["pdt"], semkey="s6")
    C("scalar", "activation", r=["pdt"], w=["pdt"], out=pdt[:], in_=pdt[:], func=AF.Exp)
    VT(r=["pre", "pdt"], w=["rho_s"], out=rho_s[:], in0=pre[:], in1=pdt[:], op=ALU.mult)
    C("scalar", "activation", r=["rho_s"], w=["rho_s"], out=rho_s[:], in_=rho_s[:], func=AF.Exp)
    VT(r=["pim", "pdt"], w=["th_s"], out=th_s[:], in0=pim[:], in1=pdt[:], op=ALU.mult)
    VT(r=["rho_s"], w=["rho2_s"], out=rho2_s[:], in0=rho_s[:], in1=rho_s[:], op=ALU.mult)
    C("vector", "tensor_scalar", r=["th_s"], w=["th2_s"], out=th2_s[:], in0=th_s[:], scalar1=2.0, scalar2=None, op0=ALU.mult)
    lrp = P.sb("lrp", [128, 32], F32); lip = P.sb("lip", [128, 32], F32)
    trig(th_s[:], "th_s", 32, lip[:], "lip", lrp[:], "lrp", 0, 1)
    VT(r=["lrp", "rho_s"], w=["lrp"], out=lrp[:], in0=lrp[:], in1=rho_s[:], op=ALU.mult)
    VT(r=["lip", "rho_s"], w=["lip"], out=lip[:], in0=lip[:], in1=rho_s[:], op=ALU.mult)
    ccr = P.sb("ccr", [128, 256], F32); cci = P.sb("cci", [128, 256], F32)
    P.dma("sync", ccr[:], T["cC_re"][:], w=["ccr"], semkey="s7")
    P.dma("sync", cci[:], T["cC_im"][:], w=["cci"], semkey="s8")
    for mp in range(2):
        C("vector", "tensor_scalar", r=["ccr", "cmask"], w=["WC"], out=WC[:, :, 0, mp, :], in0=ccr[:].rearrange("p (a c) -> p a c", c=16), scalar1=cmask[:, mp:mp + 1], scalar2=None, op0=ALU.mult)
        C("vector", "tensor_scalar", r=["cci", "cmask"], w=["WC"], out=WC[:, :, 1, mp, :], in0=cci[:].rearrange("p (a c) -> p a c", c=16), scalar1=cmask[:, mp:mp + 1], scalar2=-1.0, op0=ALU.mult, op1=ALU.mult)
    cta = P.sb("cta", [128, 16, 16], F32); ctb = P.sb("ctb", [128, 16, 16], F32)
    ccr3 = ccr[:].rearrange("p (a c) -> p a c", c=16); cci3 = cci[:].rearrange("p (a c) -> p a c", c=16)
    for d in range(2):
        lrb = lrp[:, d * 16:(d + 1) * 16].unsqueeze(2).to_broadcast([128, 16, 16])
        lib = lip[:, d * 16:(d + 1) * 16].unsqueeze(2).to_broadcast([128, 16, 16])
        VT(r=["ccr", "lrp"], w=["cta"], out=cta[:], in0=ccr3, in1=lrb, op=ALU.mult)
        VT(r=["cci", "lip"], w=["ctb"], out=ctb[:], in0=cci3, in1=lib, op=ALU.mult)
        VT(r=["cta", "ctb"], w=["cta"], out=cta[:], in0=cta[:], in1=ctb[:], op=ALU.subtract)
        for mp in range(2):
            C("vector", "tensor_scalar", r=["cta", "cmask"], w=["WCL"], out=WCL[:, d, :, 0, mp, :], in0=cta[:], scalar1=cmask[:, mp:mp + 1], scalar2=None, op0=ALU.mult)
        VT(r=["ccr", "lip"], w=["cta"], out=cta[:], in0=ccr3, in1=lib, op=ALU.mult)
        VT(r=["cci", "lrp"], w=["ctb"], out=ctb[:], in0=cci3, in1=lrb, op=ALU.mult)
        VT(r=["cta", "ctb"], w=["cta"], out=cta[:], in0=cta[:], in1=ctb[:], op=ALU.add)
        for mp in range(2):
            C("vector", "tensor_scalar", r=["cta", "cmask"], w=["WCL"], out=WCL[:, d, :, 1, mp, :], in0=cta[:], scalar1=cmask[:, mp:mp + 1], scalar2=-1.0, op0=ALU.mult, op1=ALU.mult)
    e32 = P.sb("e32", [128, 32], F32); dsk = P.sb("dsk", [128, 4], F32)
    P.dma("sync", e32[:], T["eye32"][:], w=["e32"], semkey="s9")
    P.dma("sync", dsk[:], T["dskip"][:], w=["dsk"], semkey="s10")
    for Tt in range(4):
        C("vector", "tensor_scalar", r=["e32", "dsk"], w=["WD"], out=WD[:, Tt, :], in0=e32[:], scalar1=dsk[:, Tt:Tt + 1], scalar2=None, op0=ALU.mult)
    WDf = P.sb("WDf", [128, 4, 32], F32)
    BTs = P.sb("BTs", [128, 2, 32], BF16)
    tbk = env["tbs"][0]
    ident_ = env["ident"]
    for Tt in range(4):
        C("vector", "tensor_scalar", r=["e32", "dsk"], w=["WDf"], out=WDf[:, Tt, :], in0=e32[:], scalar1=dsk[:, Tt:Tt + 1], scalar2=None, op0=ALU.mult)
    for d in range(2):
        for Tt in range(4):
            kb_ = banks[4 + (d * 4 + Tt) % 2]
            kbk = "bk%d" % (4 + (d * 4 + Tt) % 2)
            for j in range(4):
                Pp = 4 * Tt + j
                for ri in range(2):
                    C("tensor", "transpose", r=["WB", "ident"], w=["tb0"], out=tbk[:, ri * 32:(ri + 1) * 32], in_=WB[32 * j:32 * j + 32, d * 4 + Tt, ri, :, :].rearrange("p a c -> p (a c)"), identity=ident_[32 * j:32 * j + 32, 32 * j:32 * j + 32], tile_position=(32 * j, 0))
                C("vector", "tensor_copy", r=["tb0"], w=["BTs"], out=BTs[:].rearrange("p a c -> p (a c)"), in_=tbk[:, 0:64])
                for ri in range(2):
                    C("tensor", "matmul", r=["BTs", "WC"], w=[kbk], out=kb_[32 * j:32 * j + 32, 0:32], lhsT=BTs[:, ri, :], rhs=WC[:, Pp, ri, :, :].rearrange("p a c -> p (a c)"), start=(ri == 0), stop=(ri == 1), tile_position=(0, 32 * j), skip_group_check=True)
            C("vector", "tensor_tensor", r=[kbk, "WDf"], w=["WK"], out=WK[:, d, Tt, :], in0=kb_[:, 0:32], in1=WDf[:, Tt, :], op=ALU.add)
    P.release(md)
    tabc = [P.sb("tabc%d" % i, [128, 512], F32) for i in range(2)]
    tabs = [P.sb("tabs%d" % i, [128, 512], F32) for i in range(2)]
    tabr = [P.sb("tabr%d" % i, [128, 512], F32) for i in range(2)]
    gg = [[P.sb("gg%d%d" % (i, r), [128, 512], F32) for r in range(2)] for i in range(2)]
    hB = [[P.sb("hB%d%d" % (i, r), [128, 514], BF16) for r in range(2)] for i in range(2)]
    car = P.sb("car", [128, 2, 8], F32)
    tq2 = [[P.sb("tqw%d%d" % (i, r), [128, 512], F32) for r in range(4 - i)] for i in range(2)]
    tq2[1].append(tq[3])

    def make_tables(d, Pp, pi):
        col = d * 16 + Pp
        if b > 0:
            P.dma("sync", tabs[pi][:], T["tab_s"][col, 0], r=["tab_d%d" % col], w=["tabs%d" % pi], semkey="tls%d" % pi)
            P.dma("sync", tabc[pi][:], T["tab_s"][col, 1], r=["tab_d%d" % col], w=["tabc%d" % pi], semkey="tlc%d" % pi)
            C("vector", "tensor_scalar", r=["iot", "rho2_s"], w=["tabr%d" % pi], out=tabr[pi][:], in0=iot[:], scalar1=0.0, scalar2=rho2_s[:, col:col + 1], op0=ALU.mult, op1=ALU.add)
            return
        C("vector", "tensor_scalar", r=["iot", "th2_s"], w=["tq2"], out=tq[2][:], in0=iot[:], scalar1=th2_s[:, col:col + 1], scalar2=None, op0=ALU.mult)
        trig(tq[2][:], "tq2", 512, tabs[pi][:], "tabs%d" % pi, tabc[pi][:], "tabc%d" % pi, 0, 1)
        C("vector", "tensor_scalar", r=["iot", "rho2_s"], w=["tabr%d" % pi], out=tabr[pi][:], in0=iot[:], scalar1=0.0, scalar2=rho2_s[:, col:col + 1], op0=ALU.mult, op1=ALU.add)
        P.dma("sync", T["tab_s"][col, 0], tabs[pi][:], r=["tabs%d" % pi], w=["tab_d%d" % col], semkey="tss%d" % pi)
        P.dma("sync", T["tab_s"][col, 1], tabc[pi][:], r=["tabc%d" % pi], w=["tab_d%d" % col], semkey="tsc%d" % pi)

    FWD_BLOCKS = [(0, 512), (512, 512), (1024, 512), (1536, 512), (2048, 128)]
    BWD_BLOCKS = [(1792, 512), (1280, 512), (768, 512), (256, 512), (128, 128)]
    for i_ in range(2):
        for r_ in range(2):
            C("gpsimd", "memset", w=["hB%d" % i_], ap=hB[i_][r_][:], constant=0.0)
    for Tt in range(4):
        P.dma("sync", upg[:], T["uT_s"][b, Tt], r=["uT_d"], w=["upg"], semkey="upg")
        for pg in range(2):
            for d in range(2):
                for pi in range(2):
                    make_tables(d, 4 * Tt + 2 * pg + pi, pi)
                C("vector", "memset", w=["car0", "car1", "cw0", "cx0", "cy0", "cz0", "cw1", "cx1", "cy1", "cz1"], ap=car[:], constant=0.0)
                for kb, (c0, n) in enumerate(FWD_BLOCKS if d == 0 else BWD_BLOCKS):
                    rv = (lambda ap: ap) if d == 0 else (lambda ap: ap[:, ::-1])

                    def pair_ops(pi):
                        j = 2 * pg + pi
                        bre_k, bim_k = "bk%d" % (2 * pi), "bk%d" % (2 * pi + 1)
                        pre_, pim_ = banks[2 * pi], banks[2 * pi + 1]
                        cT, sT, rT = rv(tabc[pi][:, 0:n]), rv(tabs[pi][:, 0:n]), tabr[pi][:, 0:n]
                        ck, sk_, rk = "tabc%d" % pi, "tabs%d" % pi, "tabr%d" % pi
                        wa_, wb_ = tq2[pi][0], tq2[pi][1]
                        wak, wbk = "tqa%d" % pi, "tqb%d" % pi
                        wc_, wd_ = tq2[pi][2], tq2[pi][3]
                        wck, wdk = "tqc%d" % pi, "tqd%d" % pi
                        gr, gi = wa_, wc_
                        grk, gik = wak, wck
                        g0, g1 = gg[pi][0], gg[pi][1]
                        g0k, g1k = "gg%d0" % pi, "gg%d1" % pi
                        lo_ = 127 if (d == 0 and kb == 0) else 0
                        if d == 0:
                            hre, him = hF[:, pi, 0, c0 + lo_ - 127:c0 + n - 127], hF[:, pi, 1, c0 + lo_ - 127:c0 + n - 127]
                            hk = "hF%d" % pi
                        else:
                            hre, him = hB[pi][0][:, 0:n], hB[pi][1][:, 0:n]
                            hk = "hB%d" % pi
                        cTd, sTd = rv(tabc[pi][:, 0:n])[:, lo_:n], rv(tabs[pi][:, 0:n])[:, lo_:n]
                        lc = n - 1 if d == 0 else 0
                        tcn = n - 1
                        cl, sl_ = tabc[pi][:, tcn:tcn + 1], tabs[pi][:, tcn:tcn + 1]
                        a0, a1 = g0[:, lc:lc + 1], g1[:, lc:lc + 1]
                        GT = lambda *a, **k: C("gpsimd", "tensor_tensor", *a, **k)
                        ev = upg[32 * j:32 * j + 32, 2 * c0:2 * c0 + 2 * n:2]
                        od = upg[32 * j:32 * j + 32, 2 * c0 + 1:2 * c0 + 2 * n:2]
                        first, second = (ev, od) if d == 0 else (od, ev)
                        ops = []
                        for ri, bk_, bkk in ((0, pre_, bre_k), (1, pim_, bim_k)):
                            ops.append(lambda ri=ri, bk_=bk_, bkk=bkk: C("tensor", "matmul", r=["WB1", "upg"], w=[bkk], out=bk_[:, 0:n], lhsT=WB1[32 * j:32 * j + 32, d * 4 + Tt, ri, :, :].rearrange("p a c -> p (a c)"), rhs=first, start=True, stop=False, tile_position=(32 * j, 0)))
                            ops.append(lambda ri=ri, bk_=bk_, bkk=bkk: C("tensor", "matmul", r=["WB", "upg"], w=[bkk], out=bk_[:, 0:n], lhsT=WB[32 * j:32 * j + 32, d * 4 + Tt, ri, :, :].rearrange("p a c -> p (a c)"), rhs=second, start=False, stop=True, tile_position=(32 * j, 0)))
                        if d == 1:
                            ops.append(lambda: C("gpsimd", "tensor_copy", r=[hk], w=[hk], out=hB[pi][0][:, n:n + 1], in_=hB[pi][0][:, 0:1]))
                            ops.append(lambda: C("gpsimd", "tensor_copy", r=[hk], w=[hk], out=hB[pi][1][:, n:n + 1], in_=hB[pi][1][:, 0:1]))
                        ops += [
                            lambda: VT(r=[bre_k, ck], w=[wak], out=wa_[:, 0:n], in0=pre_[:, 0:n], in1=cT, op=ALU.mult),
                            lambda: VT(r=[bim_k, sk_], w=[wbk], out=wb_[:, 0:n], in0=pim_[:, 0:n], in1=sT, op=ALU.mult),
                            lambda: VT(r=[bim_k, ck], w=[wck], out=wc_[:, 0:n], in0=pim_[:, 0:n], in1=cT, op=ALU.mult),
                            lambda: VT(r=[bre_k, sk_], w=[wdk], out=wd_[:, 0:n], in0=pre_[:, 0:n], in1=sT, op=ALU.mult),
                            lambda: VT(r=[wak, wbk], w=[grk], out=gr[:, 0:n], in0=wa_[:, 0:n], in1=wb_[:, 0:n], op=ALU.add),
                            lambda: VT(r=[wck, wdk], w=[gik], out=gi[:, 0:n], in0=wc_[:, 0:n], in1=wd_[:, 0:n], op=ALU.subtract),
                            lambda: C("vector", "tensor_tensor_scan", r=[rk, grk, "car%d" % pi], w=[g0k], out=rv(g0[:, 0:n]), data0=rT, data1=rv(gr[:, 0:n]), initial=car[:, pi, 0:1], op0=ALU.mult, op1=ALU.add),
                            lambda: C("vector", "tensor_tensor_scan", r=[rk, gik, "car%d" % pi], w=[g1k], out=rv(g1[:, 0:n]), data0=rT, data1=rv(gi[:, 0:n]), initial=car[:, pi, 1:2], op0=ALU.mult, op1=ALU.add),
                            lambda: VT(r=[g0k, ck], w=["cw%d" % pi], out=car[:, pi, 2:3], in0=a0, in1=cl, op=ALU.mult),
                            lambda: VT(r=[g1k, sk_], w=["cx%d" % pi], out=car[:, pi, 3:4], in0=a1, in1=sl_, op=ALU.mult),
                            lambda: VT(r=[g1k, ck], w=["cy%d" % pi], out=car[:, pi, 4:5], in0=a1, in1=cl, op=ALU.mult),
                            lambda: VT(r=[g0k, sk_], w=["cz%d" % pi], out=car[:, pi, 5:6], in0=a0, in1=sl_, op=ALU.mult),
                            lambda: VT(r=["cw%d" % pi, "cx%d" % pi], w=["car%d" % pi], out=car[:, pi, 0:1], in0=car[:, pi, 2:3], in1=car[:, pi, 3:4], op=ALU.subtract),
                            lambda: VT(r=["cy%d" % pi, "cz%d" % pi, "car%d" % pi], w=["car%d" % pi], out=car[:, pi, 1:2], in0=car[:, pi, 4:5], in1=car[:, pi, 5:6], op=ALU.add),
                            lambda: GT(r=[g0k, ck], w=[wbk], out=wb_[:, lo_:n], in0=g0[:, lo_:n], in1=cTd, op=ALU.mult),
                            lambda: GT(r=[g1k, sk_], w=[wdk], out=wd_[:, lo_:n], in0=g1[:, lo_:n], in1=sTd, op=ALU.mult),
                            lambda: GT(r=[wbk, wdk], w=[hk], out=hre, in0=wb_[:, lo_:n], in1=wd_[:, lo_:n], op=ALU.subtract),
                            lambda: GT(r=[g1k, ck], w=[wbk], out=wb_[:, lo_:n], in0=g1[:, lo_:n], in1=cTd, op=ALU.mult),
                            lambda: GT(r=[g0k, sk_], w=[wdk], out=wd_[:, lo_:n], in0=g0[:, lo_:n], in1=sTd, op=ALU.mult),
                            lambda: GT(r=[wbk, wdk], w=[hk], out=him, in0=wb_[:, lo_:n], in1=wd_[:, lo_:n], op=ALU.add),
                        ]
                        return ops

                    opl = [pair_ops(0), pair_ops(1)]
                    for k_ in range(len(opl[0])):
                        for pi in range(2):
                            opl[pi][k_]()
                    if d == 1:
                        cl0, cl1 = max(c0, 128), min(c0 + n, 2176)
                        nl = cl1 - cl0
                        o = cl0 - c0
                        for eo in range(2):
                            yb = banks[4 + eo]
                            ybk = "bk%d" % (4 + eo)
                            for pi in range(2):
                                j = 2 * pg + pi
                                Pp = 4 * Tt + j
                                outp = yb[32 * j:32 * j + 32, 0:nl]
                                w2 = lambda t, ri: t[:, Pp, ri, :, :].rearrange("p a c -> p (a c)")
                                wl = lambda dd, ri: WCL[:, dd, Pp, ri, :, :].rearrange("p a c -> p (a c)")
                                if eo == 0:
                                    terms = [(wl(0, 0), hF[:, pi, 0, cl0 - 128:cl1 - 128], "hF%d" % pi), (wl(0, 1), hF[:, pi, 1, cl0 - 128:cl1 - 128], "hF%d" % pi),
                                             (w2(WC, 0), hB[pi][0][:, o:o + nl], "hB%d" % pi), (w2(WC, 1), hB[pi][1][:, o:o + nl], "hB%d" % pi)]
                                    urhs = upg[32 * j:32 * j + 32, 2 * cl0:2 * cl1:2]
                                else:
                                    terms = [(w2(WC, 0), hF[:, pi, 0, cl0 - 127:cl1 - 127], "hF%d" % pi), (w2(WC, 1), hF[:, pi, 1, cl0 - 127:cl1 - 127], "hF%d" % pi),
                                             (wl(1, 0), hB[pi][0][:, o + 1:o + 1 + nl], "hB%d" % pi), (wl(1, 1), hB[pi][1][:, o + 1:o + 1 + nl], "hB%d" % pi)]
                                    urhs = upg[32 * j:32 * j + 32, 2 * cl0 + 1:2 * cl1:2]
                                for ti, (lw, rh, rkey) in enumerate(terms):
                                    C("tensor", "matmul", r=["WC", "WCL", rkey], w=[ybk], out=outp, lhsT=lw, rhs=rh, start=(ti == 0), stop=False, tile_position=(0, 32 * j), skip_group_check=True)
                                C("tensor", "matmul", r=["WK", "upg"], w=[ybk], out=outp, lhsT=WK[32 * j:32 * j + 32, eo, Tt, :], rhs=urhs, start=False, stop=True, tile_position=(32 * j, 32 * j), skip_group_check=True)
                            rows = slice(64 * pg, 64 * pg + 64)
                            yv = yb[rows, 0:nl]
                            tw = tq[eo]
                            twk = "tq%d" % eo
                            C("scalar", "activation", r=[ybk], w=[twk], out=tw[rows, 0:nl], in_=yv, func=AF.Square)
                            C("vector", "tensor_scalar", r=[twk], w=[twk], out=tw[rows, 0:nl], in0=tw[rows, 0:nl], scalar1=0.044715, scalar2=1.0, op0=ALU.mult, op1=ALU.add)
                            VT(r=[twk, ybk], w=[twk], out=tw[rows, 0:nl], in0=tw[rows, 0:nl], in1=yv, op=ALU.mult)
                            C("scalar", "activation", r=[twk], w=[twk], out=tw[rows, 0:nl], in_=tw[rows, 0:nl], func=AF.Sigmoid, scale=2.0 * math.sqrt(2.0 / PI))
                            VT(r=[twk, ybk], w=["ygT"], out=ygT[rows, Tt, 2 * cl0 - 256 + eo:2 * cl1 - 256:2], in0=tw[rows, 0:nl], in1=yv, op=ALU.mult)
    zc = 0
    for n_ in range(4):
        for blk in range(8):
            zb = banks[zc % 4]
            zk = "bk%d" % (zc % 4)
            sgi = zc % 2
            zc += 1
            for k in range(4):
                C("tensor", "matmul", r=["wglu", "ygT"], w=[zk], out=zb[:], lhsT=wglu[:, k, n_ * 128:(n_ + 1) * 128], rhs=ygT[:, k, blk * 512:(blk + 1) * 512], start=(k == 0), stop=(k == 3))
            C("scalar", "activation", r=[zk, "bglu"], w=["tq%d" % sgi], out=tq[sgi][:], in_=zb[:], func=AF.Sigmoid, bias=bglu[:, n_:n_ + 1], scale=1.0)
            C("vector", "tensor_tensor", r=["tq%d" % sgi, "ygT"], w=["catS"], out=catS[:, n_, blk * 512:(blk + 1) * 512], in0=tq[sgi][:], in1=ygT[:, n_, blk * 512:(blk + 1) * 512], op=ALU.mult)


_NC_CACHE = {}


def host_layouts(inp):
    f = np.float32
    L = {}
    L["w_ada"] = np.ascontiguousarray(inp["w_ada"][0]); L["b_ada"] = np.ascontiguousarray(np.broadcast_to(inp["b_ada"], (3, 6 * D)))
    rep = lambda v, n: np.ascontiguousarray(np.broadcast_to(np.tile(np.asarray(v, np.float32).reshape(1, -1), (1, n)), (128, v.size * n)))
    L["norm1_g"] = rep(inp["norm1_g"], 1); L["norm2_g"] = rep(inp["norm2_g"], 1)
    sel = np.zeros((3, 3, 128), np.float32)
    for r_ in range(3):
        sel[r_, r_, :] = 1.0
    L["sel3"] = sel.reshape(3, 384)
    L["w_in"] = np.ascontiguousarray(inp["w_in"][0])
    L["qg"] = rep(inp["q_norm_g"], 8); L["kg"] = rep(inp["k_norm_g"], 8)
    L["lamv"] = rep(np.concatenate([inp["lambda_q1"], inp["lambda_k1"], inp["lambda_q2"], inp["lambda_k2"]], axis=1).astype(f), 1)
    L["subln"] = rep(inp["subln_g"], 4)
    rows = SEQ // 64
    row = np.repeat(np.arange(rows, dtype=f), 64); col = np.tile(np.arange(64, dtype=f), rows)
    inv = (10000.0 ** (-np.arange(0, 32, 2, dtype=f) / 32)).astype(f)
    ang = np.stack([row[:, None] * inv, col[:, None] * inv], axis=1).astype(f)
    cs = np.cos(ang).astype(f); sn = np.sin(ang).astype(f)
    full = lambda t: np.ascontiguousarray(np.broadcast_to(t[:, None, :, None, :], (SEQ, 8, 2, 2, 16)).reshape(SEQ, 512))
    L["rope_cos"] = full(cs); L["rope_sin"] = full(sn)
    L["ident"] = np.eye(128, dtype=f)
    L["w_glu"] = np.ascontiguousarray(inp["w_glu"][0]); L["b_gluT"] = np.ascontiguousarray(inp["b_glu"][0].reshape(4, 128).T)
    L["w_out"] = np.ascontiguousarray(inp["w_out"][0])
    L["w_r"] = np.ascontiguousarray(np.concatenate([inp["w_route_group"][0], inp["w_route_expert"][0]], axis=1))
    L["b_r"] = rep(np.concatenate([inp["b_route_group"], inp["b_route_expert"]], axis=1), 1)
    L["w_eg"] = np.ascontiguousarray(inp["w_exp_gate"][0]); L["w_eu"] = np.ascontiguousarray(inp["w_exp_up"][0]); L["w_ed"] = np.ascontiguousarray(inp["w_exp_down"][0])
    a_re, a_im, ldt = inp["ssm_a_re"][0], inp["ssm_a_im"][0], inp["ssm_log_dt"][0]
    b_re, b_im = inp["ssm_b_re"][0], inp["ssm_b_im"][0]
    c_re, c_im, dsk = inp["ssm_c_re"][0], inp["ssm_c_im"][0], inp["ssm_d"][0]
    sA_re = np.zeros((128, 2, 4, 64), f); sA_im = np.zeros_like(sA_re); sA_dt = np.zeros_like(sA_re)
    sB_re = np.zeros_like(sA_re); sB_im = np.zeros_like(sA_re)
    sMask = np.zeros((128, 2), f); dskl = np.zeros((128, 4), f)
    for j in range(4):
        for m in range(2):
            for h in range(16):
                q = 32 * j + 16 * m + h
                sMask[q, m] = 1.0
                for Tt in range(4):
                    g = 8 * Tt + 2 * j + m
                    dskl[q, Tt] = dsk[g, h]
                    for d in range(2):
                        sA_re[q, d, Tt] = a_re[d, g]; sA_im[q, d, Tt] = a_im[d, g]; sA_dt[q, d, Tt] = ldt[d, g]
                        sB_re[q, d, Tt] = b_re[d, g, :, h]; sB_im[q, d, Tt] = b_im[d, g, :, h]
    L["sA_re"] = sA_re.reshape(128, 512); L["sA_im"] = sA_im.reshape(128, 512); L["sA_dt"] = sA_dt.reshape(128, 512)
    L["sB_re"] = sB_re.reshape(128, 512); L["sB_im"] = sB_im.reshape(128, 512); L["sMask"] = sMask; L["dskip"] = dskl
    pA_re = np.zeros((128, 2, 16), f); pA_im = np.zeros_like(pA_re); pA_dt = np.zeros_like(pA_re)
    cC_re = np.zeros((128, 16, 16), f); cC_im = np.zeros_like(cC_re); cMask = np.zeros((128, 2), f)
    for m in range(2):
        for p in range(64):
            q = 64 * m + p
            cMask[q, m] = 1.0
            for Pp in range(16):
                g = 2 * Pp + m
                cC_re[q, Pp] = c_re[g, :, p]; cC_im[q, Pp] = c_im[g, :, p]
                for d in range(2):
                    pA_re[q, d, Pp] = a_re[d, g, p]; pA_im[q, d, Pp] = a_im[d, g, p]; pA_dt[q, d, Pp] = ldt[d, g]
    L["pA_re"] = pA_re.reshape(128, 32); L["pA_im"] = pA_im.reshape(128, 32); L["pA_dt"] = pA_dt.reshape(128, 32)
    L["cC_re"] = cC_re.reshape(128, 256); L["cC_im"] = cC_im.reshape(128, 256); L["cMask"] = cMask
    L["eye32"] = np.tile(np.eye(32, dtype=f), (4, 1)); L["iota1"] = np.tile(np.arange(1, 513, dtype=f)[None, :], (128, 1))
    return L


def kernel(**inp):
    inp = {k: np.asarray(v) for k, v in inp.items()}
    if "nc" not in _NC_CACHE:
        _NC_CACHE["nc"] = build_program()
    nc = _NC_CACHE["nc"]
    L = host_layouts(inp)
    in_maps = []
    for c in range(8):
        m = dict(L)
        m["x"] = np.ascontiguousarray(inp["x"][NB * c:NB * (c + 1)].reshape(NB * SEQ, D))
        m["ctx"] = np.ascontiguousarray(inp["ctx"][NB * c:NB * (c + 1)].reshape(NB * CTX, D))
        cv = np.concatenate([inp["c"][NB * c:NB * (c + 1)], inp["c_ctx"][None, :]], axis=0)
        m["cvT"] = np.ascontiguousarray(cv.reshape(3, 8, 128).transpose(2, 1, 0))
        in_maps.append(m)
    res = run_bass_kernel_spmd(nc, in_maps, core_ids=list(range(8)))
    out = np.concatenate([r["out"].reshape(NB, SEQ, D) for r in res.results], axis=0)
    return out.astype(np.float32)
```

```python
import math
import numpy as np
import concourse.bass as bass
import concourse.mybir as mybir
from concourse.bass_utils import run_bass_kernel_spmd

F32 = mybir.dt.float32
BF16 = mybir.dt.bfloat16
I32 = mybir.dt.int32
ALU = mybir.AluOpType
AF = mybir.ActivationFunctionType
AX = mybir.AxisListType
ENGS = ("sync", "scalar", "vector", "gpsimd", "tensor")

NB = 2
SEQ = 4096
CTX = 256
D = 1024
NT = 34
TAU = 4608
EPS = 1e-6
PI = math.pi
LAM_INIT = 0.8 - 0.6 * math.exp(0.0)


class Prog:
    def __init__(self, nc):
        self.nc = nc
        self.ops = []
        self.last_w = {}
        self.readers = {}
        self.ctx = []
        self.last_eng = {}
        self.last_sem = {}

    def sb(self, name, shape, dt):
        self.uid = getattr(self, "uid", 0) + 1
        cm = self.nc.sbuf_tensor("s%d_%s" % (self.uid, name), shape, dt)
        t = cm.__enter__()
        self.ctx.append(cm)
        return t

    def ps(self, name, shape, dt):
        self.uid = getattr(self, "uid", 0) + 1
        cm = self.nc.psum_tensor("p%d_%s" % (self.uid, name), shape, dt)
        t = cm.__enter__()
        self.ctx.append(cm)
        return t

    def mark(self):
        return len(self.ctx)

    def release(self, mark):
        self.barrier()
        while len(self.ctx) > mark:
            self.ctx.pop().__exit__(None, None, None)

    def op(self, eng, fn, r=(), w=(), dma=False, semkey=None):
        i = len(self.ops)
        deps = set()
        for k in list(r) + list(w):
            if k in self.last_w:
                deps.add(self.last_w[k])
        for k in w:
            for q in self.readers.get(k, ()):
                deps.add(q)
        self.ops.append(dict(eng=eng, fn=fn, deps=deps, dma=dma, semkey=semkey))
        for k in w:
            self.last_w[k] = i
            self.readers[k] = []
        for k in r:
            self.readers.setdefault(k, []).append(i)
        if dma:
            self.last_sem[semkey] = i
        else:
            self.last_eng[eng] = i
        return i

    def call(self, eng, name, r=(), w=(), **kw):
        return self.op(eng, lambda e: getattr(e, name)(**kw), r=r, w=w)

    def dma(self, eng, out, in_, r=(), w=(), semkey=None, **kw):
        return self.op(eng, lambda e: e.dma_start(out=out, in_=in_, **kw), r=r, w=w, dma=True, semkey=semkey)

    def barrier(self):
        deps = set(self.last_eng.values()) | set(self.last_sem.values())
        for e in ENGS:
            i = len(self.ops)
            self.ops.append(dict(eng=e, fn=None, deps=set(deps), dma=False, semkey=None))
        self.last_w = {}
        self.readers = {}

    def emit(self, final_wait_ops=()):
        nc = self.nc
        ops = self.ops
        eng_sem = {}
        for e in ENGS:
            cm = nc.semaphore("p_" + e)
            eng_sem[e] = cm.__enter__()
            self.ctx.append(cm)
        needed = set(final_wait_ops)
        for o in ops:
            needed |= o["deps"]
        dma_sem = {}
        cnt = {e: 0 for e in ENGS}
        dcnt = {}
        for i, o in enumerate(ops):
            o["sem"] = None
            if o["fn"] is None:
                continue
            if o["dma"]:
                k = o["semkey"]
                if k not in dma_sem:
                    cm = nc.semaphore("d_%d" % len(dma_sem))
                    dma_sem[k] = cm.__enter__()
                    self.ctx.append(cm)
                    dcnt[k] = 0
                dcnt[k] += 16
                o["sem"] = dma_sem[k]
                o["ticket"] = dcnt[k]
                o["inc"] = 16
            elif i in needed:
                cnt[o["eng"]] += 1
                o["sem"] = eng_sem[o["eng"]]
                o["ticket"] = cnt[o["eng"]]
                o["inc"] = 1
        self.n_sems = len(dma_sem) + len(ENGS)
        per_eng = {e: [] for e in ENGS}
        for i, o in enumerate(ops):
            per_eng[o["eng"]].append(i)

        def run_engine(ename, eobj):
            waited = {}
            for i in per_eng[ename]:
                o = ops[i]
                for d in sorted(o["deps"]):
                    p = ops[d]
                    if p["fn"] is None:
                        continue
                    if p["eng"] == ename and not p["dma"] and ename == "tensor":
                        continue
                    sem = p["sem"]
                    key = id(sem)
                    if waited.get(key, 0) >= p["ticket"]:
                        continue
                    eobj.wait_ge(sem, p["ticket"])
                    waited[key] = p["ticket"]
                if o["fn"] is None:
                    continue
                ins = o["fn"](eobj)
                if o["sem"] is not None:
                    ins.then_inc(o["sem"], o["inc"])
            if ename == "sync":
                for d in final_wait_ops:
                    p = ops[d]
                    eobj.wait_ge(p["sem"], p["ticket"])

        with nc.Block() as block:
            @block.sync
            def _(e):
                run_engine("sync", e)

            @block.scalar
            def _(e):
                run_engine("scalar", e)

            @block.vector
            def _(e):
                run_engine("vector", e)

            @block.gpsimd
            def _(e):
                run_engine("gpsimd", e)

            @block.tensor
            def _(e):
                run_engine("tensor", e)

    def close(self):
        while self.ctx:
            self.ctx.pop().__exit__(None, None, None)


def build_program(dbg=None):
    nc = bass.Bass("TRN2", target_bir_lowering=False)
    T = {}

    def din(name, shape, dt=F32):
        T[name] = nc.dram_tensor(name, list(shape), dt, kind="ExternalInput").ap()
        return T[name]

    def dint(name, shape, dt):
        T[name] = nc.dram_tensor(name, list(shape), dt, kind="Internal").ap()
        return T[name]

    x_d = din("x", [NB * SEQ, D])
    ctx_d = din("ctx", [NB * CTX, D])
    cvT_d = din("cvT", [128, 8, 3])
    wada_d = din("w_ada", [D, 6 * D])
    bada_d = din("b_ada", [3, 6 * D])
    n1g_d = din("norm1_g", [128, D])
    n2g_d = din("norm2_g", [128, D])
    sel3_d = din("sel3", [3, 3 * 128])
    win_d = din("w_in", [D, 2048])
    qg_d = din("qg", [128, 512])
    kg_d = din("kg", [128, 512])
    lam_d = din("lamv", [128, 256])
    sub_d = din("subln", [128, 512])
    ropec_d = din("rope_cos", [SEQ, 512])
    ropes_d = din("rope_sin", [SEQ, 512])
    ident_d = din("ident", [128, 128])
    sA_re = din("sA_re", [128, 512]); sA_im = din("sA_im", [128, 512]); sA_dt = din("sA_dt", [128, 512])
    sB_re = din("sB_re", [128, 512]); sB_im = din("sB_im", [128, 512]); sMask = din("sMask", [128, 2])
    pA_re = din("pA_re", [128, 32]); pA_im = din("pA_im", [128, 32]); pA_dt = din("pA_dt", [128, 32])
    cC_re = din("cC_re", [128, 256]); cC_im = din("cC_im", [128, 256]); cMask = din("cMask", [128, 2])
    dsk_d = din("dskip", [128, 4]); eye32_d = din("eye32", [128, 32]); iota_d = din("iota1", [128, 512])
    wglu_d = din("w_glu", [512, 512]); bglu_d = din("b_gluT", [128, 4])
    wout_d = din("w_out", [D, D])
    wr_d = din("w_r", [D, 36]); br_d = din("b_r", [128, 36])
    weg_d = din("w_eg", [32, D, 512]); weu_d = din("w_eu", [32, D, 512]); wed_d = din("w_ed", [32, 512, D])
    out_d = nc.dram_tensor("out", [NB * SEQ, D], F32, kind="ExternalOutput").ap()
    mod_d = dint("mod_s", [3, 128, 6 * D], F32)
    qT_d = dint("qT_s", [NB, 4, 128, SEQ], BF16)
    uT_d = dint("uT_s", [NB, 4, 128, TAU], BF16)
    dint("tab_s", [32, 2, 128, 512], F32)
    x2T_d = dint("x2T_s", [NB * SEQ // 2048, 128, 8, 2048], BF16)
    dbg_t = {}
    if dbg:
        for name, shape in dbg.items():
            dbg_t[name] = nc.dram_tensor("dbg_" + name, list(shape), F32, kind="ExternalOutput").ap()

    P = Prog(nc)
    C = P.call
    fin = []

    ident = P.sb("ident", [128, 128], BF16)
    identf = P.sb("identf", [128, 128], F32)
    epst = P.sb("epst", [128, 1], F32)
    gates = P.sb("gates", [128, NB * 32, 32], F32)
    neglam = P.sb("neglam", [128, 1], F32)
    banks = [P.ps("bk%d" % i, [128, 512], F32) for i in range(6)]
    tbs = [P.ps("tb%d" % i, [128, 1024], BF16) for i in range(2)]
    P.dma("sync", identf[:], ident_d[:], w=["identf"], semkey="c0")
    C("vector", "tensor_copy", r=["identf"], w=["ident"], out=ident[:], in_=identf[:])
    C("vector", "memset", w=["epst"], ap=epst[:], constant=EPS)

    def rsqrt_rows(src, dst, scale, n, rk, wk):
        C("scalar", "activation", r=rk + ["epst"], w=wk, out=dst, in_=src, func=AF.Sqrt, bias=epst[:, 0:1], scale=scale)
        C("vector", "reciprocal", r=wk, w=wk, out=dst, in_=dst)

    m0 = P.mark()
    cvT = P.sb("cvT", [128, 8, 3], F32)
    cvS = P.sb("cvS", [128, 8, 3], BF16)
    wa = [P.sb("wa%d" % i, [128, 8, 512], BF16) for i in range(2)]
    modsb = P.sb("modsb", [3, 6 * D], F32)
    bad = P.sb("bad", [3, 6 * D], F32)
    lamt = P.sb("lamt", [128, 256], F32)
    lamw = P.sb("lamw", [128, 128], F32)
    lams = P.sb("lams", [128, 4], F32)
    P.dma("sync", cvT[:], cvT_d[:], w=["cvT"], semkey="c1")
    P.dma("sync", bad[:], bada_d[:], w=["bad"], semkey="c2")
    P.dma("sync", lamt[:], lam_d[:], w=["lamt"], semkey="c3")
    sel3f = P.sb("sel3f", [3, 384], F32); sel3 = P.sb("sel3", [3, 384], BF16)
    mhi = P.sb("mhi", [3, 6 * D], BF16); mlo = P.sb("mlo", [3, 6 * D], BF16)
    mstg = [P.sb("mstg%d" % i, [128, 512], F32) for i in range(2)]
    P.dma("sync", sel3f[:], sel3_d[:], w=["sel3f"], semkey="c2b")
    C("vector", "tensor_copy", r=["sel3f"], w=["sel3"], out=sel3[:], in_=sel3f[:])
    C("scalar", "activation", r=["cvT"], w=["cvS"], out=cvS[:], in_=cvT[:], func=AF.Silu)
    wada_v = wada_d.rearrange("(k p) n -> p k n", p=128)
    for cb in range(12):
        s = cb % 2
        P.dma("gpsimd", wa[s][:], wada_v[:, :, cb * 512:(cb + 1) * 512], w=["wa%d" % s], semkey="wa%d" % s)
        for k in range(8):
            C("tensor", "matmul", r=["wa%d" % s, "cvS"], w=["bk0"], out=banks[0][0:3, :], lhsT=cvS[:, k, :], rhs=wa[s][:, k, :], start=(k == 0), stop=(k == 7))
        C("vector", "tensor_tensor", r=["bk0", "bad"], w=["modsb"], out=modsb[:, cb * 512:(cb + 1) * 512], in0=banks[0][0:3, :], in1=bad[:, cb * 512:(cb + 1) * 512], op=ALU.add)
    C("vector", "tensor_copy", r=["modsb"], w=["mhi"], out=mhi[:], in_=modsb[:])
    C("vector", "tensor_tensor", r=["modsb", "mhi"], w=["mlo"], out=mlo[:], in0=modsb[:], in1=mhi[:], op=ALU.subtract)
    bc = 0
    for r_ in range(3):
        for cb in range(12):
            st = bc % 2
            bk = 1 + bc % 2
            bc += 1
            C("tensor", "matmul", r=["sel3", "mhi"], w=["bk%d" % bk], out=banks[bk][:], lhsT=sel3[:, r_ * 128:(r_ + 1) * 128], rhs=mhi[:, cb * 512:(cb + 1) * 512], start=True, stop=False)
            C("tensor", "matmul", r=["sel3", "mlo"], w=["bk%d" % bk], out=banks[bk][:], lhsT=sel3[:, r_ * 128:(r_ + 1) * 128], rhs=mlo[:, cb * 512:(cb + 1) * 512], start=False, stop=True)
            C("vector", "tensor_copy", r=["bk%d" % bk], w=["mstg%d" % st], out=mstg[st][:], in_=banks[bk][:])
            P.dma("sync", mod_d[r_, :, cb * 512:(cb + 1) * 512], mstg[st][:], r=["mstg%d" % st], w=["mod_d"], semkey="mstg%d" % st)
    C("vector", "tensor_tensor", r=["lamt"], w=["lamw"], out=lamw[:, 0:64], in0=lamt[:, 0:64], in1=lamt[:, 64:128], op=ALU.mult)
    C("vector", "tensor_tensor", r=["lamt"], w=["lamw"], out=lamw[:, 64:128], in0=lamt[:, 128:192], in1=lamt[:, 192:256], op=ALU.mult)
    C("vector", "tensor_reduce", r=["lamw"], w=["lams"], out=lams[:, 0:2], in_=lamw[:].rearrange("p (a b) -> p a b", b=64), axis=AX.X, op=ALU.add)
    C("scalar", "activation", r=["lams"], w=["lams"], out=lams[:, 2:4], in_=lams[:, 0:2], func=AF.Exp)
    C("vector", "tensor_tensor", r=["lams"], w=["lams"], out=lams[:, 0:1], in0=lams[:, 3:4], in1=lams[:, 2:3], op=ALU.subtract)
    C("vector", "tensor_scalar", r=["lams"], w=["neglam"], out=neglam[:], in0=lams[:, 0:1], scalar1=-LAM_INIT, scalar2=None, op0=ALU.add)
    P.release(m0)

    def load_mod_bcast(dst, key, row, chunk, semkey):
        P.dma("sync", dst[:], mod_d[row, :, chunk * D:(chunk + 1) * D], r=["mod_d"], w=[key], semkey=semkey)

    def norm_mod(xt, xk, A, Ak, B, Bk, outt, outk, W):
        sq, sqk, ss, ssk, t_, tk = W
        C("scalar", "activation", r=[xk], w=[sqk], out=sq[:], in_=xt, func=AF.Square)
        C("vector", "tensor_reduce", r=[sqk], w=[ssk], out=ss[:, 0:1], in_=sq[:], axis=AX.X, op=ALU.add)
        rsqrt_rows(ss[:, 0:1], ss[:, 1:2], 1.0 / D, 1, [ssk], [ssk])
        C("vector", "scalar_tensor_tensor", r=[xk, ssk, Ak], w=[tk], out=t_[:], in0=xt, scalar=ss[:, 1:2], in1=A[:], op0=ALU.mult, op1=ALU.mult)
        C("gpsimd", "tensor_tensor", r=[tk, Bk], w=[outk], out=outt, in0=t_[:], in1=B[:], op=ALU.add)

    nm_sq = P.sb("nm_sq", [128, D], F32)
    nm_ss = P.sb("nm_ss", [128, 2], F32)
    nm_ss2 = P.sb("nm_ss2", [128, 2], F32)
    nm_t = P.sb("nm_t", [128, D], F32)

    for b in range(NB):
        mb = P.mark()
        catA = P.sb("catA", [128, 4, SEQ], BF16)
        m1 = P.mark()
        KT = P.sb("KT", [128, 4, NT * 128], BF16)
        Vaug = P.sb("Vaug", [128, NT, 4, 130], BF16)
        mA = P.mark()
        winb = P.sb("winb", [128, 8, 2048], BF16)
        A1 = P.sb("A1", [128, D], F32); B1 = P.sb("B1", [128, D], F32)
        A1c = P.sb("A1c", [128, D], F32); B1c = P.sb("B1c", [128, D], F32)
        g1b = nm_sq
        Gq = P.sb("Gq", [128, 8, 64], F32); Gk = P.sb("Gk", [128, 8, 64], F32)
        xts = [P.sb("xt%d" % i, [128, D], F32) for i in range(2)]
        xmbs = [P.sb("xmb%d" % i, [128, D], BF16) for i in range(2)]
        NW = [(nm_sq, "nm_sq", nm_ss, "nm_ss", nm_t, "nm_t")] * 2
        xmT = [P.sb("xmT%d" % i, [128, 8, 128], BF16) for i in range(2)]
        sqt = P.sb("sqt", [128, 512], F32)
        ssq = P.sb("ssq", [128, 16], F32)
        ssq2 = P.sb("ssq2", [128, 16], F32)
        qn = P.sb("qn", [128, 512], F32)
        qn2 = P.sb("qn2", [128, 512], F32)
        qr = P.sb("qr", [128, 512], BF16)
        rt = [P.sb("rt%d" % i, [128, 512], F32) for i in range(4)]
        rcs = [P.sb("rc0", [128, 512], F32)] * 2; rss = [P.sb("rs_0", [128, 512], F32)] * 2
        usb = P.sb("usb", [128, 4, 128], BF16)
        qTs = P.sb("qTs", [128, 4, 128], BF16)
        P.dma("gpsimd", winb[:], win_d.rearrange("(k p) n -> p k n", p=128), w=["winb"], semkey="winb")
        P.dma("sync", g1b[:], n1g_d[:], w=["nm_sq"], semkey="c5")
        load_mod_bcast(B1, "B1", b, 0, "c6"); load_mod_bcast(A1, "A1", b, 1, "c7")
        load_mod_bcast(B1c, "B1c", 2, 0, "c8"); load_mod_bcast(A1c, "A1c", 2, 1, "c9")
        for Ax, k_ in ((A1, "A1"), (A1c, "A1c")):
            C("vector", "scalar_tensor_tensor", r=[k_, "nm_sq"], w=[k_], out=Ax[:], in0=Ax[:], scalar=1.0, in1=g1b[:], op0=ALU.add, op1=ALU.mult)
        P.dma("sync", Gq[:].rearrange("p a b -> p (a b)"), qg_d[:], w=["Gq"], semkey="c10")
        P.dma("sync", Gk[:].rearrange("p a b -> p (a b)"), kg_d[:], w=["Gk"], semkey="c11")
        C("vector", "memset", w=["Vaug"], ap=Vaug[:, :, :, 128:130], constant=1.0)

        caf = [catA[:, hh, :].bitcast(F32) for hh in range(3)]
        QT_ = dict(sqt=caf[0][:, 0:512], qn=caf[0][:, 512:1024], qn2=caf[0][:, 1024:1536], rt=[caf[1][:, i * 512:(i + 1) * 512] for i in range(4)],
                   qr=catA[:, 3, 0:512], ssq=ssq2)

        def qk_post(bank, bkey, G, Gkey, rope_lt, dst_is_q, tt):
            if dst_is_q:
                return qk_post_q(bank, bkey, G, Gkey, rope_lt, tt)
            sqt_, sqk_ = sqt, "sqt"
            rc, rs_ = rcs[tt % 2], rss[tt % 2]
            rck, rsk = "rc0", "rs_0"
            C("scalar", "activation", r=[bkey], w=[sqk_], out=sqt_[:], in_=bank[:], func=AF.Square)
            C("vector", "tensor_reduce", r=[sqk_], w=["ssq"], out=ssq[:, 0:8], in_=sqt_[:].rearrange("p (a b) -> p a b", b=64), axis=AX.X, op=ALU.add)
            rsqrt_rows(ssq[:, 0:8], ssq[:, 8:16], 1.0 / 64, 8, ["ssq"], ["ssq"])
            C("vector", "tensor_tensor", r=[bkey, "ssq"], w=["qn"], out=qn[:].rearrange("p (a b) -> p a b", b=64), in0=bank[:].rearrange("p (a b) -> p a b", b=64), in1=ssq[:, 8:16].unsqueeze(2).to_broadcast([128, 8, 64]), op=ALU.mult)
            if rope_lt is None:
                C("gpsimd", "tensor_tensor", r=["qn", Gkey], w=["qr"], out=qr[:], in0=qn[:], in1=G[:].rearrange("p a b -> p (a b)"), op=ALU.mult)
            else:
                C("gpsimd", "tensor_tensor", r=["qn", Gkey], w=["qn2"], out=qn2[:], in0=qn[:], in1=G[:].rearrange("p a b -> p (a b)"), op=ALU.mult)
                v = lambda t, h: t[:].rearrange("p (a h f) -> p a h f", h=2, f=16)[:, :, h, :]
                C("vector", "tensor_tensor", r=["qn2", rck], w=["rt0"], out=v(rt[0], 0), in0=v(qn2, 0), in1=v(rc, 0), op=ALU.mult)
                C("vector", "tensor_tensor", r=["qn2", rsk], w=["rt1"], out=v(rt[1], 0), in0=v(qn2, 1), in1=v(rs_, 0), op=ALU.mult)
                C("vector", "tensor_tensor", r=["rt0", "rt1"], w=["qr"], out=v(qr, 0), in0=v(rt[0], 0), in1=v(rt[1], 0), op=ALU.subtract)
                C("gpsimd", "tensor_tensor", r=["qn2", rck], w=["rt2"], out=v(rt[2], 0), in0=v(qn2, 1), in1=v(rc, 0), op=ALU.mult)
                C("gpsimd", "tensor_tensor", r=["qn2", rsk], w=["rt3"], out=v(rt[3], 0), in0=v(qn2, 0), in1=v(rs_, 0), op=ALU.mult)
                C("gpsimd", "tensor_tensor", r=["rt2", "rt3"], w=["qr"], out=v(qr, 1), in0=v(rt[2], 0), in1=v(rt[3], 0), op=ALU.add)
            for h in range(4):
                C("tensor", "transpose", r=["qr", "ident"], w=["tb1"], out=tbs[1][:, h * 128:(h + 1) * 128], in_=qr[:, h * 128:(h + 1) * 128], identity=ident[:])
            if dst_is_q:
                C("scalar", "copy", r=["tb1"], w=["qTs"], out=qTs[:], in_=tbs[1][:, 0:512].rearrange("p (h t) -> p h t", t=128))
                P.dma("sync", qT_d[b].rearrange("h p n -> p h n")[:, :, rope_lt * 128:(rope_lt + 1) * 128], qTs[:], r=["qTs"], w=["qT_d"], semkey="qTs")
            else:
                C("scalar", "copy", r=["tb1"], w=["KT"], out=KT[:, :, tt * 128:(tt + 1) * 128], in_=tbs[1][:, 0:512].rearrange("p (h t) -> p h t", t=128))

        def qk_post_q(bank, bkey, G, Gkey, lt_, tt):
            q_sq, q_n, q_n2, q_rt, q_r, q_ss = QT_["sqt"], QT_["qn"], QT_["qn2"], QT_["rt"], QT_["qr"], QT_["ssq"]
            rc, rs_ = rcs[0], rss[0]
            g3 = lambda ap: ap.rearrange("p (a b) -> p a b", b=64)
            C("scalar", "activation", r=[bkey], w=["q_sq"], out=q_sq, in_=bank[:], func=AF.Square)
            C("vector", "tensor_reduce", r=["q_sq"], w=["q_ss"], out=q_ss[:, 0:8], in_=g3(q_sq), axis=AX.X, op=ALU.add)
            rsqrt_rows(q_ss[:, 0:8], q_ss[:, 8:16], 1.0 / 64, 8, ["q_ss"], ["q_ss"])
            C("vector", "tensor_tensor", r=[bkey, "q_ss"], w=["q_n"], out=g3(q_n), in0=g3(bank[:]), in1=q_ss[:, 8:16].unsqueeze(2).to_broadcast([128, 8, 64]), op=ALU.mult)
            C("gpsimd", "tensor_tensor", r=["q_n", Gkey], w=["q_n2"], out=q_n2, in0=q_n, in1=G[:].rearrange("p a b -> p (a b)"), op=ALU.mult)
            v = lambda ap, h: ap.rearrange("p (a h f) -> p a h f", h=2, f=16)[:, :, h, :]
            C("vector", "tensor_tensor", r=["q_n2", "rc0"], w=["q_rt0"], out=v(q_rt[0], 0), in0=v(q_n2, 0), in1=v(rc[:], 0), op=ALU.mult)
            C("vector", "tensor_tensor", r=["q_n2", "rs_0"], w=["q_rt1"], out=v(q_rt[1], 0), in0=v(q_n2, 1), in1=v(rs_[:], 0), op=ALU.mult)
            C("vector", "tensor_tensor", r=["q_rt0", "q_rt1"], w=["q_r"], out=v(q_r, 0), in0=v(q_rt[0], 0), in1=v(q_rt[1], 0), op=ALU.subtract)
            C("gpsimd", "tensor_tensor", r=["q_n2", "rc0"], w=["q_rt2"], out=v(q_rt[2], 0), in0=v(q_n2, 1), in1=v(rc[:], 0), op=ALU.mult)
            C("gpsimd", "tensor_tensor", r=["q_n2", "rs_0"], w=["q_rt3"], out=v(q_rt[3], 0), in0=v(q_n2, 0), in1=v(rs_[:], 0), op=ALU.mult)
            C("gpsimd", "tensor_tensor", r=["q_rt2", "q_rt3"], w=["q_r"], out=v(q_r, 1), in0=v(q_rt[2], 0), in1=v(q_rt[3], 0), op=ALU.add)
            for h in range(4):
                C("tensor", "transpose", r=["q_r", "ident"], w=["tb1"], out=tbs[1][:, h * 128:(h + 1) * 128], in_=q_r[:, h * 128:(h + 1) * 128], identity=ident[:])
            C("scalar", "copy", r=["tb1"], w=["qTs"], out=qTs[:], in_=tbs[1][:, 0:512].rearrange("p (h t) -> p h t", t=128))
            P.dma("sync", qT_d[b].rearrange("h p n -> p h n")[:, :, lt_ * 128:(lt_ + 1) * 128], qTs[:], r=["qTs"], w=["qT_d"], semkey="qTs")

        def p1_load_x(tt_):
            s_ = tt_ % 2
            lt_ = tt_ - 2
            src = ctx_d[b * CTX + tt_ * 128: b * CTX + (tt_ + 1) * 128, :] if tt_ < 2 else x_d[b * SEQ + lt_ * 128: b * SEQ + (lt_ + 1) * 128, :]
            P.dma("sync", xts[s_][:], src, w=["xt%d" % s_], semkey="xt%d" % s_)

        def p1_load_rope(tt_):
            lt_ = tt_ - 2
            if lt_ >= 0:
                P.dma("sync", rcs[0][:], ropec_d[lt_ * 128:(lt_ + 1) * 128, :], w=["rc0"], semkey="rc0")
                P.dma("sync", rss[0][:], ropes_d[lt_ * 128:(lt_ + 1) * 128, :], w=["rs_0"], semkey="rs_0")

        p1_load_x(0)
        for tt in range(NT):
            s = tt % 2
            lt = tt - 2
            isctx = tt < 2
            if tt + 1 < NT:
                p1_load_x(tt + 1)
            xmb = xmbs[s]
            if isctx:
                norm_mod(xts[s][:], "xt%d" % s, A1c, "A1c", B1c, "B1c", xmb[:], "xmb%d" % s, NW[s])
            else:
                norm_mod(xts[s][:], "xt%d" % s, A1, "A1", B1, "B1", xmb[:], "xmb%d" % s, NW[s])
            for k in range(8):
                C("tensor", "transpose", r=["xmb%d" % s, "ident"], w=["tb0"], out=tbs[0][:, k * 128:(k + 1) * 128], in_=xmb[:, k * 128:(k + 1) * 128], identity=ident[:])
            C("scalar", "copy", r=["tb0"], w=["xmT%d" % s], out=xmT[s][:].rearrange("p k t -> p (k t)"), in_=tbs[0][:])
            todo = [(512, 1), (1024, 2)] if isctx else [(0, 0), (512, 1), (1024, 2)]
            for col, bi in todo:
                for k in range(8):
                    C("tensor", "matmul", r=["xmT%d" % s, "winb"], w=["bk%d" % bi], out=banks[bi][:], lhsT=xmT[s][:, k, :], rhs=winb[:, k, col:col + 512], start=(k == 0), stop=(k == 7))
            for j in range(4):
                for k in range(8):
                    C("tensor", "matmul", r=["xmT%d" % s, "winb"], w=["bk3"], out=banks[3][:, j * 128:(j + 1) * 128], lhsT=winb[:, k, 1536 + 128 * j:1536 + 128 * (j + 1)], rhs=xmT[s][:, k, :], start=(k == 0), stop=(k == 7))
            C("scalar", "copy", r=["bk2"], w=["Vaug"], out=Vaug[:, tt, :, 0:128], in_=banks[2][:].rearrange("p (h e) -> p h e", e=128))
            C("vector", "tensor_copy", r=["bk3"], w=["usb"], out=usb[:].rearrange("p j t -> p (j t)"), in_=banks[3][:])
            uv = uT_d[b].rearrange("j p n -> p j n")
            if isctx:
                P.dma("sync", uv[:, :, tt * 128:(tt + 1) * 128], usb[:], r=["usb"], w=["uT_d"], semkey="usb")
                P.dma("sync", uv[:, :, 4352 + tt * 128:4352 + (tt + 1) * 128], usb[:], r=["usb"], w=["uT_d"], semkey="usb")
            else:
                P.dma("sync", uv[:, :, tt * 128:(tt + 1) * 128], usb[:], r=["usb"], w=["uT_d"], semkey="usb")
            qk_post(banks[1], "bk1", Gk, "Gk", None if isctx else lt, False, tt)
            if not isctx:
                qk_post(banks[0], "bk0", Gq, "Gq", lt, True, tt)
            if tt + 1 < NT:
                p1_load_rope(tt + 1)
        P.release(mA)

        m2 = P.mark()
        SG4 = P.sb("SG4", [128, 4, 128], F32)
        qblk = [P.sb("qblk%d" % i, [128, 512], BF16) for i in range(2)]
        pts = [P.sb("pt%d" % i, [128, 512], BF16) for i in range(4)]
        stv = [banks[0][:], banks[1][:], banks[2][:], tbs[1][:].bitcast(F32)]
        stk = ["bk0", "bk1", "bk2", "tb1"]
        osb = [P.sb("osb%d" % i, [128, 8, 129], F32) for i in range(2)]
        rr8 = P.sb("rr8", [128, 2, 8], F32)
        ss4 = P.sb("ss4", [128, 2, 4], F32)
        ept0 = P.sb("ept0", [128, 4, 128], F32)
        ept1 = P.sb("ept1", [128, 4, 128], F32)
        epoo = P.sb("epoo", [128, 4, 128], F32)
        ab4 = P.sb("ab4", [128, 4, 128], BF16)
        P.dma("sync", SG4[:].rearrange("p a b -> p (a b)"), sub_d[:], w=["SG4"], semkey="c12")
        C("vector", "tensor_scalar", r=["SG4"], w=["SG4"], out=SG4[:], in0=SG4[:], scalar1=1.0 - LAM_INIT, scalar2=None, op0=ALU.mult)
        obank = [banks[3], banks[4], banks[5]]

        def oreg(c, qt, lo, hi):
            r_ = c * 4 + qt
            return obank[r_ // 3][:, (r_ % 3) * 129 + lo:(r_ % 3) * 129 + hi], "bk%d" % (3 + r_ // 3)

        units = [(h, qb) for h in range(4) for qb in range(8)]
        steps = [(kt, c) for kt in range(NT) for c in range(2)]
        NS = len(steps)

        def load_q(ui):
            h, qb = units[ui]
            s = ui % 2
            P.dma("sync", qblk[s][:], qT_d[b, h, :, qb * 512:(qb + 1) * 512], r=["qT_d"], w=["qblk%d" % s], semkey="qblk%d" % s)

        def ep_stage1(ui):
            s = ui % 2
            for bi, (r0, r1) in enumerate(((0, 3), (3, 6), (6, 8))):
                nr = r1 - r0
                C("vector", "tensor_copy", r=["bk%d" % (3 + bi)], w=["osb%d" % s], out=osb[s][:, r0:r1, :], in_=obank[bi][:, 0:nr * 129].rearrange("p (a b) -> p a b", b=129))
            C("vector", "reciprocal", r=["osb%d" % s], w=["rr8"], out=rr8[:, s, :], in_=osb[s][:, :, 128])
            C("vector", "tensor_scalar", r=["rr8", "neglam"], w=["rr8"], out=rr8[:, s, 4:8], in0=rr8[:, s, 4:8], scalar1=neglam[:, 0:1], scalar2=None, op0=ALU.mult)
            C("vector", "tensor_tensor", r=["osb%d" % s, "rr8"], w=["ept0"], out=ept0[:], in0=osb[s][:, 0:4, 0:128], in1=rr8[:, s, 0:4].unsqueeze(2).to_broadcast([128, 4, 128]), op=ALU.mult)
            C("vector", "tensor_tensor", r=["osb%d" % s, "rr8"], w=["ept1"], out=ept1[:], in0=osb[s][:, 4:8, 0:128], in1=rr8[:, s, 4:8].unsqueeze(2).to_broadcast([128, 4, 128]), op=ALU.mult)
            C("gpsimd", "tensor_tensor", r=["ept0", "ept1"], w=["epoo"], out=epoo[:], in0=ept0[:], in1=ept1[:], op=ALU.add)
            C("gpsimd", "tensor_tensor", r=["epoo"], w=["ept0"], out=ept0[:], in0=epoo[:], in1=epoo[:], op=ALU.mult)
            C("vector", "tensor_reduce", r=["ept0"], w=["ss4"], out=ss4[:, s, :], in_=ept0[:], axis=AX.X, op=ALU.add)

        def ep_stage2(ui):
            s = ui % 2
            C("scalar", "activation", r=["ss4", "epst"], w=["ss4"], out=ss4[:, s, :], in_=ss4[:, s, :], func=AF.Sqrt, bias=epst[:, 0:1], scale=1.0 / 128)

        def ep_stage3(ui):
            s = ui % 2
            C("vector", "reciprocal", r=["ss4"], w=["ss4"], out=ss4[:, s, :], in_=ss4[:, s, :])
            C("vector", "tensor_tensor", r=["epoo", "ss4"], w=["ept1"], out=ept1[:], in0=epoo[:], in1=ss4[:, s, :].unsqueeze(2).to_broadcast([128, 4, 128]), op=ALU.mult)
            C("gpsimd", "tensor_tensor", r=["ept1", "SG4"], w=["ab4"], out=ab4[:], in0=ept1[:], in1=SG4[:], op=ALU.mult)
            for qt in range(4):
                C("tensor", "transpose", r=["ab4", "ident"], w=["tb0"], out=tbs[0][:, qt * 128:(qt + 1) * 128], in_=ab4[:, qt, :], identity=ident[:])

        def ep_stage4(ui):
            h, qb = units[ui]
            C("scalar", "copy", r=["tb0"], w=["catA"], out=catA[:, h, qb * 512:(qb + 1) * 512], in_=tbs[0][:, 0:512])

        gstep = 0
        load_q(0)
        load_q(1)
        for ui, (h, qb) in enumerate(units):
            s = ui % 2
            base = gstep

            def qk(i):
                kt, c = steps[i]
                si = (base + i) % 4
                C("tensor", "matmul", r=["KT", "qblk%d" % s], w=[stk[si]], out=stv[si], lhsT=KT[64 * c:64 * c + 64, h, kt * 128:(kt + 1) * 128], rhs=qblk[s][64 * c:64 * c + 64, :], start=True, stop=True)

            qk(0)
            qk(1)
            for i in range(NS):
                kt, c = steps[i]
                si = (base + i) % 4
                if i % 2 == 0 and i + 2 < NS:
                    qk(i + 2)
                    qk(i + 3)
                C("scalar", "activation", r=[stk[si]], w=["pt%d" % si], out=pts[si][:], in_=stv[si], func=AF.Exp, scale=0.125)
                for qt in range(4):
                    o_ap, o_key = oreg(c, qt, 0, 129)
                    C("tensor", "matmul", r=["pt%d" % si, "Vaug"], w=[o_key], out=o_ap, lhsT=pts[si][:, qt * 128:(qt + 1) * 128], rhs=Vaug[:, kt, h, 0:129], start=(kt == 0), stop=(kt == NT - 1), skip_group_check=True)
                if ui > 0:
                    if i == 12:
                        ep_stage2(ui - 1)
                    elif i == 18:
                        ep_stage3(ui - 1)
                    elif i == 30:
                        ep_stage4(ui - 1)
            gstep += NS
            ep_stage1(ui)
            if ui + 2 < len(units):
                load_q(ui + 2)
        ep_stage2(len(units) - 1)
        ep_stage3(len(units) - 1)
        ep_stage4(len(units) - 1)
        P.release(m1)

        catS = P.sb("catS", [128, 4, SEQ], BF16)
        m3 = P.mark()
        ssm_phase(P, C, T, b, banks, catS, epst, locals())
        P.release(m3)

        m4 = P.mark()
        woutb = P.sb("woutb", [128, 8, D], BF16)
        G1 = P.sb("G1", [128, D], F32); A2 = P.sb("A2", [128, D], F32); B2 = P.sb("B2", [128, D], F32)
        g2b = nm_sq
        xts = [P.sb("xq%d" % i, [128, D], F32) for i in range(2)]
        x1 = [P.sb("x1_%d" % i, [128, D], F32) for i in range(2)]
        nsq4 = P.sb("nsq4", [128, D], F32); nt4 = P.sb("nt4", [128, D], F32)
        NW4 = [(nm_sq, "nm_sq", nm_ss, "nm_ss", nm_t, "nm_t"), (nsq4, "nsq4", nm_ss2, "nm_ss2", nt4, "nt4")]
        gt4 = [P.sb("gt4_%d" % i, [128, D], F32) for i in range(2)]
        xm2s = [P.sb("xm2_%d" % i, [128, D], F32) for i in range(2)]
        x2Tfs = [P.sb("x2Tf%d" % i, [128, 8, 128], F32) for i in range(2)]
        x2Tbs = [P.sb("x2Tb%d" % i, [128, 8, 128], BF16) for i in range(2)]
        wrf = P.sb("wrf", [128, 8, 36], F32)
        brb = P.sb("brb", [128, 36], F32)
        lgs = [P.sb("lg%d" % i, [128, 36], F32) for i in range(2)]
        gws = [P.sb("gw%d" % i, [128, 16], F32) for i in range(2)]
        ohgs = [P.sb("ohg%d" % i, [128, 4], F32) for i in range(2)]
        msks = [P.sb("msk%d" % i, [128, 32], F32) for i in range(2)]
        msk2s = [P.sb("msk2%d" % i, [128, 32], F32) for i in range(2)]
        oh1s = [P.sb("oh1%d" % i, [128, 32], F32) for i in range(2)]
        oh2s = [P.sb("oh2%d" % i, [128, 32], F32) for i in range(2)]
        P.dma("gpsimd", woutb[:], wout_d.rearrange("(k p) n -> p k n", p=128), w=["woutb"], semkey="woutb")
        P.dma("sync", g2b[:], n2g_d[:], w=["nm_sq"], semkey="c13")
        load_mod_bcast(G1, "G1", b, 2, "c14"); load_mod_bcast(B2, "B2", b, 3, "c15"); load_mod_bcast(A2, "A2", b, 4, "c16")
        C("vector", "scalar_tensor_tensor", r=["A2", "nm_sq"], w=["A2"], out=A2[:], in0=A2[:], scalar=1.0, in1=g2b[:], op0=ALU.add, op1=ALU.mult)
        P.dma("sync", wrf[:], wr_d.rearrange("(k p) n -> p k n", p=128), w=["wrf"], semkey="c17")
        P.dma("sync", brb[:], br_d[:], w=["brb"], semkey="c18")
        for lt in range(32):
            s = lt % 2
            sx = "_%d" % s
            ob = (0, 1) if s == 0 else (4, 5)
            xm2, x2Tf, x2Tb = xm2s[s], x2Tfs[s], x2Tbs[s]
            lg, gw, ohg, msk, msk2, oh1, oh2 = lgs[s], gws[s], ohgs[s], msks[s], msk2s[s], oh1s[s], oh2s[s]
            K = lambda nme: nme + sx
            row0 = b * SEQ + lt * 128
            if lt == 0:
                P.dma("sync", xts[0][:], x_d[row0:row0 + 128, :], w=["xq0"], semkey="xq0")
            if lt + 1 < 32:
                P.dma("sync", xts[1 - s][:], x_d[row0 + 128:row0 + 256, :], w=["xq%d" % (1 - s)], semkey="xq%d" % (1 - s))
            for half in range(2):
                for k in range(8):
                    C("tensor", "matmul", r=["catA", "catS", "woutb"], w=["bk%d" % ob[half]], out=banks[ob[half]][:], lhsT=(catA if k < 4 else catS)[:, k % 4, lt * 128:(lt + 1) * 128], rhs=woutb[:, k, half * 512:(half + 1) * 512], start=(k == 0), stop=(k == 7))
            for half in range(2):
                sl = slice(half * 512, (half + 1) * 512)
                C("vector", "tensor_tensor", r=["bk%d" % ob[half], "G1"], w=[K("gt4")], out=gt4[s][:, sl], in0=banks[ob[half]][:], in1=G1[:, sl], op=ALU.mult)
            C("gpsimd", "tensor_tensor", r=[K("gt4"), "xq%d" % s], w=["x1_%d" % s], out=x1[s][:], in0=gt4[s][:], in1=xts[s][:], op=ALU.add)
            P.dma("sync", out_d[row0:row0 + 128, :], x1[s][:], r=["x1_%d" % s], w=["out_d%d" % (row0 // 128)], semkey="x1_%d" % s)
            norm_mod(x1[s][:], "x1_%d" % s, A2, "A2", B2, "B2", xm2[:], K("xm2"), NW4[s])
            for k in range(8):
                bi = 2 + k // 4
                C("tensor", "transpose", r=[K("xm2"), "identf"], w=["bk%d" % bi], out=banks[bi][:, (k % 4) * 128:(k % 4 + 1) * 128], in_=xm2[:, k * 128:(k + 1) * 128], identity=identf[:])
            for hh in range(2):
                C("scalar", "copy", r=["bk%d" % (2 + hh)], w=[K("x2Tf")], out=x2Tf[:, hh * 4:(hh + 1) * 4, :].rearrange("p k t -> p (k t)"), in_=banks[2 + hh][:])
            C("vector", "tensor_copy", r=[K("x2Tf")], w=[K("x2Tb")], out=x2Tb[:], in_=x2Tf[:])
            P.dma("sync", x2T_d[row0 // 2048, :, :, row0 % 2048:row0 % 2048 + 128], x2Tb[:], r=[K("x2Tb")], w=["x2T_d"], semkey="x2Tb%d" % s)
            rb = banks[ob[0]]
            rbk = "bk%d" % ob[0]
            for k in range(8):
                C("tensor", "matmul", r=[K("x2Tf"), "wrf"], w=[rbk], out=rb[:, 0:36], lhsT=x2Tf[:, k, :], rhs=wrf[:, k, :], start=(k == 0), stop=(k == 7))
            C("vector", "tensor_tensor", r=[rbk, "brb"], w=[K("lg")], out=lg[:], in0=rb[:, 0:36], in1=brb[:], op=ALU.add)
            gidx = b * 32 + lt
            gk, lk, ok_, mk, m2k, o1k, o2k = K("gw"), K("lg"), K("ohg"), K("msk"), K("msk2"), K("oh1"), K("oh2")
            C("vector", "tensor_reduce", r=[lk], w=[gk], out=gw[:, 0:1], in_=lg[:, 0:4], axis=AX.X, op=ALU.max)
            C("vector", "tensor_scalar", r=[lk, gk], w=[ok_], out=ohg[:], in0=lg[:, 0:4], scalar1=gw[:, 0:1], scalar2=None, op0=ALU.is_ge)
            C("vector", "tensor_scalar", r=[gk], w=[gk], out=gw[:, 1:2], in0=gw[:, 0:1], scalar1=-1.0, scalar2=None, op0=ALU.mult)
            C("scalar", "activation", r=[lk, gk], w=[gk], out=gw[:, 4:8], in_=lg[:, 0:4], func=AF.Exp, bias=gw[:, 1:2], scale=1.0)
            C("vector", "tensor_reduce", r=[gk], w=[gk], out=gw[:, 2:3], in_=gw[:, 4:8], axis=AX.X, op=ALU.add)
            C("vector", "reciprocal", r=[gk], w=[gk], out=gw[:, 3:4], in_=gw[:, 2:3])
            C("vector", "tensor_scalar", r=[ok_], w=[ok_], out=ohg[:], in0=ohg[:], scalar1=-1.0, scalar2=1e30, op0=ALU.add, op1=ALU.mult)
            C("vector", "tensor_tensor", r=[lk, ok_], w=[mk], out=msk[:].rearrange("p (g e) -> p g e", e=8), in0=lg[:, 4:36].rearrange("p (g e) -> p g e", e=8), in1=ohg[:].unsqueeze(2).to_broadcast([128, 4, 8]), op=ALU.add)
            C("vector", "tensor_reduce", r=[mk], w=[gk], out=gw[:, 8:9], in_=msk[:], axis=AX.X, op=ALU.max)
            C("vector", "tensor_scalar", r=[mk, gk], w=[o1k], out=oh1[:], in0=msk[:], scalar1=gw[:, 8:9], scalar2=None, op0=ALU.is_ge)
            C("vector", "scalar_tensor_tensor", r=[o1k, mk], w=[m2k], out=msk2[:], in0=oh1[:], scalar=-1e30, in1=msk[:], op0=ALU.mult, op1=ALU.add)
            C("vector", "tensor_reduce", r=[m2k], w=[gk], out=gw[:, 9:10], in_=msk2[:], axis=AX.X, op=ALU.max)
            C("vector", "tensor_scalar", r=[m2k, gk], w=[o2k], out=oh2[:], in0=msk2[:], scalar1=gw[:, 9:10], scalar2=None, op0=ALU.is_ge)
            C("vector", "tensor_tensor", r=[gk], w=[gk], out=gw[:, 10:11], in0=gw[:, 9:10], in1=gw[:, 8:9], op=ALU.subtract)
            C("scalar", "activation", r=[gk], w=[gk], out=gw[:, 11:12], in_=gw[:, 10:11], func=AF.Exp)
            C("vector", "tensor_scalar", r=[gk], w=[gk], out=gw[:, 12:13], in0=gw[:, 11:12], scalar1=1.0, scalar2=None, op0=ALU.add)
            C("vector", "reciprocal", r=[gk], w=[gk], out=gw[:, 12:13], in_=gw[:, 12:13])
            C("vector", "tensor_tensor", r=[gk], w=[gk], out=gw[:, 13:14], in0=gw[:, 12:13], in1=gw[:, 3:4], op=ALU.mult)
            C("vector", "tensor_tensor", r=[gk], w=[gk], out=gw[:, 14:15], in0=gw[:, 13:14], in1=gw[:, 11:12], op=ALU.mult)
            C("vector", "tensor_scalar", r=[o1k, gk], w=[o1k], out=oh1[:], in0=oh1[:], scalar1=gw[:, 13:14], scalar2=None, op0=ALU.mult)
            C("vector", "scalar_tensor_tensor", r=[o2k, gk, o1k], w=["gates"], out=gates[:, gidx, :], in0=oh2[:], scalar=gw[:, 14:15], in1=oh1[:], op0=ALU.mult, op1=ALU.add)
        P.release(mb)

    m5 = P.mark()
    SB = 2048
    acc = P.sb("acc", [128, 16, D], F32)
    x2Ts = [P.sb("x2T%d" % i, [128, 8, SB], BF16) for i in range(2)]
    wg = [P.sb("wg%d" % i, [128, 8, 512], BF16) for i in range(2)]
    wu = [P.sb("wu%d" % i, [128, 8, 512], BF16) for i in range(2)]
    wd = [P.sb("wd%d" % i, [128, 4, D], BF16) for i in range(2)]
    silb = P.sb("silb", [128, 2, 512], F32)
    sil = [silb[:, 0, :], silb[:, 1, :]]
    hid = [P.sb("hid%d" % i, [128, 4, 512], BF16) for i in range(2)]
    G2 = silb[:].rearrange("p a b -> p (a b)")
    xr = [nm_sq, nm_t]
    ecnt = 0
    hcnt = 0
    NSB = NB * SEQ // SB

    def load_x2T(i):
        P.dma("sync", x2Ts[i % 2][:], x2T_d[i], r=["x2T_d"], w=["x2T%d" % (i % 2)], semkey="x2T%d" % (i % 2))

    load_x2T(0)
    for sbk in range(NSB):
        b = sbk // 2
        tok0 = sbk * SB
        x2T = x2Ts[sbk % 2]
        x2k = "x2T%d" % (sbk % 2)
        if sbk + 1 < NSB:
            load_x2T(sbk + 1)
        C("vector", "memset", w=["acc"], ap=acc[:], constant=0.0)
        for e in range(32):
            s = ecnt % 2
            ecnt += 1
            P.dma("gpsimd", wg[s][:], weg_d[e].rearrange("(k p) f -> p k f", p=128), w=["wg%d" % s], semkey="wg%d" % s)
            P.dma("gpsimd", wu[s][:], weu_d[e].rearrange("(k p) f -> p k f", p=128), w=["wu%d" % s], semkey="wu%d" % s)
            P.dma("gpsimd", wd[s][:], wed_d[e].rearrange("(k p) f -> p k f", p=128), w=["wd%d" % s], semkey="wd%d" % s)
            for blk in range(SB // 512):
                hs = hcnt % 2
                hcnt += 1
                for f in range(4):
                    pg = (2 * f) % 4
                    pu = (2 * f + 1) % 4
                    for k in range(8):
                        C("tensor", "matmul", r=[x2k, "wg%d" % s], w=["bk%d" % pg], out=banks[pg][:], lhsT=wg[s][:, k, f * 128:(f + 1) * 128], rhs=x2T[:, k, blk * 512:(blk + 1) * 512], start=(k == 0), stop=(k == 7))
                    for k in range(8):
                        C("tensor", "matmul", r=[x2k, "wu%d" % s], w=["bk%d" % pu], out=banks[pu][:], lhsT=wu[s][:, k, f * 128:(f + 1) * 128], rhs=x2T[:, k, blk * 512:(blk + 1) * 512], start=(k == 0), stop=(k == 7))
                    C("scalar", "activation", r=["bk%d" % pg], w=["sil%d" % (f % 2)], out=sil[f % 2], in_=banks[pg][:], func=AF.Silu)
                    C("vector", "tensor_tensor", r=["bk%d" % pu, "sil%d" % (f % 2)], w=["hid%d" % hs], out=hid[hs][:, f, :], in0=banks[pu][:], in1=sil[f % 2], op=ALU.mult)
                for tl in range(4):
                    ti = blk * 4 + tl
                    for half in range(2):
                        bi = 4 + half
                        for f in range(4):
                            C("tensor", "matmul", r=["hid%d" % hs, "wd%d" % s], w=["bk%d" % bi], out=banks[bi][:], lhsT=hid[hs][:, f, tl * 128:(tl + 1) * 128], rhs=wd[s][:, f, half * 512:(half + 1) * 512], start=(f == 0), stop=(f == 3))
                        C("vector", "scalar_tensor_tensor", r=["bk%d" % bi, "gates", "acc"], w=["acc"], out=acc[:, ti, half * 512:(half + 1) * 512], in0=banks[bi][:], scalar=gates[:, sbk * 16 + ti, e:e + 1], in1=acc[:, ti, half * 512:(half + 1) * 512], op0=ALU.mult, op1=ALU.add)
        P.dma("sync", G2, mod_d[b, :, 5 * D:6 * D], r=["mod_d"], w=["sil0", "sil1"], semkey="c19")
        for ti in range(16):
            s = ti % 2
            row0 = tok0 + ti * 128
            okey = "out_d%d" % (row0 // 128)
            P.dma("sync", xr[s][:], out_d[row0:row0 + 128, :], r=[okey], w=["xr%d" % s], semkey="xr%d" % s)
            C("vector", "tensor_tensor", r=["acc", "sil0", "sil1"], w=["acc"], out=acc[:, ti, :], in0=acc[:, ti, :], in1=G2, op=ALU.mult)
            C("vector", "tensor_tensor", r=["acc", "xr%d" % s], w=["xr%d" % s], out=xr[s][:], in0=acc[:, ti, :], in1=xr[s][:], op=ALU.add)
            fin.append(P.dma("sync", out_d[row0:row0 + 128, :], xr[s][:], r=["xr%d" % s], w=[okey], semkey="xo%d" % s))
    P.emit(final_wait_ops=fin[-2:])
    P.close()
    return nc


def ssm_phase(P, C, T, b, banks, catS, epst, env):
    TWO_PI = 2.0 * PI
    PIB = 3.141592
    ygT = P.sb("ygT", [128, 4, SEQ], BF16)
    hF = P.sb("hF", [128, 2, 2, 2050], BF16)
    WB1 = P.sb("WB1", [128, 8, 2, 2, 64], BF16)
    WCL = P.sb("WCL", [128, 2, 16, 2, 2, 16], BF16)
    WK = P.sb("WK", [128, 2, 4, 32], BF16)
    rho2_s = P.sb("rho2_s", [128, 32], F32)
    th2_s = P.sb("th2_s", [128, 32], F32)
    upg = P.sb("upg", [128, TAU], BF16)
    WB = P.sb("WB", [128, 8, 2, 2, 64], BF16)
    WC = P.sb("WC", [128, 16, 2, 2, 16], BF16)
    WD = P.sb("WD", [128, 4, 32], BF16)
    rho_s = P.sb("rho_s", [128, 32], F32)
    th_s = P.sb("th_s", [128, 32], F32)
    wglu = P.sb("wglu", [128, 4, 512], BF16)
    bglu = P.sb("bglu", [128, 4], F32)
    iot = P.sb("iot", [128, 512], F32)
    nm_sq_, nm_t_ = env["nm_sq"], env["nm_t"]
    tq = [nm_sq_[:, 0:512], nm_sq_[:, 512:1024], nm_t_[:, 0:512], nm_t_[:, 512:1024]]
    ki = P.sb("ki", [128, 512], I32)
    P.dma("gpsimd", wglu[:], T["w_glu"].rearrange("(k p) n -> p k n", p=128), w=["wglu"], semkey="wglu")
    P.dma("sync", bglu[:], T["b_gluT"][:], w=["bglu"], semkey="s0")
    P.dma("sync", iot[:], T["iota1"][:], w=["iot"], semkey="s1")

    def trig(src, sk, n, sin_out, sok, cos_out, cok, w0, w1):
        a, bq = tq[w0], tq[w1]
        C("vector", "tensor_scalar", r=[sk], w=["tq%d" % w0], out=a[:, 0:n], in0=src, scalar1=1.0 / TWO_PI, scalar2=None, op0=ALU.mult)
        C("vector", "tensor_copy", r=["tq%d" % w0], w=["ki"], out=ki[:, 0:n], in_=a[:, 0:n])
        C("vector", "tensor_copy", r=["ki"], w=["tq%d" % w0], out=a[:, 0:n], in_=ki[:, 0:n])
        C("vector", "scalar_tensor_tensor", r=["tq%d" % w0, sk], w=["tq%d" % w0], out=a[:, 0:n], in0=a[:, 0:n], scalar=-TWO_PI, in1=src, op0=ALU.mult, op1=ALU.add)
        C("vector", "tensor_scalar", r=["tq%d" % w0], w=["tq%d" % w1], out=bq[:, 0:n], in0=a[:, 0:n], scalar1=PI, scalar2=TWO_PI, op0=ALU.is_gt, op1=ALU.mult)
        C("vector", "tensor_tensor", r=["tq%d" % w0, "tq%d" % w1], w=[sok], out=sin_out, in0=a[:, 0:n], in1=bq[:, 0:n], op=ALU.subtract)
        C("vector", "tensor_scalar", r=[sok], w=["tq%d" % w1], out=bq[:, 0:n], in0=sin_out, scalar1=-PI, scalar2=TWO_PI, op0=ALU.is_lt, op1=ALU.mult)
        C("vector", "tensor_tensor", r=[sok, "tq%d" % w1], w=[sok], out=sin_out, in0=sin_out, in1=bq[:, 0:n], op=ALU.add)
        C("vector", "tensor_scalar", r=[sok], w=[sok], out=sin_out, in0=sin_out, scalar1=-PIB, scalar2=PIB, op0=ALU.max, op1=ALU.min)
        C("scalar", "activation", r=[sok], w=[sok], out=sin_out, in_=sin_out, func=AF.Sin)
        C("vector", "tensor_scalar", r=["tq%d" % w0], w=["tq%d" % w1], out=bq[:, 0:n], in0=a[:, 0:n], scalar1=PI / 2, scalar2=TWO_PI, op0=ALU.is_gt, op1=ALU.mult)
        C("vector", "scalar_tensor_tensor", r=["tq%d" % w0, "tq%d" % w1], w=[cok], out=cos_out, in0=a[:, 0:n], scalar=PI / 2, in1=bq[:, 0:n], op0=ALU.add, op1=ALU.subtract)
        C("vector", "tensor_scalar", r=[cok], w=["tq%d" % w1], out=bq[:, 0:n], in0=cos_out, scalar1=-PI, scalar2=TWO_PI, op0=ALU.is_lt, op1=ALU.mult)
        C("vector", "tensor_tensor", r=[cok, "tq%d" % w1], w=[cok], out=cos_out, in0=cos_out, in1=bq[:, 0:n], op=ALU.add)
        C("vector", "tensor_scalar", r=[cok], w=[cok], out=cos_out, in0=cos_out, scalar1=-PIB, scalar2=PIB, op0=ALU.max, op1=ALU.min)
        C("scalar", "activation", r=[cok], w=[cok], out=cos_out, in_=cos_out, func=AF.Sin)

    md = P.mark()
    L_ = {}
    for nm in ("sA_re", "sA_im", "sA_dt", "sB_re", "sB_im"):
        L_[nm] = P.sb("l_" + nm, [128, 512], F32)
        P.dma("sync", L_[nm][:], T[nm][:], w=[nm], semkey="s_" + nm)
    smask = P.sb("smask", [128, 2], F32); cmask = P.sb("cmask", [128, 2], F32)
    P.dma("sync", smask[:], T["sMask"][:], w=["smask"], semkey="s2")
    P.dma("sync", cmask[:], T["cMask"][:], w=["cmask"], semkey="s3")
    W = [P.sb("dw%d" % i, [128, 512], F32) for i in range(8)]
    wk = ["dw%d" % i for i in range(8)]
    VT = lambda *a, **k: C("vector", "tensor_tensor", *a, **k)
    dtv, mag, ang, sn, cs, lr, li, den = W
    C("scalar", "activation", r=["sA_dt"], w=[wk[0]], out=dtv[:], in_=L_["sA_dt"][:], func=AF.Exp)
    VT(r=["sA_re", wk[0]], w=[wk[1]], out=mag[:], in0=L_["sA_re"][:], in1=dtv[:], op=ALU.mult)
    C("scalar", "activation", r=[wk[1]], w=[wk[1]], out=mag[:], in_=mag[:], func=AF.Exp)
    VT(r=["sA_im", wk[0]], w=[wk[2]], out=ang[:], in0=L_["sA_im"][:], in1=dtv[:], op=ALU.mult)
    trig(ang[:], wk[2], 512, sn[:], wk[3], cs[:], wk[4], 0, 1)
    VT(r=[wk[1], wk[4]], w=[wk[5]], out=lr[:], in0=mag[:], in1=cs[:], op=ALU.mult)
    VT(r=[wk[1], wk[3]], w=[wk[6]], out=li[:], in0=mag[:], in1=sn[:], op=ALU.mult)
    C("vector", "tensor_scalar", r=[wk[5]], w=[wk[5]], out=lr[:], in0=lr[:], scalar1=-1.0, scalar2=None, op0=ALU.add)
    are, aim = L_["sA_re"], L_["sA_im"]
    VT(r=["sA_re"], w=[wk[7]], out=den[:], in0=are[:], in1=are[:], op=ALU.mult)
    VT(r=["sA_im"], w=[wk[0]], out=dtv[:], in0=aim[:], in1=aim[:], op=ALU.mult)
    VT(r=[wk[7], wk[0]], w=[wk[7]], out=den[:], in0=den[:], in1=dtv[:], op=ALU.add)
    C("vector", "reciprocal", r=[wk[7]], w=[wk[7]], out=den[:], in_=den[:])
    VT(r=[wk[5], "sA_re"], w=[wk[1]], out=mag[:], in0=lr[:], in1=are[:], op=ALU.mult)
    VT(r=[wk[6], "sA_im"], w=[wk[0]], out=dtv[:], in0=li[:], in1=aim[:], op=ALU.mult)
    VT(r=[wk[1], wk[0]], w=[wk[1]], out=mag[:], in0=mag[:], in1=dtv[:], op=ALU.add)
    VT(r=[wk[1], wk[7]], w=[wk[1]], out=mag[:], in0=mag[:], in1=den[:], op=ALU.mult)
    VT(r=[wk[6], "sA_re"], w=[wk[2]], out=ang[:], in0=li[:], in1=are[:], op=ALU.mult)
    VT(r=[wk[5], "sA_im"], w=[wk[0]], out=dtv[:], in0=lr[:], in1=aim[:], op=ALU.mult)
    VT(r=[wk[2], wk[0]], w=[wk[2]], out=ang[:], in0=ang[:], in1=dtv[:], op=ALU.subtract)
    VT(r=[wk[2], wk[7]], w=[wk[2]], out=ang[:], in0=ang[:], in1=den[:], op=ALU.mult)
    bre, bim = L_["sB_re"], L_["sB_im"]
    VT(r=[wk[1], "sB_re"], w=[wk[3]], out=sn[:], in0=mag[:], in1=bre[:], op=ALU.mult)
    VT(r=[wk[2], "sB_im"], w=[wk[0]], out=dtv[:], in0=ang[:], in1=bim[:], op=ALU.mult)
    VT(r=[wk[3], wk[0]], w=[wk[3]], out=sn[:], in0=sn[:], in1=dtv[:], op=ALU.subtract)
    VT(r=[wk[1], "sB_im"], w=[wk[4]], out=cs[:], in0=mag[:], in1=bim[:], op=ALU.mult)
    VT(r=[wk[2], "sB_re"], w=[wk[0]], out=dtv[:], in0=ang[:], in1=bre[:], op=ALU.mult)
    VT(r=[wk[4], wk[0]], w=[wk[4]], out=cs[:], in0=cs[:], in1=dtv[:], op=ALU.add)
    for ri, (src, sk) in enumerate(((sn, wk[3]), (cs, wk[4]))):
        for mp in range(2):
            C("vector", "tensor_scalar", r=[sk, "smask"], w=["WB"], out=WB[:, :, ri, mp, :], in0=src[:].rearrange("p (a c) -> p a c", c=64), scalar1=smask[:, mp:mp + 1], scalar2=None, op0=ALU.mult)
    VT(r=[wk[5], wk[3]], w=[wk[1]], out=mag[:], in0=lr[:], in1=sn[:], op=ALU.mult)
    VT(r=[wk[1], wk[3]], w=[wk[1]], out=mag[:], in0=mag[:], in1=sn[:], op=ALU.add)
    VT(r=[wk[6], wk[4]], w=[wk[0]], out=dtv[:], in0=li[:], in1=cs[:], op=ALU.mult)
    VT(r=[wk[1], wk[0]], w=[wk[1]], out=mag[:], in0=mag[:], in1=dtv[:], op=ALU.subtract)
    VT(r=[wk[5], wk[4]], w=[wk[2]], out=ang[:], in0=lr[:], in1=cs[:], op=ALU.mult)
    VT(r=[wk[2], wk[4]], w=[wk[2]], out=ang[:], in0=ang[:], in1=cs[:], op=ALU.add)
    VT(r=[wk[6], wk[3]], w=[wk[0]], out=dtv[:], in0=li[:], in1=sn[:], op=ALU.mult)
    VT(r=[wk[2], wk[0]], w=[wk[2]], out=ang[:], in0=ang[:], in1=dtv[:], op=ALU.add)
    for ri, (src, sk) in enumerate(((mag, wk[1]), (ang, wk[2]))):
        for mp in range(2):
            C("vector", "tensor_scalar", r=[sk, "smask"], w=["WB1"], out=WB1[:, :, ri, mp, :], in0=src[:].rearrange("p (a c) -> p a c", c=64), scalar1=smask[:, mp:mp + 1], scalar2=None, op0=ALU.mult)
    pre = P.sb("pre", [128, 32], F32); pim = P.sb("pim", [128, 32], F32); pdt = P.sb("pdt", [128, 32], F32)
    P.dma("sync", pre[:], T["pA_re"][:], w=["pre"], semkey="s4")
    P.dma("sync", pim[:], T["pA_im"][:], w=["pim"], semkey="s5")
    P.dma("sync", pdt[:], T["pA_dt"][:], w=["pdt"], semkey="s6")
    C("scalar", "activation", r=["pdt"], w=["pdt"], out=pdt[:], in_=pdt[:], func=AF.Exp)
    VT(r=["pre", "pdt"], w=["rho_s"], out=rho_s[:], in0=pre[:], in1=pdt[:], op=ALU.mult)
    C("scalar", "activation", r=["rho_s"], w=["rho_s"], out=rho_s[:], in_=rho_s[:], func=AF.Exp)
    VT(r=["pim", "pdt"], w=["th_s"], out=th_s[:], in0=pim[:], in1=pdt[:], op=ALU.mult)
    VT(r=["rho_s"], w=["rho2_s"], out=rho2_s[:], in0=rho_s[:], in1=rho_s[:], op=ALU.mult)
    C("vector", "tensor_scalar", r=["th_s"], w=["th2_s"], out=th2_s[:], in0=th_s[:], scalar1=2.0, scalar2=None, op0=ALU.mult)
    lrp = P.sb("lrp", [128, 32], F32); lip = P.sb("lip", [128, 32], F32)
    trig(th_s[:], "th_s", 32, lip[:], "lip", lrp[:], "lrp", 0, 1)
    VT(r=["lrp", "rho_s"], w=["lrp"], out=lrp[:], in0=lrp[:], in1=rho_s[:], op=ALU.mult)
    VT(r=["lip", "rho_s"], w=["lip"], out=lip[:], in0=lip[:], in1=rho_s[:], op=ALU.mult)
    ccr = P.sb("ccr", [128, 256], F32); cci = P.sb("cci", [128, 256], F32)
    P.dma("sync", ccr[:], T["cC_re"][:], w=["ccr"], semkey="s7")
    P.dma("sync", cci[:], T["cC_im"][:], w=["cci"], semkey="s8")
    for mp in range(2):
        C("vector", "tensor_scalar", r=["ccr", "cmask"], w=["WC"], out=WC[:, :, 0, mp, :], in0=ccr[:].rearrange("p (a c) -> p a c", c=16), scalar1=cmask[:, mp:mp + 1], scalar2=None, op0=ALU.mult)
        C("vector", "tensor_scalar", r=["cci", "cmask"], w=["WC"], out=WC[:, :, 1, mp, :], in0=cci[:].rearrange("p (a c) -> p a c", c=16), scalar1=cmask[:, mp:mp + 1], scalar2=-1.0, op0=ALU.mult, op1=ALU.mult)
    cta = P.sb("cta", [128, 16, 16], F32); ctb = P.sb("ctb", [128, 16, 16], F32)
    ccr3 = ccr[:].rearrange("p (a c) -> p a c", c=16); cci3 = cci[:].rearrange("p (a c) -> p a c", c=16)
    for d in range(2):
        lrb = lrp[:, d * 16:(d + 1) * 16].unsqueeze(2).to_broadcast([128, 16, 16])
        lib = lip[:, d * 16:(d + 1) * 16].unsqueeze(2).to_broadcast([128, 16, 16])
        VT(r=["ccr", "lrp"], w=["cta"], out=cta[:], in0=ccr3, in1=lrb, op=ALU.mult)
        VT(r=["cci", "lip"], w=["ctb"], out=ctb[:], in0=cci3, in1=lib, op=ALU.mult)
        VT(r=["cta", "ctb"], w=["cta"], out=cta[:], in0=cta[:], in1=ctb[:], op=ALU.subtract)
        for mp in range(2):
            C("vector", "tensor_scalar", r=["cta", "cmask"], w=["WCL"], out=WCL[:, d, :, 0, mp, :], in0=cta[:], scalar1=cmask[:, mp:mp + 1], scalar2=None, op0=ALU.mult)
        VT(r=["ccr", "lip"], w=["cta"], out=cta[:], in0=ccr3, in1=lib, op=ALU.mult)
        VT(r=["cci", "lrp"], w=["ctb"], out=ctb[:], in0=cci3, in1=lrb, op=ALU.mult)
        VT(r=["cta", "ctb"], w=["cta"], out=cta[:], in0=cta[:], in1=ctb[:], op=ALU.add)
        for mp in range(2):
            C("vector", "tensor_scalar", r=["cta", "cmask"], w=["WCL"], out=WCL[:, d, :, 1, mp, :], in0=cta[:], scalar1=cmask[:, mp:mp + 1], scalar2=-1.0, op0=ALU.mult, op1=ALU.mult)
    e32 = P.sb("e32", [128, 32], F32); dsk = P.sb("dsk", [128, 4], F32)
    P.dma("sync", e32[:], T["eye32"][:], w=["e32"], semkey="s9")
    P.dma("sync", dsk[:], T["dskip"][:], w=["dsk"], semkey="s10")
    for Tt in range(4):
        C("vector", "tensor_scalar", r=["e32", "dsk"], w=["WD"], out=WD[:, Tt, :], in0=e32[:], scalar1=dsk[:, Tt:Tt + 1], scalar2=None, op0=ALU.mult)
    WDf = P.sb("WDf", [128, 4, 32], F32)
    BTs = P.sb("BTs", [128, 2, 32], BF16)
    tbk = env["tbs"][0]
    ident_ = env["ident"]
    for Tt in range(4):
        C("vector", "tensor_scalar", r=["e32", "dsk"], w=["WDf"], out=WDf[:, Tt, :], in0=e32[:], scalar1=dsk[:, Tt:Tt + 1], scalar2=None, op0=ALU.mult)
    for d in range(2):
        for Tt in range(4):
            kb_ = banks[4 + (d * 4 + Tt) % 2]
            kbk = "bk%d" % (4 + (d * 4 + Tt) % 2)
            for j in range(4):
                Pp = 4 * Tt + j
                for ri in range(2):
                    C("tensor", "transpose", r=["WB", "ident"], w=["tb0"], out=tbk[:, ri * 32:(ri + 1) * 32], in_=WB[32 * j:32 * j + 32, d * 4 + Tt, ri, :, :].rearrange("p a c -> p (a c)"), identity=ident_[32 * j:32 * j + 32, 32 * j:32 * j + 32], tile_position=(32 * j, 0))
                C("vector", "tensor_copy", r=["tb0"], w=["BTs"], out=BTs[:].rearrange("p a c -> p (a c)"), in_=tbk[:, 0:64])
                for ri in range(2):
                    C("tensor", "matmul", r=["BTs", "WC"], w=[kbk], out=kb_[32 * j:32 * j + 32, 0:32], lhsT=BTs[:, ri, :], rhs=WC[:, Pp, ri, :, :].rearrange("p a c -> p (a c)"), start=(ri == 0), stop=(ri == 1), tile_position=(0, 32 * j), skip_group_check=True)
            C("vector", "tensor_tensor", r=[kbk, "WDf"], w=["WK"], out=WK[:, d, Tt, :], in0=kb_[:, 0:32], in1=WDf[:, Tt, :], op=ALU.add)
    P.release(md)
    tabc = [P.sb("tabc%d" % i, [128, 512], F32) for i in range(2)]
    tabs = [P.sb("tabs%d" % i, [128, 512], F32) for i in range(2)]
    tabr = [P.sb("tabr%d" % i, [128, 512], F32) for i in range(2)]
    gg = [[P.sb("gg%d%d" % (i, r), [128, 512], F32) for r in range(2)] for i in range(2)]
    hB = [[P.sb("hB%d%d" % (i, r), [128, 514], BF16) for r in range(2)] for i in range(2)]
    car = P.sb("car", [128, 2, 8], F32)
    tneg = [P.sb("tneg%d" % i, [128, 2], F32) for i in range(2)]
    tq2 = [[P.sb("tqw%d%d" % (i, r), [128, 512], F32) for r in range(4 - i)] for i in range(2)]
    tq2[1].append(tq[3])

    def make_tables(d, Pp, pi):
        col = d * 16 + Pp
        if b > 0:
            P.dma("sync", tabs[pi][:], T["tab_s"][col, 0], r=["tab_d%d" % col], w=["tabs%d" % pi], semkey="tls%d" % pi)
            P.dma("sync", tabc[pi][:], T["tab_s"][col, 1], r=["tab_d%d" % col], w=["tabc%d" % pi], semkey="tlc%d" % pi)
            C("vector", "tensor_scalar", r=["iot", "rho2_s"], w=["tabr%d" % pi], out=tabr[pi][:], in0=iot[:], scalar1=0.0, scalar2=rho2_s[:, col:col + 1], op0=ALU.mult, op1=ALU.add)
            return
        C("vector", "tensor_scalar", r=["iot", "th2_s"], w=["tq2"], out=tq[2][:], in0=iot[:], scalar1=th2_s[:, col:col + 1], scalar2=None, op0=ALU.mult)
        trig(tq[2][:], "tq2", 512, tabs[pi][:], "tabs%d" % pi, tabc[pi][:], "tabc%d" % pi, 0, 1)
        C("vector", "tensor_scalar", r=["iot", "rho2_s"], w=["tabr%d" % pi], out=tabr[pi][:], in0=iot[:], scalar1=0.0, scalar2=rho2_s[:, col:col + 1], op0=ALU.mult, op1=ALU.add)
        P.dma("sync", T["tab_s"][col, 0], tabs[pi][:], r=["tabs%d" % pi], w=["tab_d%d" % col], semkey="tss%d" % pi)
        P.dma("sync", T["tab_s"][col, 1], tabc[pi][:], r=["tabc%d" % pi], w=["tab_d%d" % col], semkey="tsc%d" % pi)

    FWD_BLOCKS = [(0, 512), (512, 512), (1024, 512), (1536, 512), (2048, 128)]
    BWD_BLOCKS = [(1792, 512), (1280, 512), (768, 512), (256, 512), (128, 128)]
    for i_ in range(2):
        for r_ in range(2):
            C("gpsimd", "memset", w=["hB%d" % i_], ap=hB[i_][r_][:], constant=0.0)
    for Tt in range(4):
        P.dma("sync", upg[:], T["uT_s"][b, Tt], r=["uT_d"], w=["upg"], semkey="upg")
        for pg in range(2):
            for d in range(2):
                for pi in range(2):
                    make_tables(d, 4 * Tt + 2 * pg + pi, pi)
                    C("vector", "tensor_scalar", r=["tabs%d" % pi], w=["tneg%d" % pi], out=tneg[pi][:, 0:1], in0=tabs[pi][:, 511:512], scalar1=-1.0, scalar2=None, op0=ALU.mult)
                    C("vector", "tensor_scalar", r=["tabs%d" % pi], w=["tneg%d" % pi], out=tneg[pi][:, 1:2], in0=tabs[pi][:, 127:128], scalar1=-1.0, scalar2=None, op0=ALU.mult)
                C("vector", "memset", w=["car0", "car1", "cw0", "cx0", "cy0", "cz0", "cw1", "cx1", "cy1", "cz1"], ap=car[:], constant=0.0)
                for kb, (c0, n) in enumerate(FWD_BLOCKS if d == 0 else BWD_BLOCKS):
                    rv = (lambda ap: ap) if d == 0 else (lambda ap: ap[:, ::-1])

                    def pair_ops(pi):
                        j = 2 * pg + pi
                        bre_k, bim_k = "bk%d" % (2 * pi), "bk%d" % (2 * pi + 1)
                        pre_, pim_ = banks[2 * pi], banks[2 * pi + 1]
                        cT, sT, rT = rv(tabc[pi][:, 0:n]), rv(tabs[pi][:, 0:n]), tabr[pi][:, 0:n]
                        ck, sk_, rk = "tabc%d" % pi, "tabs%d" % pi, "tabr%d" % pi
                        wa_, wb_ = tq2[pi][0], tq2[pi][1]
                        wak, wbk = "tqa%d" % pi, "tqb%d" % pi
                        wc_, wd_ = tq2[pi][2], tq2[pi][3]
                        wck, wdk = "tqc%d" % pi, "tqd%d" % pi
                        gr, gi = wa_, wc_
                        grk, gik = wak, wck
                        g0, g1 = gg[pi][0], gg[pi][1]
                        g0k, g1k = "gg%d0" % pi, "gg%d1" % pi
                        lo_ = 127 if (d == 0 and kb == 0) else 0
                        if d == 0:
                            hre, him = hF[:, pi, 0, c0 + lo_ - 127:c0 + n - 127], hF[:, pi, 1, c0 + lo_ - 127:c0 + n - 127]
                            hk = "hF%d" % pi
                        else:
                            hre, him = hB[pi][0][:, 0:n], hB[pi][1][:, 0:n]
                            hk = "hB%d" % pi
                        cTd, sTd = rv(tabc[pi][:, 0:n])[:, lo_:n], rv(tabs[pi][:, 0:n])[:, lo_:n]
                        lc = n - 1 if d == 0 else 0
                        tcn = n - 1
                        cl, sl_ = tabc[pi][:, tcn:tcn + 1], tabs[pi][:, tcn:tcn + 1]
                        nsl = tneg[pi][:, (0 if n == 512 else 1):(1 if n == 512 else 2)]
                        a0, a1 = g0[:, lc:lc + 1], g1[:, lc:lc + 1]
                        GT = lambda *a, **k: C("gpsimd", "tensor_tensor", *a, **k)
                        ev = upg[32 * j:32 * j + 32, 2 * c0:2 * c0 + 2 * n:2]
                        od = upg[32 * j:32 * j + 32, 2 * c0 + 1:2 * c0 + 2 * n:2]
                        first, second = (ev, od) if d == 0 else (od, ev)
                        ops = []
                        for ri, bk_, bkk in ((0, pre_, bre_k), (1, pim_, bim_k)):
                            ops.append(lambda ri=ri, bk_=bk_, bkk=bkk: C("tensor", "matmul", r=["WB1", "upg"], w=[bkk], out=bk_[:, 0:n], lhsT=WB1[32 * j:32 * j + 32, d * 4 + Tt, ri, :, :].rearrange("p a c -> p (a c)"), rhs=first, start=True, stop=False, tile_position=(32 * j, 0)))
                            ops.append(lambda ri=ri, bk_=bk_, bkk=bkk: C("tensor", "matmul", r=["WB", "upg"], w=[bkk], out=bk_[:, 0:n], lhsT=WB[32 * j:32 * j + 32, d * 4 + Tt, ri, :, :].rearrange("p a c -> p (a c)"), rhs=second, start=False, stop=True, tile_position=(32 * j, 0)))
                        if d == 1:
                            ops.append(lambda: C("gpsimd", "tensor_copy", r=[hk], w=[hk], out=hB[pi][0][:, n:n + 1], in_=hB[pi][0][:, 0:1]))
                            ops.append(lambda: C("gpsimd", "tensor_copy", r=[hk], w=[hk], out=hB[pi][1][:, n:n + 1], in_=hB[pi][1][:, 0:1]))
                        ops += [
                            lambda: VT(r=[bre_k, ck], w=[wak], out=wa_[:, 0:n], in0=pre_[:, 0:n], in1=cT, op=ALU.mult),
                            lambda: VT(r=[bim_k, sk_], w=[wbk], out=wb_[:, 0:n], in0=pim_[:, 0:n], in1=sT, op=ALU.mult),
                            lambda: VT(r=[bim_k, ck], w=[wck], out=wc_[:, 0:n], in0=pim_[:, 0:n], in1=cT, op=ALU.mult),
                            lambda: VT(r=[bre_k, sk_], w=[wdk], out=wd_[:, 0:n], in0=pre_[:, 0:n], in1=sT, op=ALU.mult),
                            lambda: VT(r=[wak, wbk], w=[grk], out=gr[:, 0:n], in0=wa_[:, 0:n], in1=wb_[:, 0:n], op=ALU.add),
                            lambda: VT(r=[wck, wdk], w=[gik], out=gi[:, 0:n], in0=wc_[:, 0:n], in1=wd_[:, 0:n], op=ALU.subtract),
                            lambda: C("vector", "tensor_tensor_scan", r=[rk, grk, "car%d" % pi], w=[g0k], out=rv(g0[:, 0:n]), data0=rT, data1=rv(gr[:, 0:n]), initial=car[:, pi, 0:1], op0=ALU.mult, op1=ALU.add),
                            lambda: C("vector", "tensor_tensor_scan", r=[rk, gik, "car%d" % pi], w=[g1k], out=rv(g1[:, 0:n]), data0=rT, data1=rv(gi[:, 0:n]), initial=car[:, pi, 1:2], op0=ALU.mult, op1=ALU.add),
                            lambda: C("scalar", "activation", r=[g1k, "tneg%d" % pi], w=["cx%d" % pi], out=car[:, pi, 3:4], in_=a1, func=AF.Identity, scale=nsl),
                            lambda: C("scalar", "activation", r=[g0k, sk_], w=["cz%d" % pi], out=car[:, pi, 5:6], in_=a0, func=AF.Identity, scale=sl_),
                            lambda: C("scalar", "activation", r=[g0k, ck, "cx%d" % pi, "car%d" % pi], w=["car%d" % pi], out=car[:, pi, 0:1], in_=a0, func=AF.Identity, scale=cl, bias=car[:, pi, 3:4]),
                            lambda: C("scalar", "activation", r=[g1k, ck, "cz%d" % pi, "car%d" % pi], w=["car%d" % pi], out=car[:, pi, 1:2], in_=a1, func=AF.Identity, scale=cl, bias=car[:, pi, 5:6]),
                            lambda: GT(r=[g0k, ck], w=[wbk], out=wb_[:, lo_:n], in0=g0[:, lo_:n], in1=cTd, op=ALU.mult),
                            lambda: GT(r=[g1k, sk_], w=[wdk], out=wd_[:, lo_:n], in0=g1[:, lo_:n], in1=sTd, op=ALU.mult),
                            lambda: GT(r=[wbk, wdk], w=[hk], out=hre, in0=wb_[:, lo_:n], in1=wd_[:, lo_:n], op=ALU.subtract),
                            lambda: GT(r=[g1k, ck], w=[wbk], out=wb_[:, lo_:n], in0=g1[:, lo_:n], in1=cTd, op=ALU.mult),
                            lambda: GT(r=[g0k, sk_], w=[wdk], out=wd_[:, lo_:n], in0=g0[:, lo_:n], in1=sTd, op=ALU.mult),
                            lambda: GT(r=[wbk, wdk], w=[hk], out=him, in0=wb_[:, lo_:n], in1=wd_[:, lo_:n], op=ALU.add),
                        ]
                        return ops

                    opl = [pair_ops(0), pair_ops(1)]
                    for k_ in range(len(opl[0])):
                        for pi in range(2):
                            opl[pi][k_]()
                    if d == 1:
                        cl0, cl1 = max(c0, 128), min(c0 + n, 2176)
                        nl = cl1 - cl0
                        o = cl0 - c0
                        for eo in range(2):
                            yb = banks[4 + eo]
                            ybk = "bk%d" % (4 + eo)
                            for pi in range(2):
                                j = 2 * pg + pi
                                Pp = 4 * Tt + j
                                outp = yb[32 * j:32 * j + 32, 0:nl]
                                w2 = lambda t, ri: t[:, Pp, ri, :, :].rearrange("p a c -> p (a c)")
                                wl = lambda dd, ri: WCL[:, dd, Pp, ri, :, :].rearrange("p a c -> p (a c)")
                                if eo == 0:
                                    terms = [(wl(0, 0), hF[:, pi, 0, cl0 - 128:cl1 - 128], "hF%d" % pi), (wl(0, 1), hF[:, pi, 1, cl0 - 128:cl1 - 128], "hF%d" % pi),
                                             (w2(WC, 0), hB[pi][0][:, o:o + nl], "hB%d" % pi), (w2(WC, 1), hB[pi][1][:, o:o + nl], "hB%d" % pi)]
                                    urhs = upg[32 * j:32 * j + 32, 2 * cl0:2 * cl1:2]
                                else:
                                    terms = [(w2(WC, 0), hF[:, pi, 0, cl0 - 127:cl1 - 127], "hF%d" % pi), (w2(WC, 1), hF[:, pi, 1, cl0 - 127:cl1 - 127], "hF%d" % pi),
                                             (wl(1, 0), hB[pi][0][:, o + 1:o + 1 + nl], "hB%d" % pi), (wl(1, 1), hB[pi][1][:, o + 1:o + 1 + nl], "hB%d" % pi)]
                                    urhs = upg[32 * j:32 * j + 32, 2 * cl0 + 1:2 * cl1:2]
                                for ti, (lw, rh, rkey) in enumerate(terms):
                                    C("tensor", "matmul", r=["WC", "WCL", rkey], w=[ybk], out=outp, lhsT=lw, rhs=rh, start=(ti == 0), stop=False, tile_position=(0, 32 * j), skip_group_check=True)
                                C("tensor", "matmul", r=["WK", "upg"], w=[ybk], out=outp, lhsT=WK[32 * j:32 * j + 32, eo, Tt, :], rhs=urhs, start=False, stop=True, tile_position=(32 * j, 32 * j), skip_group_check=True)
                            rows = slice(64 * pg, 64 * pg + 64)
                            yv = yb[rows, 0:nl]
                            tw = tq[eo]
                            twk = "tq%d" % eo
                            C("scalar", "activation", r=[ybk], w=[twk], out=tw[rows, 0:nl], in_=yv, func=AF.Square)
                            C("vector", "tensor_scalar", r=[twk], w=[twk], out=tw[rows, 0:nl], in0=tw[rows, 0:nl], scalar1=0.044715, scalar2=1.0, op0=ALU.mult, op1=ALU.add)
                            VT(r=[twk, ybk], w=[twk], out=tw[rows, 0:nl], in0=tw[rows, 0:nl], in1=yv, op=ALU.mult)
                            C("scalar", "activation", r=[twk], w=[twk], out=tw[rows, 0:nl], in_=tw[rows, 0:nl], func=AF.Sigmoid, scale=2.0 * math.sqrt(2.0 / PI))
                            VT(r=[twk, ybk], w=["ygT"], out=ygT[rows, Tt, 2 * cl0 - 256 + eo:2 * cl1 - 256:2], in0=tw[rows, 0:nl], in1=yv, op=ALU.mult)
    zc = 0
    for n_ in range(4):
        for blk in range(8):
            zb = banks[zc % 4]
            zk = "bk%d" % (zc % 4)
            sgi = zc % 2
            zc += 1
            for k in range(4):
                C("tensor", "matmul", r=["wglu", "ygT"], w=[zk], out=zb[:], lhsT=wglu[:, k, n_ * 128:(n_ + 1) * 128], rhs=ygT[:, k, blk * 512:(blk + 1) * 512], start=(k == 0), stop=(k == 3))
            C("scalar", "activation", r=[zk, "bglu"], w=["tq%d" % sgi], out=tq[sgi][:], in_=zb[:], func=AF.Sigmoid, bias=bglu[:, n_:n_ + 1], scale=1.0)
            C("vector", "tensor_tensor", r=["tq%d" % sgi, "ygT"], w=["catS"], out=catS[:, n_, blk * 512:(blk + 1) * 512], in0=tq[sgi][:], in1=ygT[:, n_, blk * 512:(blk + 1) * 512], op=ALU.mult)


_NC_CACHE = {}


def host_layouts(inp):
    f = np.float32
    L = {}
    L["w_ada"] = np.ascontiguousarray(inp["w_ada"][0]); L["b_ada"] = np.ascontiguousarray(np.broadcast_to(inp["b_ada"], (3, 6 * D)))
    rep = lambda v, n: np.ascontiguousarray(np.broadcast_to(np.tile(np.asarray(v, np.float32).reshape(1, -1), (1, n)), (128, v.size * n)))
    L["norm1_g"] = rep(inp["norm1_g"], 1); L["norm2_g"] = rep(inp["norm2_g"], 1)
    sel = np.zeros((3, 3, 128), np.float32)
    for r_ in range(3):
        sel[r_, r_, :] = 1.0
    L["sel3"] = sel.reshape(3, 384)
    L["w_in"] = np.ascontiguousarray(inp["w_in"][0])
    L["qg"] = rep(inp["q_norm_g"], 8); L["kg"] = rep(inp["k_norm_g"], 8)
    L["lamv"] = rep(np.concatenate([inp["lambda_q1"], inp["lambda_k1"], inp["lambda_q2"], inp["lambda_k2"]], axis=1).astype(f), 1)
    L["subln"] = rep(inp["subln_g"], 4)
    rows = SEQ // 64
    row = np.repeat(np.arange(rows, dtype=f), 64); col = np.tile(np.arange(64, dtype=f), rows)
    inv = (10000.0 ** (-np.arange(0, 32, 2, dtype=f) / 32)).astype(f)
    ang = np.stack([row[:, None] * inv, col[:, None] * inv], axis=1).astype(f)
    cs = np.cos(ang).astype(f); sn = np.sin(ang).astype(f)
    full = lambda t: np.ascontiguousarray(np.broadcast_to(t[:, None, :, None, :], (SEQ, 8, 2, 2, 16)).reshape(SEQ, 512))
    L["rope_cos"] = full(cs); L["rope_sin"] = full(sn)
    L["ident"] = np.eye(128, dtype=f)
    L["w_glu"] = np.ascontiguousarray(inp["w_glu"][0]); L["b_gluT"] = np.ascontiguousarray(inp["b_glu"][0].reshape(4, 128).T)
    L["w_out"] = np.ascontiguousarray(inp["w_out"][0])
    L["w_r"] = np.ascontiguousarray(np.concatenate([inp["w_route_group"][0], inp["w_route_expert"][0]], axis=1))
    L["b_r"] = rep(np.concatenate([inp["b_route_group"], inp["b_route_expert"]], axis=1), 1)
    L["w_eg"] = np.ascontiguousarray(inp["w_exp_gate"][0]); L["w_eu"] = np.ascontiguousarray(inp["w_exp_up"][0]); L["w_ed"] = np.ascontiguousarray(inp["w_exp_down"][0])
    a_re, a_im, ldt = inp["ssm_a_re"][0], inp["ssm_a_im"][0], inp["ssm_log_dt"][0]
    b_re, b_im = inp["ssm_b_re"][0], inp["ssm_b_im"][0]
    c_re, c_im, dsk = inp["ssm_c_re"][0], inp["ssm_c_im"][0], inp["ssm_d"][0]
    sA_re = np.zeros((128, 2, 4, 64), f); sA_im = np.zeros_like(sA_re); sA_dt = np.zeros_like(sA_re)
    sB_re = np.zeros_like(sA_re); sB_im = np.zeros_like(sA_re)
    sMask = np.zeros((128, 2), f); dskl = np.zeros((128, 4), f)
    for j in range(4):
        for m in range(2):
            for h in range(16):
                q = 32 * j + 16 * m + h
                sMask[q, m] = 1.0
                for Tt in range(4):
                    g = 8 * Tt + 2 * j + m
                    dskl[q, Tt] = dsk[g, h]
                    for d in range(2):
                        sA_re[q, d, Tt] = a_re[d, g]; sA_im[q, d, Tt] = a_im[d, g]; sA_dt[q, d, Tt] = ldt[d, g]
                        sB_re[q, d, Tt] = b_re[d, g, :, h]; sB_im[q, d, Tt] = b_im[d, g, :, h]
    L["sA_re"] = sA_re.reshape(128, 512); L["sA_im"] = sA_im.reshape(128, 512); L["sA_dt"] = sA_dt.reshape(128, 512)
    L["sB_re"] = sB_re.reshape(128, 512); L["sB_im"] = sB_im.reshape(128, 512); L["sMask"] = sMask; L["dskip"] = dskl
    pA_re = np.zeros((128, 2, 16), f); pA_im = np.zeros_like(pA_re); pA_dt = np.zeros_like(pA_re)
    cC_re = np.zeros((128, 16, 16), f); cC_im = np.zeros_like(cC_re); cMask = np.zeros((128, 2), f)
    for m in range(2):
        for p in range(64):
            q = 64 * m + p
            cMask[q, m] = 1.0
            for Pp in range(16):
                g = 2 * Pp + m
                cC_re[q, Pp] = c_re[g, :, p]; cC_im[q, Pp] = c_im[g, :, p]
                for d in range(2):
                    pA_re[q, d, Pp] = a_re[d, g, p]; pA_im[q, d, Pp] = a_im[d, g, p]; pA_dt[q, d, Pp] = ldt[d, g]
    L["pA_re"] = pA_re.reshape(128, 32); L["pA_im"] = pA_im.reshape(128, 32); L["pA_dt"] = pA_dt.reshape(128, 32)
    L["cC_re"] = cC_re.reshape(128, 256); L["cC_im"] = cC_im.reshape(128, 256); L["cMask"] = cMask
    L["eye32"] = np.tile(np.eye(32, dtype=f), (4, 1)); L["iota1"] = np.tile(np.arange(1, 513, dtype=f)[None, :], (128, 1))
    return L


def kernel(**inp):
    inp = {k: np.asarray(v) for k, v in inp.items()}
    if "nc" not in _NC_CACHE:
        _NC_CACHE["nc"] = build_program()
    nc = _NC_CACHE["nc"]
    L = host_layouts(inp)
    in_maps = []
    for c in range(8):
        m = dict(L)
        m["x"] = np.ascontiguousarray(inp["x"][NB * c:NB * (c + 1)].reshape(NB * SEQ, D))
        m["ctx"] = np.ascontiguousarray(inp["ctx"][NB * c:NB * (c + 1)].reshape(NB * CTX, D))
        cv = np.concatenate([inp["c"][NB * c:NB * (c + 1)], inp["c_ctx"][None, :]], axis=0)
        m["cvT"] = np.ascontiguousarray(cv.reshape(3, 8, 128).transpose(2, 1, 0))
        in_maps.append(m)
    res = run_bass_kernel_spmd(nc, in_maps, core_ids=list(range(8)))
    out = np.concatenate([r["out"].reshape(NB, SEQ, D) for r in res.results], axis=0)
    return out.astype(np.float32)
```

```python
import math
import numpy as np
import concourse.bass as bass
import concourse.mybir as mybir
from concourse.bass_utils import run_bass_kernel_spmd

F32 = mybir.dt.float32
BF16 = mybir.dt.bfloat16
I32 = mybir.dt.int32
ALU = mybir.AluOpType
AF = mybir.ActivationFunctionType
AX = mybir.AxisListType
ENGS = ("sync", "scalar", "vector", "gpsimd", "tensor")

NB = 2
SEQ = 4096
CTX = 256
D = 1024
NT = 34
TAU = 4608
EPS = 1e-6
PI = math.pi
LAM_INIT = 0.8 - 0.6 * math.exp(0.0)


class Prog:
    def __init__(self, nc):
        self.nc = nc
        self.ops = []
        self.last_w = {}
        self.readers = {}
        self.ctx = []
        self.last_eng = {}
        self.last_sem = {}

    def sb(self, name, shape, dt):
        self.uid = getattr(self, "uid", 0) + 1
        cm = self.nc.sbuf_tensor("s%d_%s" % (self.uid, name), shape, dt)
        t = cm.__enter__()
        self.ctx.append(cm)
        return t

    def ps(self, name, shape, dt):
        self.uid = getattr(self, "uid", 0) + 1
        cm = self.nc.psum_tensor("p%d_%s" % (self.uid, name), shape, dt)
        t = cm.__enter__()
        self.ctx.append(cm)
        return t

    def mark(self):
        return len(self.ctx)

    def release(self, mark):
        self.barrier()
        while len(self.ctx) > mark:
            self.ctx.pop().__exit__(None, None, None)

    def op(self, eng, fn, r=(), w=(), dma=False, semkey=None):
        i = len(self.ops)
        deps = set()
        for k in list(r) + list(w):
            if k in self.last_w:
                deps.add(self.last_w[k])
        for k in w:
            for q in self.readers.get(k, ()):
                deps.add(q)
        self.ops.append(dict(eng=eng, fn=fn, deps=deps, dma=dma, semkey=semkey))
        for k in w:
            self.last_w[k] = i
            self.readers[k] = []
        for k in r:
            self.readers.setdefault(k, []).append(i)
        if dma:
            self.last_sem[semkey] = i
        else:
            self.last_eng[eng] = i
        return i

    def call(self, eng, name, r=(), w=(), **kw):
        return self.op(eng, lambda e: getattr(e, name)(**kw), r=r, w=w)

    def dma(self, eng, out, in_, r=(), w=(), semkey=None, **kw):
        return self.op(eng, lambda e: e.dma_start(out=out, in_=in_, **kw), r=r, w=w, dma=True, semkey=semkey)

    def barrier(self):
        deps = set(self.last_eng.values()) | set(self.last_sem.values())
        for e in ENGS:
            i = len(self.ops)
            self.ops.append(dict(eng=e, fn=None, deps=set(deps), dma=False, semkey=None))
        self.last_w = {}
        self.readers = {}

    def emit(self, final_wait_ops=()):
        nc = self.nc
        ops = self.ops
        eng_sem = {}
        for e in ENGS:
            cm = nc.semaphore("p_" + e)
            eng_sem[e] = cm.__enter__()
            self.ctx.append(cm)
        needed = set(final_wait_ops)
        for o in ops:
            needed |= o["deps"]
        dma_sem = {}
        cnt = {e: 0 for e in ENGS}
        dcnt = {}
        for i, o in enumerate(ops):
            o["sem"] = None
            if o["fn"] is None:
                continue
            if o["dma"]:
                k = o["semkey"]
                if k not in dma_sem:
                    cm = nc.semaphore("d_%d" % len(dma_sem))
                    dma_sem[k] = cm.__enter__()
                    self.ctx.append(cm)
                    dcnt[k] = 0
                dcnt[k] += 16
                o["sem"] = dma_sem[k]
                o["ticket"] = dcnt[k]
                o["inc"] = 16
            elif i in needed:
                cnt[o["eng"]] += 1
                o["sem"] = eng_sem[o["eng"]]
                o["ticket"] = cnt[o["eng"]]
                o["inc"] = 1
        self.n_sems = len(dma_sem) + len(ENGS)
        per_eng = {e: [] for e in ENGS}
        for i, o in enumerate(ops):
            per_eng[o["eng"]].append(i)

        def run_engine(ename, eobj):
            waited = {}
            for i in per_eng[ename]:
                o = ops[i]
                for d in sorted(o["deps"]):
                    p = ops[d]
                    if p["fn"] is None:
                        continue
                    if p["eng"] == ename and not p["dma"] and ename == "tensor":
                        continue
                    sem = p["sem"]
                    key = id(sem)
                    if waited.get(key, 0) >= p["ticket"]:
                        continue
                    eobj.wait_ge(sem, p["ticket"])
                    waited[key] = p["ticket"]
                if o["fn"] is None:
                    continue
                ins = o["fn"](eobj)
                if o["sem"] is not None:
                    ins.then_inc(o["sem"], o["inc"])
            if ename == "sync":
                for d in final_wait_ops:
                    p = ops[d]
                    eobj.wait_ge(p["sem"], p["ticket"])

        with nc.Block() as block:
            @block.sync
            def _(e):
                run_engine("sync", e)

            @block.scalar
            def _(e):
                run_engine("scalar", e)

            @block.vector
            def _(e):
                run_engine("vector", e)

            @block.gpsimd
            def _(e):
                run_engine("gpsimd", e)

            @block.tensor
            def _(e):
                run_engine("tensor", e)

    def close(self):
        while self.ctx:
            self.ctx.pop().__exit__(None, None, None)


def build_program(dbg=None):
    nc = bass.Bass("TRN2", target_bir_lowering=False)
    T = {}

    def din(name, shape, dt=F32):
        T[name] = nc.dram_tensor(name, list(shape), dt, kind="ExternalInput").ap()
        return T[name]

    def dint(name, shape, dt):
        T[name] = nc.dram_tensor(name, list(shape), dt, kind="Internal").ap()
        return T[name]

    x_d = din("x", [NB * SEQ, D])
    ctx_d = din("ctx", [NB * CTX, D])
    cvT_d = din("cvT", [128, 8, 3])
    wada_d = din("w_ada", [D, 6 * D])
    bada_d = din("b_ada", [3, 6 * D])
    n1g_d = din("norm1_g", [128, D])
    n2g_d = din("norm2_g", [128, D])
    sel3_d = din("sel3", [3, 3 * 128])
    win_d = din("w_in", [D, 2048])
    qg_d = din("qg", [128, 512])
    kg_d = din("kg", [128, 512])
    lam_d = din("lamv", [128, 256])
    sub_d = din("subln", [128, 512])
    ropec_d = din("rope_cos", [SEQ, 512])
    ropes_d = din("rope_sin", [SEQ, 512])
    ident_d = din("ident", [128, 128])
    sA_re = din("sA_re", [128, 512]); sA_im = din("sA_im", [128, 512]); sA_dt = din("sA_dt", [128, 512])
    sB_re = din("sB_re", [128, 512]); sB_im = din("sB_im", [128, 512]); sMask = din("sMask", [128, 2])
    pA_re = din("pA_re", [128, 32]); pA_im = din("pA_im", [128, 32]); pA_dt = din("pA_dt", [128, 32])
    cC_re = din("cC_re", [128, 256]); cC_im = din("cC_im", [128, 256]); cMask = din("cMask", [128, 2])
    dsk_d = din("dskip", [128, 4]); eye32_d = din("eye32", [128, 32]); iota_d = din("iota1", [128, 512])
    wglu_d = din("w_glu", [512, 512]); bglu_d = din("b_gluT", [128, 4])
    wout_d = din("w_out", [D, D])
    wr_d = din("w_r", [D, 36]); br_d = din("b_r", [128, 36])
    weg_d = din("w_eg", [32, D, 512]); weu_d = din("w_eu", [32, D, 512]); wed_d = din("w_ed", [32, 512, D])
    out_d = nc.dram_tensor("out", [NB * SEQ, D], F32, kind="ExternalOutput").ap()
    mod_d = dint("mod_s", [3, 128, 6 * D], F32)
    qT_d = dint("qT_s", [NB, 4, 128, SEQ], BF16)
    uT_d = dint("uT_s", [NB, 4, 128, TAU], BF16)
    dint("tab_s", [32, 2, 128, 512], F32)
    x2T_d = dint("x2T_s", [NB * SEQ // 2048, 128, 8, 2048], BF16)
    dbg_t = {}
    if dbg:
        for name, shape in dbg.items():
            dbg_t[name] = nc.dram_tensor("dbg_" + name, list(shape), F32, kind="ExternalOutput").ap()

    P = Prog(nc)
    C = P.call
    fin = []

    ident = P.sb("ident", [128, 128], BF16)
    identf = P.sb("identf", [128, 128], F32)
    epst = P.sb("epst", [128, 1], F32)
    gates = P.sb("gates", [128, NB * 32, 32], F32)
    neglam = P.sb("neglam", [128, 1], F32)
    banks = [P.ps("bk%d" % i, [128, 512], F32) for i in range(6)]
    tbs = [P.ps("tb%d" % i, [128, 1024], BF16) for i in range(2)]
    P.dma("sync", identf[:], ident_d[:], w=["identf"], semkey="c0")
    C("vector", "tensor_copy", r=["identf"], w=["ident"], out=ident[:], in_=identf[:])
    C("vector", "memset", w=["epst"], ap=epst[:], constant=EPS)

    def rsqrt_rows(src, dst, scale, n, rk, wk):
        C("scalar", "activation", r=rk + ["epst"], w=wk, out=dst, in_=src, func=AF.Sqrt, bias=epst[:, 0:1], scale=scale)
        C("vector", "reciprocal", r=wk, w=wk, out=dst, in_=dst)

    m0 = P.mark()
    cvT = P.sb("cvT", [128, 8, 3], F32)
    cvS = P.sb("cvS", [128, 8, 3], BF16)
    wa = [P.sb("wa%d" % i, [128, 8, 512], BF16) for i in range(2)]
    modsb = P.sb("modsb", [3, 6 * D], F32)
    bad = P.sb("bad", [3, 6 * D], F32)
    lamt = P.sb("lamt", [128, 256], F32)
    lamw = P.sb("lamw", [128, 128], F32)
    lams = P.sb("lams", [128, 4], F32)
    P.dma("sync", cvT[:], cvT_d[:], w=["cvT"], semkey="c1")
    P.dma("sync", bad[:], bada_d[:], w=["bad"], semkey="c2")
    P.dma("sync", lamt[:], lam_d[:], w=["lamt"], semkey="c3")
    sel3f = P.sb("sel3f", [3, 384], F32); sel3 = P.sb("sel3", [3, 384], BF16)
    mhi = P.sb("mhi", [3, 6 * D], BF16); mlo = P.sb("mlo", [3, 6 * D], BF16)
    mstg = [P.sb("mstg%d" % i, [128, 512], F32) for i in range(2)]
    P.dma("sync", sel3f[:], sel3_d[:], w=["sel3f"], semkey="c2b")
    C("vector", "tensor_copy", r=["sel3f"], w=["sel3"], out=sel3[:], in_=sel3f[:])
    C("scalar", "activation", r=["cvT"], w=["cvS"], out=cvS[:], in_=cvT[:], func=AF.Silu)
    wada_v = wada_d.rearrange("(k p) n -> p k n", p=128)
    for cb in range(12):
        s = cb % 2
        P.dma("gpsimd", wa[s][:], wada_v[:, :, cb * 512:(cb + 1) * 512], w=["wa%d" % s], semkey="wa%d" % s)
        for k in range(8):
            C("tensor", "matmul", r=["wa%d" % s, "cvS"], w=["bk0"], out=banks[0][0:3, :], lhsT=cvS[:, k, :], rhs=wa[s][:, k, :], start=(k == 0), stop=(k == 7))
        C("vector", "tensor_tensor", r=["bk0", "bad"], w=["modsb"], out=modsb[:, cb * 512:(cb + 1) * 512], in0=banks[0][0:3, :], in1=bad[:, cb * 512:(cb + 1) * 512], op=ALU.add)
    C("vector", "tensor_copy", r=["modsb"], w=["mhi"], out=mhi[:], in_=modsb[:])
    C("vector", "tensor_tensor", r=["modsb", "mhi"], w=["mlo"], out=mlo[:], in0=modsb[:], in1=mhi[:], op=ALU.subtract)
    bc = 0
    for r_ in range(3):
        for cb in range(12):
            st = bc % 2
            bk = 1 + bc % 2
            bc += 1
            C("tensor", "matmul", r=["sel3", "mhi"], w=["bk%d" % bk], out=banks[bk][:], lhsT=sel3[:, r_ * 128:(r_ + 1) * 128], rhs=mhi[:, cb * 512:(cb + 1) * 512], start=True, stop=False)
            C("tensor", "matmul", r=["sel3", "mlo"], w=["bk%d" % bk], out=banks[bk][:], lhsT=sel3[:, r_ * 128:(r_ + 1) * 128], rhs=mlo[:, cb * 512:(cb + 1) * 512], start=False, stop=True)
            C("vector", "tensor_copy", r=["bk%d" % bk], w=["mstg%d" % st], out=mstg[st][:], in_=banks[bk][:])
            P.dma("sync", mod_d[r_, :, cb * 512:(cb + 1) * 512], mstg[st][:], r=["mstg%d" % st], w=["mod_d"], semkey="mstg%d" % st)
    C("vector", "tensor_tensor", r=["lamt"], w=["lamw"], out=lamw[:, 0:64], in0=lamt[:, 0:64], in1=lamt[:, 64:128], op=ALU.mult)
    C("vector", "tensor_tensor", r=["lamt"], w=["lamw"], out=lamw[:, 64:128], in0=lamt[:, 128:192], in1=lamt[:, 192:256], op=ALU.mult)
    C("vector", "tensor_reduce", r=["lamw"], w=["lams"], out=lams[:, 0:2], in_=lamw[:].rearrange("p (a b) -> p a b", b=64), axis=AX.X, op=ALU.add)
    C("scalar", "activation", r=["lams"], w=["lams"], out=lams[:, 2:4], in_=lams[:, 0:2], func=AF.Exp)
    C("vector", "tensor_tensor", r=["lams"], w=["lams"], out=lams[:, 0:1], in0=lams[:, 3:4], in1=lams[:, 2:3], op=ALU.subtract)
    C("vector", "tensor_scalar", r=["lams"], w=["neglam"], out=neglam[:], in0=lams[:, 0:1], scalar1=-LAM_INIT, scalar2=None, op0=ALU.add)
    P.release(m0)

    def load_mod_bcast(dst, key, row, chunk, semkey):
        P.dma("sync", dst[:], mod_d[row, :, chunk * D:(chunk + 1) * D], r=["mod_d"], w=[key], semkey=semkey)

    def norm_mod(xt, xk, A, Ak, B, Bk, outt, outk, W):
        sq, sqk, ss, ssk, t_, tk = W
        C("scalar", "activation", r=[xk], w=[sqk], out=sq[:], in_=xt, func=AF.Square)
        C("vector", "tensor_reduce", r=[sqk], w=[ssk], out=ss[:, 0:1], in_=sq[:], axis=AX.X, op=ALU.add)
        rsqrt_rows(ss[:, 0:1], ss[:, 1:2], 1.0 / D, 1, [ssk], [ssk])
        C("vector", "scalar_tensor_tensor", r=[xk, ssk, Ak], w=[tk], out=t_[:], in0=xt, scalar=ss[:, 1:2], in1=A[:], op0=ALU.mult, op1=ALU.mult)
        C("gpsimd", "tensor_tensor", r=[tk, Bk], w=[outk], out=outt, in0=t_[:], in1=B[:], op=ALU.add)

    nm_sq = P.sb("nm_sq", [128, D], F32)
    nm_ss = P.sb("nm_ss", [128, 2], F32)
    nm_ss2 = P.sb("nm_ss2", [128, 2], F32)
    nm_t = P.sb("nm_t", [128, D], F32)

    for b in range(NB):
        mb = P.mark()
        catA = P.sb("catA", [128, 4, SEQ], BF16)
        m1 = P.mark()
        KT = P.sb("KT", [128, 4, NT * 128], BF16)
        Vaug = P.sb("Vaug", [128, NT, 4, 130], BF16)
        mA = P.mark()
        winb = P.sb("winb", [128, 8, 2048], BF16)
        A1 = P.sb("A1", [128, D], F32); B1 = P.sb("B1", [128, D], F32)
        A1c = P.sb("A1c", [128, D], F32); B1c = P.sb("B1c", [128, D], F32)
        g1b = nm_sq
        Gq = P.sb("Gq", [128, 8, 64], F32); Gk = P.sb("Gk", [128, 8, 64], F32)
        xts = [P.sb("xt%d" % i, [128, D], F32) for i in range(2)]
        xmbs = [P.sb("xmb%d" % i, [128, D], BF16) for i in range(2)]
        NW = [(nm_sq, "nm_sq", nm_ss, "nm_ss", nm_t, "nm_t")] * 2
        xmT = [P.sb("xmT%d" % i, [128, 8, 128], BF16) for i in range(2)]
        sqt = P.sb("sqt", [128, 512], F32)
        ssq = P.sb("ssq", [128, 16], F32)
        ssq2 = P.sb("ssq2", [128, 16], F32)
        qn = P.sb("qn", [128, 512], F32)
        qn2 = P.sb("qn2", [128, 512], F32)
        qr = P.sb("qr", [128, 512], BF16)
        rt = [P.sb("rt%d" % i, [128, 512], F32) for i in range(4)]
        rcs = [P.sb("rc0", [128, 512], F32)] * 2; rss = [P.sb("rs_0", [128, 512], F32)] * 2
        usb = P.sb("usb", [128, 4, 128], BF16)
        qTs = P.sb("qTs", [128, 4, 128], BF16)
        P.dma("gpsimd", winb[:], win_d.rearrange("(k p) n -> p k n", p=128), w=["winb"], semkey="winb")
        P.dma("sync", g1b[:], n1g_d[:], w=["nm_sq"], semkey="c5")
        load_mod_bcast(B1, "B1", b, 0, "c6"); load_mod_bcast(A1, "A1", b, 1, "c7")
        load_mod_bcast(B1c, "B1c", 2, 0, "c8"); load_mod_bcast(A1c, "A1c", 2, 1, "c9")
        for Ax, k_ in ((A1, "A1"), (A1c, "A1c")):
            C("vector", "scalar_tensor_tensor", r=[k_, "nm_sq"], w=[k_], out=Ax[:], in0=Ax[:], scalar=1.0, in1=g1b[:], op0=ALU.add, op1=ALU.mult)
        P.dma("sync", Gq[:].rearrange("p a b -> p (a b)"), qg_d[:], w=["Gq"], semkey="c10")
        P.dma("sync", Gk[:].rearrange("p a b -> p (a b)"), kg_d[:], w=["Gk"], semkey="c11")
        C("vector", "memset", w=["Vaug"], ap=Vaug[:, :, :, 128:130], constant=1.0)

        caf = [catA[:, hh, :].bitcast(F32) for hh in range(3)]
        QT_ = dict(sqt=caf[0][:, 0:512], qn=caf[0][:, 512:1024], qn2=caf[0][:, 1024:1536], rt=[caf[1][:, i * 512:(i + 1) * 512] for i in range(4)],
                   qr=catA[:, 3, 0:512], ssq=ssq2)

        def qk_post(bank, bkey, G, Gkey, rope_lt, dst_is_q, tt):
            if dst_is_q:
                return qk_post_q(bank, bkey, G, Gkey, rope_lt, tt)
            sqt_, sqk_ = sqt, "sqt"
            rc, rs_ = rcs[tt % 2], rss[tt % 2]
            rck, rsk = "rc0", "rs_0"
            C("scalar", "activation", r=[bkey], w=[sqk_], out=sqt_[:], in_=bank[:], func=AF.Square)
            C("vector", "tensor_reduce", r=[sqk_], w=["ssq"], out=ssq[:, 0:8], in_=sqt_[:].rearrange("p (a b) -> p a b", b=64), axis=AX.X, op=ALU.add)
            rsqrt_rows(ssq[:, 0:8], ssq[:, 8:16], 1.0 / 64, 8, ["ssq"], ["ssq"])
            C("vector", "tensor_tensor", r=[bkey, "ssq"], w=["qn"], out=qn[:].rearrange("p (a b) -> p a b", b=64), in0=bank[:].rearrange("p (a b) -> p a b", b=64), in1=ssq[:, 8:16].unsqueeze(2).to_broadcast([128, 8, 64]), op=ALU.mult)
            if rope_lt is None:
                C("gpsimd", "tensor_tensor", r=["qn", Gkey], w=["qr"], out=qr[:], in0=qn[:], in1=G[:].rearrange("p a b -> p (a b)"), op=ALU.mult)
            else:
                C("gpsimd", "tensor_tensor", r=["qn", Gkey], w=["qn2"], out=qn2[:], in0=qn[:], in1=G[:].rearrange("p a b -> p (a b)"), op=ALU.mult)
                v = lambda t, h: t[:].rearrange("p (a h f) -> p a h f", h=2, f=16)[:, :, h, :]
                C("vector", "tensor_tensor", r=["qn2", rck], w=["rt0"], out=v(rt[0], 0), in0=v(qn2, 0), in1=v(rc, 0), op=ALU.mult)
                C("vector", "tensor_tensor", r=["qn2", rsk], w=["rt1"], out=v(rt[1], 0), in0=v(qn2, 1), in1=v(rs_, 0), op=ALU.mult)
                C("vector", "tensor_tensor", r=["rt0", "rt1"], w=["qr"], out=v(qr, 0), in0=v(rt[0], 0), in1=v(rt[1], 0), op=ALU.subtract)
                C("gpsimd", "tensor_tensor", r=["qn2", rck], w=["rt2"], out=v(rt[2], 0), in0=v(qn2, 1), in1=v(rc, 0), op=ALU.mult)
                C("gpsimd", "tensor_tensor", r=["qn2", rsk], w=["rt3"], out=v(rt[3], 0), in0=v(qn2, 0), in1=v(rs_, 0), op=ALU.mult)
                C("gpsimd", "tensor_tensor", r=["rt2", "rt3"], w=["qr"], out=v(qr, 1), in0=v(rt[2], 0), in1=v(rt[3], 0), op=ALU.add)
            for h in range(4):
                C("tensor", "transpose", r=["qr", "ident"], w=["tb1"], out=tbs[1][:, h * 128:(h + 1) * 128], in_=qr[:, h * 128:(h + 1) * 128], identity=ident[:])
            if dst_is_q:
                C("scalar", "copy", r=["tb1"], w=["qTs"], out=qTs[:], in_=tbs[1][:, 0:512].rearrange("p (h t) -> p h t", t=128))
                P.dma("sync", qT_d[b].rearrange("h p n -> p h n")[:, :, rope_lt * 128:(rope_lt + 1) * 128], qTs[:], r=["qTs"], w=["qT_d"], semkey="qTs")
            else:
                C("scalar", "copy", r=["tb1"], w=["KT"], out=KT[:, :, tt * 128:(tt + 1) * 128], in_=tbs[1][:, 0:512].rearrange("p (h t) -> p h t", t=128))

        def qk_post_q(bank, bkey, G, Gkey, lt_, tt):
            q_sq, q_n, q_n2, q_rt, q_r, q_ss = QT_["sqt"], QT_["qn"], QT_["qn2"], QT_["rt"], QT_["qr"], QT_["ssq"]
            rc, rs_ = rcs[0], rss[0]
            g3 = lambda ap: ap.rearrange("p (a b) -> p a b", b=64)
            C("scalar", "activation", r=[bkey], w=["q_sq"], out=q_sq, in_=bank[:], func=AF.Square)
            C("vector", "tensor_reduce", r=["q_sq"], w=["q_ss"], out=q_ss[:, 0:8], in_=g3(q_sq), axis=AX.X, op=ALU.add)
            rsqrt_rows(q_ss[:, 0:8], q_ss[:, 8:16], 1.0 / 64, 8, ["q_ss"], ["q_ss"])
            C("vector", "tensor_tensor", r=[bkey, "q_ss"], w=["q_n"], out=g3(q_n), in0=g3(bank[:]), in1=q_ss[:, 8:16].unsqueeze(2).to_broadcast([128, 8, 64]), op=ALU.mult)
            C("gpsimd", "tensor_tensor", r=["q_n", Gkey], w=["q_n2"], out=q_n2, in0=q_n, in1=G[:].rearrange("p a b -> p (a b)"), op=ALU.mult)
            v = lambda ap, h: ap.rearrange("p (a h f) -> p a h f", h=2, f=16)[:, :, h, :]
            C("vector", "tensor_tensor", r=["q_n2", "rc0"], w=["q_rt0"], out=v(q_rt[0], 0), in0=v(q_n2, 0), in1=v(rc[:], 0), op=ALU.mult)
            C("vector", "tensor_tensor", r=["q_n2", "rs_0"], w=["q_rt1"], out=v(q_rt[1], 0), in0=v(q_n2, 1), in1=v(rs_[:], 0), op=ALU.mult)
            C("vector", "tensor_tensor", r=["q_rt0", "q_rt1"], w=["q_r"], out=v(q_r, 0), in0=v(q_rt[0], 0), in1=v(q_rt[1], 0), op=ALU.subtract)
            C("gpsimd", "tensor_tensor", r=["q_n2", "rc0"], w=["q_rt2"], out=v(q_rt[2], 0), in0=v(q_n2, 1), in1=v(rc[:], 0), op=ALU.mult)
            C("gpsimd", "tensor_tensor", r=["q_n2", "rs_0"], w=["q_rt3"], out=v(q_rt[3], 0), in0=v(q_n2, 0), in1=v(rs_[:], 0), op=ALU.mult)
            C("gpsimd", "tensor_tensor", r=["q_rt2", "q_rt3"], w=["q_r"], out=v(q_r, 1), in0=v(q_rt[2], 0), in1=v(q_rt[3], 0), op=ALU.add)
            for h in range(4):
                C("tensor", "transpose", r=["q_r", "ident"], w=["tb1"], out=tbs[1][:, h * 128:(h + 1) * 128], in_=q_r[:, h * 128:(h + 1) * 128], identity=ident[:])
            C("scalar", "copy", r=["tb1"], w=["qTs"], out=qTs[:], in_=tbs[1][:, 0:512].rearrange("p (h t) -> p h t", t=128))
            P.dma("sync", qT_d[b].rearrange("h p n -> p h n")[:, :, lt_ * 128:(lt_ + 1) * 128], qTs[:], r=["qTs"], w=["qT_d"], semkey="qTs")

        def p1_load_x(tt_):
            s_ = tt_ % 2
            lt_ = tt_ - 2
            src = ctx_d[b * CTX + tt_ * 128: b * CTX + (tt_ + 1) * 128, :] if tt_ < 2 else x_d[b * SEQ + lt_ * 128: b * SEQ + (lt_ + 1) * 128, :]
            P.dma("sync", xts[s_][:], src, w=["xt%d" % s_], semkey="xt%d" % s_)

        def p1_load_rope(tt_):
            lt_ = tt_ - 2
            if lt_ >= 0:
                P.dma("sync", rcs[0][:], ropec_d[lt_ * 128:(lt_ + 1) * 128, :], w=["rc0"], semkey="rc0")
                P.dma("sync", rss[0][:], ropes_d[lt_ * 128:(lt_ + 1) * 128, :], w=["rs_0"], semkey="rs_0")

        p1_load_x(0)
        for tt in range(NT):
            s = tt % 2
            lt = tt - 2
            isctx = tt < 2
            if tt + 1 < NT:
                p1_load_x(tt + 1)
            xmb = xmbs[s]
            if isctx:
                norm_mod(xts[s][:], "xt%d" % s, A1c, "A1c", B1c, "B1c", xmb[:], "xmb%d" % s, NW[s])
            else:
                norm_mod(xts[s][:], "xt%d" % s, A1, "A1", B1, "B1", xmb[:], "xmb%d" % s, NW[s])
            for k in range(8):
                C("tensor", "transpose", r=["xmb%d" % s, "ident"], w=["tb0"], out=tbs[0][:, k * 128:(k + 1) * 128], in_=xmb[:, k * 128:(k + 1) * 128], identity=ident[:])
            C("scalar", "copy", r=["tb0"], w=["xmT%d" % s], out=xmT[s][:].rearrange("p k t -> p (k t)"), in_=tbs[0][:])
            todo = [(512, 1), (1024, 2)] if isctx else [(0, 0), (512, 1), (1024, 2)]
            for col, bi in todo:
                for k in range(8):
                    C("tensor", "matmul", r=["xmT%d" % s, "winb"], w=["bk%d" % bi], out=banks[bi][:], lhsT=xmT[s][:, k, :], rhs=winb[:, k, col:col + 512], start=(k == 0), stop=(k == 7))
            for j in range(4):
                for k in range(8):
                    C("tensor", "matmul", r=["xmT%d" % s, "winb"], w=["bk3"], out=banks[3][:, j * 128:(j + 1) * 128], lhsT=winb[:, k, 1536 + 128 * j:1536 + 128 * (j + 1)], rhs=xmT[s][:, k, :], start=(k == 0), stop=(k == 7))
            C("scalar", "copy", r=["bk2"], w=["Vaug"], out=Vaug[:, tt, :, 0:128], in_=banks[2][:].rearrange("p (h e) -> p h e", e=128))
            C("vector", "tensor_copy", r=["bk3"], w=["usb"], out=usb[:].rearrange("p j t -> p (j t)"), in_=banks[3][:])
            uv = uT_d[b].rearrange("j p n -> p j n")
            if isctx:
                P.dma("sync", uv[:, :, tt * 128:(tt + 1) * 128], usb[:], r=["usb"], w=["uT_d"], semkey="usb")
                P.dma("sync", uv[:, :, 4352 + tt * 128:4352 + (tt + 1) * 128], usb[:], r=["usb"], w=["uT_d"], semkey="usb")
            else:
                P.dma("sync", uv[:, :, tt * 128:(tt + 1) * 128], usb[:], r=["usb"], w=["uT_d"], semkey="usb")
            qk_post(banks[1], "bk1", Gk, "Gk", None if isctx else lt, False, tt)
            if not isctx:
                qk_post(banks[0], "bk0", Gq, "Gq", lt, True, tt)
            if tt + 1 < NT:
                p1_load_rope(tt + 1)
        P.release(mA)

        m2 = P.mark()
        SG4 = P.sb("SG4", [128, 4, 128], F32)
        qblk = [P.sb("qblk%d" % i, [128, 512], BF16) for i in range(2)]
        pts = [P.sb("pt%d" % i, [128, 512], BF16) for i in range(4)]
        stv = [banks[0][:], banks[1][:], banks[2][:], tbs[1][:].bitcast(F32)]
        stk = ["bk0", "bk1", "bk2", "tb1"]
        osb = [P.sb("osb%d" % i, [128, 8, 129], F32) for i in range(2)]
        rr8 = P.sb("rr8", [128, 2, 8], F32)
        ss4 = P.sb("ss4", [128, 2, 4], F32)
        ept0 = P.sb("ept0", [128, 4, 128], F32)
        ept1 = P.sb("ept1", [128, 4, 128], F32)
        epoo = P.sb("epoo", [128, 4, 128], F32)
        ab4 = P.sb("ab4", [128, 4, 128], BF16)
        P.dma("sync", SG4[:].rearrange("p a b -> p (a b)"), sub_d[:], w=["SG4"], semkey="c12")
        C("vector", "tensor_scalar", r=["SG4"], w=["SG4"], out=SG4[:], in0=SG4[:], scalar1=1.0 - LAM_INIT, scalar2=None, op0=ALU.mult)
        obank = [banks[3], banks[4], banks[5]]

        def oreg(c, qt, lo, hi):
            r_ = c * 4 + qt
            return obank[r_ // 3][:, (r_ % 3) * 129 + lo:(r_ % 3) * 129 + hi], "bk%d" % (3 + r_ // 3)

        units = [(h, qb) for h in range(4) for qb in range(8)]
        steps = [(kt, c) for kt in range(NT) for c in range(2)]
        NS = len(steps)

        def load_q(ui):
            h, qb = units[ui]
            s = ui % 2
            P.dma("sync", qblk[s][:], qT_d[b, h, :, qb * 512:(qb + 1) * 512], r=["qT_d"], w=["qblk%d" % s], semkey="qblk%d" % s)

        def ep_stage1(ui):
            s = ui % 2
            for bi, (r0, r1) in enumerate(((0, 3), (3, 6), (6, 8))):
                nr = r1 - r0
                C("vector", "tensor_copy", r=["bk%d" % (3 + bi)], w=["osb%d" % s], out=osb[s][:, r0:r1, :], in_=obank[bi][:, 0:nr * 129].rearrange("p (a b) -> p a b", b=129))
            C("vector", "reciprocal", r=["osb%d" % s], w=["rr8"], out=rr8[:, s, :], in_=osb[s][:, :, 128])
            C("vector", "tensor_scalar", r=["rr8", "neglam"], w=["rr8"], out=rr8[:, s, 4:8], in0=rr8[:, s, 4:8], scalar1=neglam[:, 0:1], scalar2=None, op0=ALU.mult)
            C("vector", "tensor_tensor", r=["osb%d" % s, "rr8"], w=["ept0"], out=ept0[:], in0=osb[s][:, 0:4, 0:128], in1=rr8[:, s, 0:4].unsqueeze(2).to_broadcast([128, 4, 128]), op=ALU.mult)
            C("vector", "tensor_tensor", r=["osb%d" % s, "rr8"], w=["ept1"], out=ept1[:], in0=osb[s][:, 4:8, 0:128], in1=rr8[:, s, 4:8].unsqueeze(2).to_broadcast([128, 4, 128]), op=ALU.mult)
            C("gpsimd", "tensor_tensor", r=["ept0", "ept1"], w=["epoo"], out=epoo[:], in0=ept0[:], in1=ept1[:], op=ALU.add)
            C("gpsimd", "tensor_tensor", r=["epoo"], w=["ept0"], out=ept0[:], in0=epoo[:], in1=epoo[:], op=ALU.mult)
            C("vector", "tensor_reduce", r=["ept0"], w=["ss4"], out=ss4[:, s, :], in_=ept0[:], axis=AX.X, op=ALU.add)

        def ep_stage2(ui):
            s = ui % 2
            C("scalar", "activation", r=["ss4", "epst"], w=["ss4"], out=ss4[:, s, :], in_=ss4[:, s, :], func=AF.Sqrt, bias=epst[:, 0:1], scale=1.0 / 128)

        def ep_stage3(ui):
            s = ui % 2
            C("vector", "reciprocal", r=["ss4"], w=["ss4"], out=ss4[:, s, :], in_=ss4[:, s, :])
            C("vector", "tensor_tensor", r=["epoo", "ss4"], w=["ept1"], out=ept1[:], in0=epoo[:], in1=ss4[:, s, :].unsqueeze(2).to_broadcast([128, 4, 128]), op=ALU.mult)
            C("gpsimd", "tensor_tensor", r=["ept1", "SG4"], w=["ab4"], out=ab4[:], in0=ept1[:], in1=SG4[:], op=ALU.mult)
            for qt in range(4):
                C("tensor", "transpose", r=["ab4", "ident"], w=["tb0"], out=tbs[0][:, qt * 128:(qt + 1) * 128], in_=ab4[:, qt, :], identity=ident[:])

        def ep_stage4(ui):
            h, qb = units[ui]
            C("scalar", "copy", r=["tb0"], w=["catA"], out=catA[:, h, qb * 512:(qb + 1) * 512], in_=tbs[0][:, 0:512])

        gstep = 0
        load_q(0)
        load_q(1)
        for ui, (h, qb) in enumerate(units):
            s = ui % 2
            base = gstep

            def qk(i):
                kt, c = steps[i]
                si = (base + i) % 4
                C("tensor", "matmul", r=["KT", "qblk%d" % s], w=[stk[si]], out=stv[si], lhsT=KT[64 * c:64 * c + 64, h, kt * 128:(kt + 1) * 128], rhs=qblk[s][64 * c:64 * c + 64, :], start=True, stop=True)

            qk(0)
            qk(1)
            for i in range(NS):
                kt, c = steps[i]
                si = (base + i) % 4
                if i % 2 == 0 and i + 2 < NS:
                    qk(i + 2)
                    qk(i + 3)
                C("scalar", "activation", r=[stk[si]], w=["pt%d" % si], out=pts[si][:], in_=stv[si], func=AF.Exp, scale=0.125)
                for qt in range(4):
                    o_ap, o_key = oreg(c, qt, 0, 129)
                    C("tensor", "matmul", r=["pt%d" % si, "Vaug"], w=[o_key], out=o_ap, lhsT=pts[si][:, qt * 128:(qt + 1) * 128], rhs=Vaug[:, kt, h, 0:129], start=(kt == 0), stop=(kt == NT - 1), skip_group_check=True)
                if ui > 0:
                    if i == 12:
                        ep_stage2(ui - 1)
                    elif i == 18:
                        ep_stage3(ui - 1)
                    elif i == 30:
                        ep_stage4(ui - 1)
            gstep += NS
            ep_stage1(ui)
            if ui + 2 < len(units):
                load_q(ui + 2)
        ep_stage2(len(units) - 1)
        ep_stage3(len(units) - 1)
        ep_stage4(len(units) - 1)
        P.release(m1)

        catS = P.sb("catS", [128, 4, SEQ], BF16)
        m3 = P.mark()
        ssm_phase(P, C, T, b, banks, catS, epst, locals())
        P.release(m3)

        m4 = P.mark()
        woutb = P.sb("woutb", [128, 8, D], BF16)
        G1 = P.sb("G1", [128, D], F32); A2 = P.sb("A2", [128, D], F32); B2 = P.sb("B2", [128, D], F32)
        g2b = nm_sq
        xts = [P.sb("xq%d" % i, [128, D], F32) for i in range(2)]
        x1 = [P.sb("x1_%d" % i, [128, D], F32) for i in range(2)]
        nsq4 = P.sb("nsq4", [128, D], F32); nt4 = P.sb("nt4", [128, D], F32)
        NW4 = [(nm_sq, "nm_sq", nm_ss, "nm_ss", nm_t, "nm_t"), (nsq4, "nsq4", nm_ss2, "nm_ss2", nt4, "nt4")]
        gt4 = [P.sb("gt4_%d" % i, [128, D], F32) for i in range(2)]
        xm2s = [P.sb("xm2_%d" % i, [128, D], F32) for i in range(2)]
        x2Tfs = [P.sb("x2Tf%d" % i, [128, 8, 128], F32) for i in range(2)]
        x2Tbs = [P.sb("x2Tb%d" % i, [128, 8, 128], BF16) for i in range(2)]
        wrf = P.sb("wrf", [128, 8, 36], F32)
        brb = P.sb("brb", [128, 36], F32)
        lgs = [P.sb("lg%d" % i, [128, 36], F32) for i in range(2)]
        gws = [P.sb("gw%d" % i, [128, 16], F32) for i in range(2)]
        ohgs = [P.sb("ohg%d" % i, [128, 4], F32) for i in range(2)]
        msks = [P.sb("msk%d" % i, [128, 32], F32) for i in range(2)]
        msk2s = [P.sb("msk2%d" % i, [128, 32], F32) for i in range(2)]
        oh1s = [P.sb("oh1%d" % i, [128, 32], F32) for i in range(2)]
        oh2s = [P.sb("oh2%d" % i, [128, 32], F32) for i in range(2)]
        P.dma("gpsimd", woutb[:], wout_d.rearrange("(k p) n -> p k n", p=128), w=["woutb"], semkey="woutb")
        P.dma("sync", g2b[:], n2g_d[:], w=["nm_sq"], semkey="c13")
        load_mod_bcast(G1, "G1", b, 2, "c14"); load_mod_bcast(B2, "B2", b, 3, "c15"); load_mod_bcast(A2, "A2", b, 4, "c16")
        C("vector", "scalar_tensor_tensor", r=["A2", "nm_sq"], w=["A2"], out=A2[:], in0=A2[:], scalar=1.0, in1=g2b[:], op0=ALU.add, op1=ALU.mult)
        P.dma("sync", wrf[:], wr_d.rearrange("(k p) n -> p k n", p=128), w=["wrf"], semkey="c17")
        P.dma("sync", brb[:], br_d[:], w=["brb"], semkey="c18")
        for lt in range(32):
            s = lt % 2
            sx = "_%d" % s
            ob = (0, 1) if s == 0 else (4, 5)
            xm2, x2Tf, x2Tb = xm2s[s], x2Tfs[s], x2Tbs[s]
            lg, gw, ohg, msk, msk2, oh1, oh2 = lgs[s], gws[s], ohgs[s], msks[s], msk2s[s], oh1s[s], oh2s[s]
            K = lambda nme: nme + sx
            row0 = b * SEQ + lt * 128
            if lt == 0:
                P.dma("sync", xts[0][:], x_d[row0:row0 + 128, :], w=["xq0"], semkey="xq0")
            if lt + 1 < 32:
                P.dma("sync", xts[1 - s][:], x_d[row0 + 128:row0 + 256, :], w=["xq%d" % (1 - s)], semkey="xq%d" % (1 - s))
            for half in range(2):
                for k in range(8):
                    C("tensor", "matmul", r=["catA", "catS", "woutb"], w=["bk%d" % ob[half]], out=banks[ob[half]][:], lhsT=(catA if k < 4 else catS)[:, k % 4, lt * 128:(lt + 1) * 128], rhs=woutb[:, k, half * 512:(half + 1) * 512], start=(k == 0), stop=(k == 7))
            for half in range(2):
                sl = slice(half * 512, (half + 1) * 512)
                C("vector", "tensor_tensor", r=["bk%d" % ob[half], "G1"], w=[K("gt4")], out=gt4[s][:, sl], in0=banks[ob[half]][:], in1=G1[:, sl], op=ALU.mult)
            C("gpsimd", "tensor_tensor", r=[K("gt4"), "xq%d" % s], w=["x1_%d" % s], out=x1[s][:], in0=gt4[s][:], in1=xts[s][:], op=ALU.add)
            P.dma("sync", out_d[row0:row0 + 128, :], x1[s][:], r=["x1_%d" % s], w=["out_d%d" % (row0 // 128)], semkey="x1_%d" % s)
            norm_mod(x1[s][:], "x1_%d" % s, A2, "A2", B2, "B2", xm2[:], K("xm2"), NW4[s])
            for k in range(8):
                bi = 2 + k // 4
                C("tensor", "transpose", r=[K("xm2"), "identf"], w=["bk%d" % bi], out=banks[bi][:, (k % 4) * 128:(k % 4 + 1) * 128], in_=xm2[:, k * 128:(k + 1) * 128], identity=identf[:])
            for hh in range(2):
                C("scalar", "copy", r=["bk%d" % (2 + hh)], w=[K("x2Tf")], out=x2Tf[:, hh * 4:(hh + 1) * 4, :].rearrange("p k t -> p (k t)"), in_=banks[2 + hh][:])
            C("vector", "tensor_copy", r=[K("x2Tf")], w=[K("x2Tb")], out=x2Tb[:], in_=x2Tf[:])
            P.dma("sync", x2T_d[row0 // 2048, :, :, row0 % 2048:row0 % 2048 + 128], x2Tb[:], r=[K("x2Tb")], w=["x2T_d"], semkey="x2Tb%d" % s)
            rb = banks[ob[0]]
            rbk = "bk%d" % ob[0]
            for k in range(8):
                C("tensor", "matmul", r=[K("x2Tf"), "wrf"], w=[rbk], out=rb[:, 0:36], lhsT=x2Tf[:, k, :], rhs=wrf[:, k, :], start=(k == 0), stop=(k == 7))
            C("vector", "tensor_tensor", r=[rbk, "brb"], w=[K("lg")], out=lg[:], in0=rb[:, 0:36], in1=brb[:], op=ALU.add)
            gidx = b * 32 + lt
            gk, lk, ok_, mk, m2k, o1k, o2k = K("gw"), K("lg"), K("ohg"), K("msk"), K("msk2"), K("oh1"), K("oh2")
            C("vector", "tensor_reduce", r=[lk], w=[gk], out=gw[:, 0:1], in_=lg[:, 0:4], axis=AX.X, op=ALU.max)
            C("vector", "tensor_scalar", r=[lk, gk], w=[ok_], out=ohg[:], in0=lg[:, 0:4], scalar1=gw[:, 0:1], scalar2=None, op0=ALU.is_ge)
            C("vector", "tensor_scalar", r=[gk], w=[gk], out=gw[:, 1:2], in0=gw[:, 0:1], scalar1=-1.0, scalar2=None, op0=ALU.mult)
            C("scalar", "activation", r=[lk, gk], w=[gk], out=gw[:, 4:8], in_=lg[:, 0:4], func=AF.Exp, bias=gw[:, 1:2], scale=1.0)
            C("vector", "tensor_reduce", r=[gk], w=[gk], out=gw[:, 2:3], in_=gw[:, 4:8], axis=AX.X, op=ALU.add)
            C("vector", "reciprocal", r=[gk], w=[gk], out=gw[:, 3:4], in_=gw[:, 2:3])
            C("vector", "tensor_scalar", r=[ok_], w=[ok_], out=ohg[:], in0=ohg[:], scalar1=-1.0, scalar2=1e30, op0=ALU.add, op1=ALU.mult)
            C("vector", "tensor_tensor", r=[lk, ok_], w=[mk], out=msk[:].rearrange("p (g e) -> p g e", e=8), in0=lg[:, 4:36].rearrange("p (g e) -> p g e", e=8), in1=ohg[:].unsqueeze(2).to_broadcast([128, 4, 8]), op=ALU.add)
            C("vector", "tensor_reduce", r=[mk], w=[gk], out=gw[:, 8:9], in_=msk[:], axis=AX.X, op=ALU.max)
            C("vector", "tensor_scalar", r=[mk, gk], w=[o1k], out=oh1[:], in0=msk[:], scalar1=gw[:, 8:9], scalar2=None, op0=ALU.is_ge)
            C("vector", "scalar_tensor_tensor", r=[o1k, mk], w=[m2k], out=msk2[:], in0=oh1[:], scalar=-1e30, in1=msk[:], op0=ALU.mult, op1=ALU.add)
            C("vector", "tensor_reduce", r=[m2k], w=[gk], out=gw[:, 9:10], in_=msk2[:], axis=AX.X, op=ALU.max)
            C("vector", "tensor_scalar", r=[m2k, gk], w=[o2k], out=oh2[:], in0=msk2[:], scalar1=gw[:, 9:10], scalar2=None, op0=ALU.is_ge)
            C("vector", "tensor_tensor", r=[gk], w=[gk], out=gw[:, 10:11], in0=gw[:, 9:10], in1=gw[:, 8:9], op=ALU.subtract)
            C("scalar", "activation", r=[gk], w=[gk], out=gw[:, 11:12], in_=gw[:, 10:11], func=AF.Exp)
            C("vector", "tensor_scalar", r=[gk], w=[gk], out=gw[:, 12:13], in0=gw[:, 11:12], scalar1=1.0, scalar2=None, op0=ALU.add)
            C("vector", "reciprocal", r=[gk], w=[gk], out=gw[:, 12:13], in_=gw[:, 12:13])
            C("vector", "tensor_tensor", r=[gk], w=[gk], out=gw[:, 13:14], in0=gw[:, 12:13], in1=gw[:, 3:4], op=ALU.mult)
            C("vector", "tensor_tensor", r=[gk], w=[gk], out=gw[:, 14:15], in0=gw[:, 13:14], in1=gw[:, 11:12], op=ALU.mult)
            C("vector", "tensor_scalar", r=[o1k, gk], w=[o1k], out=oh1[:], in0=oh1[:], scalar1=gw[:, 13:14], scalar2=None, op0=ALU.mult)
            C("vector", "scalar_tensor_tensor", r=[o2k, gk, o1k], w=["gates"], out=gates[:, gidx, :], in0=oh2[:], scalar=gw[:, 14:15], in1=oh1[:], op0=ALU.mult, op1=ALU.add)
        P.release(mb)

    m5 = P.mark()
    SB = 2048
    acc = P.sb("acc", [128, 16, D], F32)
    x2Ts = [P.sb("x2T%d" % i, [128, 8, SB], BF16) for i in range(2)]
    wg = [P.sb("wg%d" % i, [128, 8, 512], BF16) for i in range(2)]
    wu = [P.sb("wu%d" % i, [128, 8, 512], BF16) for i in range(2)]
    wd = [P.sb("wd%d" % i, [128, 4, D], BF16) for i in range(2)]
    silb = P.sb("silb", [128, 2, 512], F32)
    sil = [silb[:, 0, :], silb[:, 1, :]]
    hid = [P.sb("hid%d" % i, [128, 4, 512], BF16) for i in range(2)]
    G2 = silb[:].rearrange("p a b -> p (a b)")
    xr = [nm_sq, nm_t]
    ecnt = 0
    hcnt = 0
    NSB = NB * SEQ // SB

    def load_x2T(i):
        P.dma("sync", x2Ts[i % 2][:], x2T_d[i], r=["x2T_d"], w=["x2T%d" % (i % 2)], semkey="x2T%d" % (i % 2))

    load_x2T(0)
    for sbk in range(NSB):
        b = sbk // 2
        tok0 = sbk * SB
        x2T = x2Ts[sbk % 2]
        x2k = "x2T%d" % (sbk % 2)
        if sbk + 1 < NSB:
            load_x2T(sbk + 1)
        C("vector", "memset", w=["acc"], ap=acc[:], constant=0.0)
        for e in range(32):
            s = ecnt % 2
            ecnt += 1
            P.dma("gpsimd", wg[s][:], weg_d[e].rearrange("(k p) f -> p k f", p=128), w=["wg%d" % s], semkey="wg%d" % s)
            P.dma("gpsimd", wu[s][:], weu_d[e].rearrange("(k p) f -> p k f", p=128), w=["wu%d" % s], semkey="wu%d" % s)
            P.dma("gpsimd", wd[s][:], wed_d[e].rearrange("(k p) f -> p k f", p=128), w=["wd%d" % s], semkey="wd%d" % s)
            for blk in range(SB // 512):
                hs = hcnt % 2
                hcnt += 1
                for f in range(4):
                    pg = (2 * f) % 4
                    pu = (2 * f + 1) % 4
                    for k in range(8):
                        C("tensor", "matmul", r=[x2k, "wg%d" % s], w=["bk%d" % pg], out=banks[pg][:], lhsT=wg[s][:, k, f * 128:(f + 1) * 128], rhs=x2T[:, k, blk * 512:(blk + 1) * 512], start=(k == 0), stop=(k == 7))
                    for k in range(8):
                        C("tensor", "matmul", r=[x2k, "wu%d" % s], w=["bk%d" % pu], out=banks[pu][:], lhsT=wu[s][:, k, f * 128:(f + 1) * 128], rhs=x2T[:, k, blk * 512:(blk + 1) * 512], start=(k == 0), stop=(k == 7))
                    C("scalar", "activation", r=["bk%d" % pg], w=["sil%d" % (f % 2)], out=sil[f % 2], in_=banks[pg][:], func=AF.Silu)
                    C("vector", "tensor_tensor", r=["bk%d" % pu, "sil%d" % (f % 2)], w=["hid%d" % hs], out=hid[hs][:, f, :], in0=banks[pu][:], in1=sil[f % 2], op=ALU.mult)
                for tl in range(4):
                    ti = blk * 4 + tl
                    for half in range(2):
                        bi = 4 + half
                        for f in range(4):
                            C("tensor", "matmul", r=["hid%d" % hs, "wd%d" % s], w=["bk%d" % bi], out=banks[bi][:], lhsT=hid[hs][:, f, tl * 128:(tl + 1) * 128], rhs=wd[s][:, f, half * 512:(half + 1) * 512], start=(f == 0), stop=(f == 3))
                        C("vector", "scalar_tensor_tensor", r=["bk%d" % bi, "gates", "acc"], w=["acc"], out=acc[:, ti, half * 512:(half + 1) * 512], in0=banks[bi][:], scalar=gates[:, sbk * 16 + ti, e:e + 1], in1=acc[:, ti, half * 512:(half + 1) * 512], op0=ALU.mult, op1=ALU.add)
        P.dma("sync", G2, mod_d[b, :, 5 * D:6 * D], r=["mod_d"], w=["sil0", "sil1"], semkey="c19")
        for ti in range(16):
            s = ti % 2
            row0 = tok0 + ti * 128
            okey = "out_d%d" % (row0 // 128)
            P.dma("sync", xr[s][:], out_d[row0:row0 + 128, :], r=[okey], w=["xr%d" % s], semkey="xr%d" % s)
            C("vector", "tensor_tensor", r=["acc", "sil0", "sil1"], w=["acc"], out=acc[:, ti, :], in0=acc[:, ti, :], in1=G2, op=ALU.mult)
            C("vector", "tensor_tensor", r=["acc", "xr%d" % s], w=["xr%d" % s], out=xr[s][:], in0=acc[:, ti, :], in1=xr[s][:], op=ALU.add)
            fin.append(P.dma("sync", out_d[row0:row0 + 128, :], xr[s][:], r=["xr%d" % s], w=[okey], semkey="xo%d" % s))
    P.emit(final_wait_ops=fin[-2:])
    P.close()
    return nc


def ssm_phase(P, C, T, b, banks, catS, epst, env):
    TWO_PI = 2.0 * PI
    PIB = 3.141592
    ygT = P.sb("ygT", [128, 4, SEQ], BF16)
    hF = P.sb("hF", [128, 2, 2, 2050], BF16)
    WB1 = P.sb("WB1", [128, 8, 2, 2, 64], BF16)
    WCL = P.sb("WCL", [128, 2, 16, 2, 2, 16], BF16)
    WK = P.sb("WK", [128, 2, 4, 32], BF16)
    rho2_s = P.sb("rho2_s", [128, 32], F32)
    th2_s = P.sb("th2_s", [128, 32], F32)
    upg = P.sb("upg", [128, TAU], BF16)
    WB = P.sb("WB", [128, 8, 2, 2, 64], BF16)
    WC = P.sb("WC", [128, 16, 2, 2, 16], BF16)
    WD = P.sb("WD", [128, 4, 32], BF16)
    rho_s = P.sb("rho_s", [128, 32], F32)
    th_s = P.sb("th_s", [128, 32], F32)
    wglu = P.sb("wglu", [128, 4, 512], BF16)
    bglu = P.sb("bglu", [128, 4], F32)
    iot = P.sb("iot", [128, 512], F32)
    nm_sq_, nm_t_ = env["nm_sq"], env["nm_t"]
    tq = [nm_sq_[:, 0:512], nm_sq_[:, 512:1024], nm_t_[:, 0:512], nm_t_[:, 512:1024]]
    ki = P.sb("ki", [128, 512], I32)
    P.dma("gpsimd", wglu[:], T["w_glu"].rearrange("(k p) n -> p k n", p=128), w=["wglu"], semkey="wglu")
    P.dma("sync", bglu[:], T["b_gluT"][:], w=["bglu"], semkey="s0")
    P.dma("sync", iot[:], T["iota1"][:], w=["iot"], semkey="s1")

    def trig(src, sk, n, sin_out, sok, cos_out, cok, w0, w1):
        a, bq = tq[w0], tq[w1]
        C("vector", "tensor_scalar", r=[sk], w=["tq%d" % w0], out=a[:, 0:n], in0=src, scalar1=1.0 / TWO_PI, scalar2=None, op0=ALU.mult)
        C("vector", "tensor_copy", r=["tq%d" % w0], w=["ki"], out=ki[:, 0:n], in_=a[:, 0:n])
        C("vector", "tensor_copy", r=["ki"], w=["tq%d" % w0], out=a[:, 0:n], in_=ki[:, 0:n])
        C("vector", "scalar_tensor_tensor", r=["tq%d" % w0, sk], w=["tq%d" % w0], out=a[:, 0:n], in0=a[:, 0:n], scalar=-TWO_PI, in1=src, op0=ALU.mult, op1=ALU.add)
        C("vector", "tensor_scalar", r=["tq%d" % w0], w=["tq%d" % w1], out=bq[:, 0:n], in0=a[:, 0:n], scalar1=PI, scalar2=TWO_PI, op0=ALU.is_gt, op1=ALU.mult)
        C("vector", "tensor_tensor", r=["tq%d" % w0, "tq%d" % w1], w=[sok], out=sin_out, in0=a[:, 0:n], in1=bq[:, 0:n], op=ALU.subtract)
        C("vector", "tensor_scalar", r=[sok], w=["tq%d" % w1], out=bq[:, 0:n], in0=sin_out, scalar1=-PI, scalar2=TWO_PI, op0=ALU.is_lt, op1=ALU.mult)
        C("vector", "tensor_tensor", r=[sok, "tq%d" % w1], w=[sok], out=sin_out, in0=sin_out, in1=bq[:, 0:n], op=ALU.add)
        C("vector", "tensor_scalar", r=[sok], w=[sok], out=sin_out, in0=sin_out, scalar1=-PIB, scalar2=PIB, op0=ALU.max, op1=ALU.min)
        C("scalar", "activation", r=[sok], w=[sok], out=sin_out, in_=sin_out, func=AF.Sin)
        C("vector", "tensor_scalar", r=["tq%d" % w0], w=["tq%d" % w1], out=bq[:, 0:n], in0=a[:, 0:n], scalar1=PI / 2, scalar2=TWO_PI, op0=ALU.is_gt, op1=ALU.mult)
        C("vector", "scalar_tensor_tensor", r=["tq%d" % w0, "tq%d" % w1], w=[cok], out=cos_out, in0=a[:, 0:n], scalar=PI / 2, in1=bq[:, 0:n], op0=ALU.add, op1=ALU.subtract)
        C("vector", "tensor_scalar", r=[cok], w=["tq%d" % w1], out=bq[:, 0:n], in0=cos_out, scalar1=-PI, scalar2=TWO_PI, op0=ALU.is_lt, op1=ALU.mult)
        C("vector", "tensor_tensor", r=[cok, "tq%d" % w1], w=[cok], out=cos_out, in0=cos_out, in1=bq[:, 0:n], op=ALU.add)
        C("vector", "tensor_scalar", r=[cok], w=[cok], out=cos_out, in0=cos_out, scalar1=-PIB, scalar2=PIB, op0=ALU.max, op1=ALU.min)
        C("scalar", "activation", r=[cok], w=[cok], out=cos_out, in_=cos_out, func=AF.Sin)

    md = P.mark()
    L_ = {}
    for nm in ("sA_re", "sA_im", "sA_dt", "sB_re", "sB_im"):
        L_[nm] = P.sb("l_" + nm, [128, 512], F32)
        P.dma("sync", L_[nm][:], T[nm][:], w=[nm], semkey="s_" + nm)
    smask = P.sb("smask", [128, 2], F32); cmask = P.sb("cmask", [128, 2], F32)
    P.dma("sync", smask[:], T["sMask"][:], w=["smask"], semkey="s2")
    P.dma("sync", cmask[:], T["cMask"][:], w=["cmask"], semkey="s3")
    W = [P.sb("dw%d" % i, [128, 512], F32) for i in range(8)]
    wk = ["dw%d" % i for i in range(8)]
    VT = lambda *a, **k: C("vector", "tensor_tensor", *a, **k)
    dtv, mag, ang, sn, cs, lr, li, den = W
    C("scalar", "activation", r=["sA_dt"], w=[wk[0]], out=dtv[:], in_=L_["sA_dt"][:], func=AF.Exp)
    VT(r=["sA_re", wk[0]], w=[wk[1]], out=mag[:], in0=L_["sA_re"][:], in1=dtv[:], op=ALU.mult)
    C("scalar", "activation", r=[wk[1]], w=[wk[1]], out=mag[:], in_=mag[:], func=AF.Exp)
    VT(r=["sA_im", wk[0]], w=[wk[2]], out=ang[:], in0=L_["sA_im"][:], in1=dtv[:], op=ALU.mult)
    trig(ang[:], wk[2], 512, sn[:], wk[3], cs[:], wk[4], 0, 1)
    VT(r=[wk[1], wk[4]], w=[wk[5]], out=lr[:], in0=mag[:], in1=cs[:], op=ALU.mult)
    VT(r=[wk[1], wk[3]], w=[wk[6]], out=li[:], in0=mag[:], in1=sn[:], op=ALU.mult)
    C("vector", "tensor_scalar", r=[wk[5]], w=[wk[5]], out=lr[:], in0=lr[:], scalar1=-1.0, scalar2=None, op0=ALU.add)
    are, aim = L_["sA_re"], L_["sA_im"]
    VT(r=["sA_re"], w=[wk[7]], out=den[:], in0=are[:], in1=are[:], op=ALU.mult)
    VT(r=["sA_im"], w=[wk[0]], out=dtv[:], in0=aim[:], in1=aim[:], op=ALU.mult)
    VT(r=[wk[7], wk[0]], w=[wk[7]], out=den[:], in0=den[:], in1=dtv[:], op=ALU.add)
    C("vector", "reciprocal", r=[wk[7]], w=[wk[7]], out=den[:], in_=den[:])
    VT(r=[wk[5], "sA_re"], w=[wk[1]], out=mag[:], in0=lr[:], in1=are[:], op=ALU.mult)
    VT(r=[wk[6], "sA_im"], w=[wk[0]], out=dtv[:], in0=li[:], in1=aim[:], op=ALU.mult)
    VT(r=[wk[1], wk[0]], w=[wk[1]], out=mag[:], in0=mag[:], in1=dtv[:], op=ALU.add)
    VT(r=[wk[1], wk[7]], w=[wk[1]], out=mag[:], in0=mag[:], in1=den[:], op=ALU.mult)
    VT(r=[wk[6], "sA_re"], w=[wk[2]], out=ang[:], in0=li[:], in1=are[:], op=ALU.mult)
    VT(r=[wk[5], "sA_im"], w=[wk[0]], out=dtv[:], in0=lr[:], in1=aim[:], op=ALU.mult)
    VT(r=[wk[2], wk[0]], w=[wk[2]], out=ang[:], in0=ang[:], in1=dtv[:], op=ALU.subtract)
    VT(r=[wk[2], wk[7]], w=[wk[2]], out=ang[:], in0=ang[:], in1=den[:], op=ALU.mult)
    bre, bim = L_["sB_re"], L_["sB_im"]
    VT(r=[wk[1], "sB_re"], w=[wk[3]], out=sn[:], in0=mag[:], in1=bre[:], op=ALU.mult)
    VT(r=[wk[2], "sB_im"], w=[wk[0]], out=dtv[:], in0=ang[:], in1=bim[:], op=ALU.mult)
    VT(r=[wk[3], wk[0]], w=[wk[3]], out=sn[:], in0=sn[:], in1=dtv[:], op=ALU.subtract)
    VT(r=[wk[1], "sB_im"], w=[wk[4]], out=cs[:], in0=mag[:], in1=bim[:], op=ALU.mult)
    VT(r=[wk[2], "sB_re"], w=[wk[0]], out=dtv[:], in0=ang[:], in1=bre[:], op=ALU.mult)
    VT(r=[wk[4], wk[0]], w=[wk[4]], out=cs[:], in0=cs[:], in1=dtv[:], op=ALU.add)
    for ri, (src, sk) in enumerate(((sn, wk[3]), (cs, wk[4]))):
        for mp in range(2):
            C("vector", "tensor_scalar", r=[sk, "smask"], w=["WB"], out=WB[:, :, ri, mp, :], in0=src[:].rearrange("p (a c) -> p a c", c=64), scalar1=smask[:, mp:mp + 1], scalar2=None, op0=ALU.mult)
    VT(r=[wk[5], wk[3]], w=[wk[1]], out=mag[:], in0=lr[:], in1=sn[:], op=ALU.mult)
    VT(r=[wk[1], wk[3]], w=[wk[1]], out=mag[:], in0=mag[:], in1=sn[:], op=ALU.add)
    VT(r=[wk[6], wk[4]], w=[wk[0]], out=dtv[:], in0=li[:], in1=cs[:], op=ALU.mult)
    VT(r=[wk[1], wk[0]], w=[wk[1]], out=mag[:], in0=mag[:], in1=dtv[:], op=ALU.subtract)
    VT(r=[wk[5], wk[4]], w=[wk[2]], out=ang[:], in0=lr[:], in1=cs[:], op=ALU.mult)
    VT(r=[wk[2], wk[4]], w=[wk[2]], out=ang[:], in0=ang[:], in1=cs[:], op=ALU.add)
    VT(r=[wk[6], wk[3]], w=[wk[0]], out=dtv[:], in0=li[:], in1=sn[:], op=ALU.mult)
    VT(r=[wk[2], wk[0]], w=[wk[2]], out=ang[:], in0=ang[:], in1=dtv[:], op=ALU.add)
    for ri, (src, sk) in enumerate(((mag, wk[1]), (ang, wk[2]))):
        for mp in range(2):
            C("vector", "tensor_scalar", r=[sk, "smask"], w=["WB1"], out=WB1[:, :, ri, mp, :], in0=src[:].rearrange("p (a c) -> p a c", c=64), scalar1=smask[:, mp:mp + 1], scalar2=None, op0=ALU.mult)
    pre = P.sb("pre", [128, 32], F32); pim = P.sb("pim", [128, 32], F32); pdt = P.sb("pdt", [128, 32], F32)
    P.dma("sync", pre[:], T["pA_re"][:], w=["pre"], semkey="s4")
    P.dma("sync", pim[:], T["pA_im"][:], w=["pim"], semkey="s5")
    P.dma("sync", pdt[:], T["pA_dt"][:], w=["pdt"], semkey="s6")
    C("scalar", "activation", r=["pdt"], w=["pdt"], out=pdt[:], in_=pdt[:], func=AF.Exp)
    VT(r=["pre", "pdt"], w=["rho_s"], out=rho_s[:], in0=pre[:], in1=pdt[:], op=ALU.mult)
    C("scalar", "activation", r=["rho_s"], w=["rho_s"], out=rho_s[:], in_=rho_s[:], func=AF.Exp)
    VT(r=["pim", "pdt"], w=["th_s"], out=th_s[:], in0=pim[:], in1=pdt[:], op=ALU.mult)
    VT(r=["rho_s"], w=["rho2_s"], out=rho2_s[:], in0=rho_s[:], in1=rho_s[:], op=ALU.mult)
    C("vector", "tensor_scalar", r=["th_s"], w=["th2_s"], out=th2_s[:], in0=th_s[:], scalar1=2.0, scalar2=None, op0=ALU.mult)
    lrp = P.sb("lrp", [128, 32], F32); lip = P.sb("lip", [128, 32], F32)
    trig(th_s[:], "th_s", 32, lip[:], "lip", lrp[:], "lrp", 0, 1)
    VT(r=["lrp", "rho_s"], w=["lrp"], out=lrp[:], in0=lrp[:], in1=rho_s[:], op=ALU.mult)
    VT(r=["lip", "rho_s"], w=["lip"], out=lip[:], in0=lip[:], in1=rho_s[:], op=ALU.mult)
    ccr = P.sb("ccr", [128, 256], F32); cci = P.sb("cci", [128, 256], F32)
    P.dma("sync", ccr[:], T["cC_re"][:], w=["ccr"], semkey="s7")
    P.dma("sync", cci[:], T["cC_im"][:], w=["cci"], semkey="s8")
    for mp in range(2):
        C("vector", "tensor_scalar", r=["ccr", "cmask"], w=["WC"], out=WC[:, :, 0, mp, :], in0=ccr[:].rearrange("p (a c) -> p a c", c=16), scalar1=cmask[:, mp:mp + 1], scalar2=None, op0=ALU.mult)
        C("vector", "tensor_scalar", r=["cci", "cmask"], w=["WC"], out=WC[:, :, 1, mp, :], in0=cci[:].rearrange("p (a c) -> p a c", c=16), scalar1=cmask[:, mp:mp + 1], scalar2=-1.0, op0=ALU.mult, op1=ALU.mult)
    cta = P.sb("cta", [128, 16, 16], F32); ctb = P.sb("ctb", [128, 16, 16], F32)
    ccr3 = ccr[:].rearrange("p (a c) -> p a c", c=16); cci3 = cci[:].rearrange("p (a c) -> p a c", c=16)
    for d in range(2):
        lrb = lrp[:, d * 16:(d + 1) * 16].unsqueeze(2).to_broadcast([128, 16, 16])
        lib = lip[:, d * 16:(d + 1) * 16].unsqueeze(2).to_broadcast([128, 16, 16])
        VT(r=["ccr", "lrp"], w=["cta"], out=cta[:], in0=ccr3, in1=lrb, op=ALU.mult)
        VT(r=["cci", "lip"], w=["ctb"], out=ctb[:], in0=cci3, in1=lib, op=ALU.mult)
        VT(r=["cta", "ctb"], w=["cta"], out=cta[:], in0=cta[:], in1=ctb[:], op=ALU.subtract)
        for mp in range(2):
            C("vector", "tensor_scalar", r=["cta", "cmask"], w=["WCL"], out=WCL[:, d, :, 0, mp, :], in0=cta[:], scalar1=cmask[:, mp:mp + 1], scalar2=None, op0=ALU.mult)
        VT(r=["ccr", "lip"], w=["cta"], out=cta[:], in0=ccr3, in1=lib, op=ALU.mult)
        VT(r=["cci", "lrp"], w=["ctb"], out=ctb[:], in0=cci3, in1=lrb, op=ALU.mult)
        VT(r=["cta", "ctb"], w=["cta"], out=cta[:], in0=cta[:], in1=ctb[:], op=ALU.add)
        for mp in range(2):
            C("vector", "tensor_scalar", r=["cta", "cmask"], w=["WCL"], out=WCL[:, d, :, 1, mp, :], in0=cta[:], scalar1=cmask[:, mp:mp + 1], scalar2=-1.0, op0=ALU.mult, op1=ALU.mult)
    e32 = P.sb("e32", [128, 32], F32); dsk = P.sb("dsk", [128, 4], F32)
    P.dma("sync", e32[:], T["eye32"][:], w=["e32"], semkey="s9")
    P.dma("sync", dsk[:], T["dskip"][:], w=["dsk"], semkey="s10")
    for Tt in range(4):
        C("vector", "tensor_scalar", r=["e32", "dsk"], w=["WD"], out=WD[:, Tt, :], in0=e32[:], scalar1=dsk[:, Tt:Tt + 1], scalar2=None, op0=ALU.mult)
    WDf = P.sb("WDf", [128, 4, 32], F32)
    BTs = P.sb("BTs", [128, 2, 32], BF16)
    tbk = env["tbs"][0]
    ident_ = env["ident"]
    for Tt in range(4):
        C("vector", "tensor_scalar", r=["e32", "dsk"], w=["WDf"], out=WDf[:, Tt, :], in0=e32[:], scalar1=dsk[:, Tt:Tt + 1], scalar2=None, op0=ALU.mult)
    for d in range(2):
        for Tt in range(4):
            kb_ = banks[4 + (d * 4 + Tt) % 2]
            kbk = "bk%d" % (4 + (d * 4 + Tt) % 2)
            for j in range(4):
                Pp = 4 * Tt + j
                for ri in range(2):
                    C("tensor", "transpose", r=["WB", "ident"], w=["tb0"], out=tbk[:, ri * 32:(ri + 1) * 32], in_=WB[32 * j:32 * j + 32, d * 4 + Tt, ri, :, :].rearrange("p a c -> p (a c)"), identity=ident_[32 * j:32 * j + 32, 32 * j:32 * j + 32], tile_position=(32 * j, 0))
                C("vector", "tensor_copy", r=["tb0"], w=["BTs"], out=BTs[:].rearrange("p a c -> p (a c)"), in_=tbk[:, 0:64])
                for ri in range(2):
                    C("tensor", "matmul", r=["BTs", "WC"], w=[kbk], out=kb_[32 * j:32 * j + 32, 0:32], lhsT=BTs[:, ri, :], rhs=WC[:, Pp, ri, :, :].rearrange("p a c -> p (a c)"), start=(ri == 0), stop=(ri == 1), tile_position=(0, 32 * j), skip_group_check=True)
            C("vector", "tensor_tensor", r=[kbk, "WDf"], w=["WK"], out=WK[:, d, Tt, :], in0=kb_[:, 0:32], in1=WDf[:, Tt, :], op=ALU.add)
    P.release(md)
    tabc = [P.sb("tabc%d" % i, [128, 512], F32) for i in range(2)]
    tabs = [P.sb("tabs%d" % i, [128, 512], F32) for i in range(2)]
    tabr = [P.sb("tabr%d" % i, [128, 512], F32) for i in range(2)]
    gg = [[P.sb("gg%d%d" % (i, r), [128, 512], F32) for r in range(2)] for i in range(2)]
    hB = [[P.sb("hB%d%d" % (i, r), [128, 514], BF16) for r in range(2)] for i in range(2)]
    car = P.sb("car", [128, 2, 8], F32)
    tneg = [P.sb("tneg%d" % i, [128, 2], F32) for i in range(2)]
    dmt = [[P.sb("dmt%d%d" % (i, r), [128, 512], F32) for r in range(2)] for i in range(2)]
    tq2 = [[P.sb("tqw%d%d" % (i, r), [128, 512], F32) for r in range(4 - i)] for i in range(2)]
    tq2[1].append(tq[3])

    def make_tables(d, Pp, pi):
        col = d * 16 + Pp
        if b > 0:
            P.dma("sync", tabs[pi][:], T["tab_s"][col, 0], r=["tab_d%d" % col], w=["tabs%d" % pi], semkey="tls%d" % pi)
            P.dma("sync", tabc[pi][:], T["tab_s"][col, 1], r=["tab_d%d" % col], w=["tabc%d" % pi], semkey="tlc%d" % pi)
            C("vector", "tensor_scalar", r=["iot", "rho2_s"], w=["tabr%d" % pi], out=tabr[pi][:], in0=iot[:], scalar1=0.0, scalar2=rho2_s[:, col:col + 1], op0=ALU.mult, op1=ALU.add)
            return
        C("vector", "tensor_scalar", r=["iot", "th2_s"], w=["tq2"], out=tq[2][:], in0=iot[:], scalar1=th2_s[:, col:col + 1], scalar2=None, op0=ALU.mult)
        trig(tq[2][:], "tq2", 512, tabs[pi][:], "tabs%d" % pi, tabc[pi][:], "tabc%d" % pi, 0, 1)
        C("vector", "tensor_scalar", r=["iot", "rho2_s"], w=["tabr%d" % pi], out=tabr[pi][:], in0=iot[:], scalar1=0.0, scalar2=rho2_s[:, col:col + 1], op0=ALU.mult, op1=ALU.add)
        P.dma("sync", T["tab_s"][col, 0], tabs[pi][:], r=["tabs%d" % pi], w=["tab_d%d" % col], semkey="tss%d" % pi)
        P.dma("sync", T["tab_s"][col, 1], tabc[pi][:], r=["tabc%d" % pi], w=["tab_d%d" % col], semkey="tsc%d" % pi)

    FWD_BLOCKS = [(0, 512), (512, 512), (1024, 512), (1536, 512), (2048, 128)]
    BWD_BLOCKS = [(1792, 512), (1280, 512), (768, 512), (256, 512), (128, 128)]
    for i_ in range(2):
        for r_ in range(2):
            C("gpsimd", "memset", w=["hB%d" % i_], ap=hB[i_][r_][:], constant=0.0)
    for Tt in range(4):
        P.dma("sync", upg[:], T["uT_s"][b, Tt], r=["uT_d"], w=["upg"], semkey="upg")
        for pg in range(2):
            for d in range(2):
                for pi in range(2):
                    make_tables(d, 4 * Tt + 2 * pg + pi, pi)
                    C("vector", "tensor_scalar", r=["tabs%d" % pi], w=["tneg%d" % pi], out=tneg[pi][:, 0:1], in0=tabs[pi][:, 511:512], scalar1=-1.0, scalar2=None, op0=ALU.mult)
                    C("vector", "tensor_scalar", r=["tabs%d" % pi], w=["tneg%d" % pi], out=tneg[pi][:, 1:2], in0=tabs[pi][:, 127:128], scalar1=-1.0, scalar2=None, op0=ALU.mult)
                C("vector", "memset", w=["car0", "car1", "cw0", "cx0", "cy0", "cz0", "cw1", "cx1", "cy1", "cz1"], ap=car[:], constant=0.0)
                for kb, (c0, n) in enumerate(FWD_BLOCKS if d == 0 else BWD_BLOCKS):
                    rv = (lambda ap: ap) if d == 0 else (lambda ap: ap[:, ::-1])

                    def pair_ops(pi):
                        j = 2 * pg + pi
                        bre_k, bim_k = "bk%d" % (2 * pi), "bk%d" % (2 * pi + 1)
                        pre_, pim_ = banks[2 * pi], banks[2 * pi + 1]
                        cT, sT, rT = rv(tabc[pi][:, 0:n]), rv(tabs[pi][:, 0:n]), tabr[pi][:, 0:n]
                        ck, sk_, rk = "tabc%d" % pi, "tabs%d" % pi, "tabr%d" % pi
                        wa_, wb_ = tq2[pi][0], tq2[pi][1]
                        wak, wbk = "tqa%d" % pi, "tqb%d" % pi
                        wc_, wd_ = tq2[pi][2], tq2[pi][3]
                        wck, wdk = "tqc%d" % pi, "tqd%d" % pi
                        gr, gi = wa_, wc_
                        grk, gik = wak, wck
                        g0, g1 = gg[pi][0], gg[pi][1]
                        g0k, g1k = "gg%d0" % pi, "gg%d1" % pi
                        lo_ = 127 if (d == 0 and kb == 0) else 0
                        if d == 0:
                            hre, him = hF[:, pi, 0, c0 + lo_ - 127:c0 + n - 127], hF[:, pi, 1, c0 + lo_ - 127:c0 + n - 127]
                            hk = "hF%d" % pi
                        else:
                            hre, him = hB[pi][0][:, 0:n], hB[pi][1][:, 0:n]
                            hk = "hB%d" % pi
                        cTd, sTd = rv(tabc[pi][:, 0:n])[:, lo_:n], rv(tabs[pi][:, 0:n])[:, lo_:n]
                        lc = n - 1 if d == 0 else 0
                        tcn = n - 1
                        cl, sl_ = tabc[pi][:, tcn:tcn + 1], tabs[pi][:, tcn:tcn + 1]
                        nsl = tneg[pi][:, (0 if n == 512 else 1):(1 if n == 512 else 2)]
                        a0, a1 = g0[:, lc:lc + 1], g1[:, lc:lc + 1]
                        GT = lambda *a, **k: C("gpsimd", "tensor_tensor", *a, **k)
                        ev = upg[32 * j:32 * j + 32, 2 * c0:2 * c0 + 2 * n:2]
                        od = upg[32 * j:32 * j + 32, 2 * c0 + 1:2 * c0 + 2 * n:2]
                        first, second = (ev, od) if d == 0 else (od, ev)
                        ops = []
                        for ri, bk_, bkk in ((0, pre_, bre_k), (1, pim_, bim_k)):
                            ops.append(lambda ri=ri, bk_=bk_, bkk=bkk: C("tensor", "matmul", r=["WB1", "upg"], w=[bkk], out=bk_[:, 0:n], lhsT=WB1[32 * j:32 * j + 32, d * 4 + Tt, ri, :, :].rearrange("p a c -> p (a c)"), rhs=first, start=True, stop=False, tile_position=(32 * j, 0)))
                            ops.append(lambda ri=ri, bk_=bk_, bkk=bkk: C("tensor", "matmul", r=["WB", "upg"], w=[bkk], out=bk_[:, 0:n], lhsT=WB[32 * j:32 * j + 32, d * 4 + Tt, ri, :, :].rearrange("p a c -> p (a c)"), rhs=second, start=False, stop=True, tile_position=(32 * j, 0)))
                        if d == 1:
                            ops.append(lambda: C("gpsimd", "tensor_copy", r=[hk], w=[hk], out=hB[pi][0][:, n:n + 1], in_=hB[pi][0][:, 0:1]))
                            ops.append(lambda: C("gpsimd", "tensor_copy", r=[hk], w=[hk], out=hB[pi][1][:, n:n + 1], in_=hB[pi][1][:, 0:1]))
                        ops += [
                            lambda: VT(r=[bre_k, ck], w=[wak], out=wa_[:, 0:n], in0=pre_[:, 0:n], in1=cT, op=ALU.mult),
                            lambda: VT(r=[bim_k, sk_], w=[wbk], out=wb_[:, 0:n], in0=pim_[:, 0:n], in1=sT, op=ALU.mult),
                            lambda: VT(r=[bim_k, ck], w=[wck], out=wc_[:, 0:n], in0=pim_[:, 0:n], in1=cT, op=ALU.mult),
                            lambda: VT(r=[bre_k, sk_], w=[wdk], out=wd_[:, 0:n], in0=pre_[:, 0:n], in1=sT, op=ALU.mult),
                            lambda: VT(r=[wak, wbk], w=[grk], out=gr[:, 0:n], in0=wa_[:, 0:n], in1=wb_[:, 0:n], op=ALU.add),
                            lambda: VT(r=[wck, wdk], w=[gik], out=gi[:, 0:n], in0=wc_[:, 0:n], in1=wd_[:, 0:n], op=ALU.subtract),
                            lambda: C("vector", "tensor_tensor_scan", r=[rk, grk, "car%d" % pi], w=[g0k], out=rv(g0[:, 0:n]), data0=rT, data1=rv(gr[:, 0:n]), initial=car[:, pi, 0:1], op0=ALU.mult, op1=ALU.add),
                            lambda: C("vector", "tensor_tensor_scan", r=[rk, gik, "car%d" % pi], w=[g1k], out=rv(g1[:, 0:n]), data0=rT, data1=rv(gi[:, 0:n]), initial=car[:, pi, 1:2], op0=ALU.mult, op1=ALU.add),
                            lambda: C("scalar", "activation", r=[g1k, "tneg%d" % pi], w=["cx%d" % pi], out=car[:, pi, 3:4], in_=a1, func=AF.Identity, scale=nsl),
                            lambda: C("scalar", "activation", r=[g0k, sk_], w=["cz%d" % pi], out=car[:, pi, 5:6], in_=a0, func=AF.Identity, scale=sl_),
                            lambda: C("scalar", "activation", r=[g0k, ck, "cx%d" % pi, "car%d" % pi], w=["car%d" % pi], out=car[:, pi, 0:1], in_=a0, func=AF.Identity, scale=cl, bias=car[:, pi, 3:4]),
                            lambda: C("scalar", "activation", r=[g1k, ck, "cz%d" % pi, "car%d" % pi], w=["car%d" % pi], out=car[:, pi, 1:2], in_=a1, func=AF.Identity, scale=cl, bias=car[:, pi, 5:6]),
                            lambda: GT(r=[g0k, ck], w=["dma%d" % pi], out=dmt[pi][0][:, lo_:n], in0=g0[:, lo_:n], in1=cTd, op=ALU.mult),
                            lambda: GT(r=[g1k, sk_], w=["dmb%d" % pi], out=dmt[pi][1][:, lo_:n], in0=g1[:, lo_:n], in1=sTd, op=ALU.mult),
                            lambda: GT(r=["dma%d" % pi, "dmb%d" % pi], w=[hk], out=hre, in0=dmt[pi][0][:, lo_:n], in1=dmt[pi][1][:, lo_:n], op=ALU.subtract),
                            lambda: GT(r=[g1k, ck], w=["dma%d" % pi], out=dmt[pi][0][:, lo_:n], in0=g1[:, lo_:n], in1=cTd, op=ALU.mult),
                            lambda: GT(r=[g0k, sk_], w=["dmb%d" % pi], out=dmt[pi][1][:, lo_:n], in0=g0[:, lo_:n], in1=sTd, op=ALU.mult),
                            lambda: GT(r=["dma%d" % pi, "dmb%d" % pi], w=[hk], out=him, in0=dmt[pi][0][:, lo_:n], in1=dmt[pi][1][:, lo_:n], op=ALU.add),
                        ]
                        return ops

                    opl = [pair_ops(0), pair_ops(1)]
                    for k_ in range(len(opl[0])):
                        for pi in range(2):
                            opl[pi][k_]()
                    if d == 1:
                        cl0, cl1 = max(c0, 128), min(c0 + n, 2176)
                        nl = cl1 - cl0
                        o = cl0 - c0
                        for eo in range(2):
                            yb = banks[4 + eo]
                            ybk = "bk%d" % (4 + eo)
                            for pi in range(2):
                                j = 2 * pg + pi
                                Pp = 4 * Tt + j
                                outp = yb[32 * j:32 * j + 32, 0:nl]
                                w2 = lambda t, ri: t[:, Pp, ri, :, :].rearrange("p a c -> p (a c)")
                                wl = lambda dd, ri: WCL[:, dd, Pp, ri, :, :].rearrange("p a c -> p (a c)")
                                if eo == 0:
                                    terms = [(wl(0, 0), hF[:, pi, 0, cl0 - 128:cl1 - 128], "hF%d" % pi), (wl(0, 1), hF[:, pi, 1, cl0 - 128:cl1 - 128], "hF%d" % pi),
                                             (w2(WC, 0), hB[pi][0][:, o:o + nl], "hB%d" % pi), (w2(WC, 1), hB[pi][1][:, o:o + nl], "hB%d" % pi)]
                                    urhs = upg[32 * j:32 * j + 32, 2 * cl0:2 * cl1:2]
                                else:
                                    terms = [(w2(WC, 0), hF[:, pi, 0, cl0 - 127:cl1 - 127], "hF%d" % pi), (w2(WC, 1), hF[:, pi, 1, cl0 - 127:cl1 - 127], "hF%d" % pi),
                                             (wl(1, 0), hB[pi][0][:, o + 1:o + 1 + nl], "hB%d" % pi), (wl(1, 1), hB[pi][1][:, o + 1:o + 1 + nl], "hB%d" % pi)]
                                    urhs = upg[32 * j:32 * j + 32, 2 * cl0 + 1:2 * cl1:2]
                                for ti, (lw, rh, rkey) in enumerate(terms):
                                    C("tensor", "matmul", r=["WC", "WCL", rkey], w=[ybk], out=outp, lhsT=lw, rhs=rh, start=(ti == 0), stop=False, tile_position=(0, 32 * j), skip_group_check=True)
                                C("tensor", "matmul", r=["WK", "upg"], w=[ybk], out=outp, lhsT=WK[32 * j:32 * j + 32, eo, Tt, :], rhs=urhs, start=False, stop=True, tile_position=(32 * j, 32 * j), skip_group_check=True)
                            rows = slice(64 * pg, 64 * pg + 64)
                            yv = yb[rows, 0:nl]
                            tw = tq[eo]
                            twk = "tq%d" % eo
                            C("scalar", "activation", r=[ybk], w=[twk], out=tw[rows, 0:nl], in_=yv, func=AF.Square)
                            C("vector", "tensor_scalar", r=[twk], w=[twk], out=tw[rows, 0:nl], in0=tw[rows, 0:nl], scalar1=0.044715, scalar2=1.0, op0=ALU.mult, op1=ALU.add)
                            VT(r=[twk, ybk], w=[twk], out=tw[rows, 0:nl], in0=tw[rows, 0:nl], in1=yv, op=ALU.mult)
                            C("scalar", "activation", r=[twk], w=[twk], out=tw[rows, 0:nl], in_=tw[rows, 0:nl], func=AF.Sigmoid, scale=2.0 * math.sqrt(2.0 / PI))
                            VT(r=[twk, ybk], w=["ygT"], out=ygT[rows, Tt, 2 * cl0 - 256 + eo:2 * cl1 - 256:2], in0=tw[rows, 0:nl], in1=yv, op=ALU.mult)
    zc = 0
    for n_ in range(4):
        for blk in range(8):
            zb = banks[zc % 4]
            zk = "bk%d" % (zc % 4)
            sgi = zc % 2
            zc += 1
            for k in range(4):
                C("tensor", "matmul", r=["wglu", "ygT"], w=[zk], out=zb[:], lhsT=wglu[:, k, n_ * 128:(n_ + 1) * 128], rhs=ygT[:, k, blk * 512:(blk + 1) * 512], start=(k == 0), stop=(k == 3))
            C("scalar", "activation", r=[zk, "bglu"], w=["tq%d" % sgi], out=tq[sgi][:], in_=zb[:], func=AF.Sigmoid, bias=bglu[:, n_:n_ + 1], scale=1.0)
            C("vector", "tensor_tensor", r=["tq%d" % sgi, "ygT"], w=["catS"], out=catS[:, n_, blk * 512:(blk + 1) * 512], in0=tq[sgi][:], in1=ygT[:, n_, blk * 512:(blk + 1) * 512], op=ALU.mult)


_NC_CACHE = {}


def host_layouts(inp):
    f = np.float32
    L = {}
    L["w_ada"] = np.ascontiguousarray(inp["w_ada"][0]); L["b_ada"] = np.ascontiguousarray(np.broadcast_to(inp["b_ada"], (3, 6 * D)))
    rep = lambda v, n: np.ascontiguousarray(np.broadcast_to(np.tile(np.asarray(v, np.float32).reshape(1, -1), (1, n)), (128, v.size * n)))
    L["norm1_g"] = rep(inp["norm1_g"], 1); L["norm2_g"] = rep(inp["norm2_g"], 1)
    sel = np.zeros((3, 3, 128), np.float32)
    for r_ in range(3):
        sel[r_, r_, :] = 1.0
    L["sel3"] = sel.reshape(3, 384)
    L["w_in"] = np.ascontiguousarray(inp["w_in"][0])
    L["qg"] = rep(inp["q_norm_g"], 8); L["kg"] = rep(inp["k_norm_g"], 8)
    L["lamv"] = rep(np.concatenate([inp["lambda_q1"], inp["lambda_k1"], inp["lambda_q2"], inp["lambda_k2"]], axis=1).astype(f), 1)
    L["subln"] = rep(inp["subln_g"], 4)
    rows = SEQ // 64
    row = np.repeat(np.arange(rows, dtype=f), 64); col = np.tile(np.arange(64, dtype=f), rows)
    inv = (10000.0 ** (-np.arange(0, 32, 2, dtype=f) / 32)).astype(f)
    ang = np.stack([row[:, None] * inv, col[:, None] * inv], axis=1).astype(f)
    cs = np.cos(ang).astype(f); sn = np.sin(ang).astype(f)
    full = lambda t: np.ascontiguousarray(np.broadcast_to(t[:, None, :, None, :], (SEQ, 8, 2, 2, 16)).reshape(SEQ, 512))
    L["rope_cos"] = full(cs); L["rope_sin"] = full(sn)
    L["ident"] = np.eye(128, dtype=f)
    L["w_glu"] = np.ascontiguousarray(inp["w_glu"][0]); L["b_gluT"] = np.ascontiguousarray(inp["b_glu"][0].reshape(4, 128).T)
    L["w_out"] = np.ascontiguousarray(inp["w_out"][0])
    L["w_r"] = np.ascontiguousarray(np.concatenate([inp["w_route_group"][0], inp["w_route_expert"][0]], axis=1))
    L["b_r"] = rep(np.concatenate([inp["b_route_group"], inp["b_route_expert"]], axis=1), 1)
    L["w_eg"] = np.ascontiguousarray(inp["w_exp_gate"][0]); L["w_eu"] = np.ascontiguousarray(inp["w_exp_up"][0]); L["w_ed"] = np.ascontiguousarray(inp["w_exp_down"][0])
    a_re, a_im, ldt = inp["ssm_a_re"][0], inp["ssm_a_im"][0], inp["ssm_log_dt"][0]
    b_re, b_im = inp["ssm_b_re"][0], inp["ssm_b_im"][0]
    c_re, c_im, dsk = inp["ssm_c_re"][0], inp["ssm_c_im"][0], inp["ssm_d"][0]
    sA_re = np.zeros((128, 2, 4, 64), f); sA_im = np.zeros_like(sA_re); sA_dt = np.zeros_like(sA_re)
    sB_re = np.zeros_like(sA_re); sB_im = np.zeros_like(sA_re)
    sMask = np.zeros((128, 2), f); dskl = np.zeros((128, 4), f)
    for j in range(4):
        for m in range(2):
            for h in range(16):
                q = 32 * j + 16 * m + h
                sMask[q, m] = 1.0
                for Tt in range(4):
                    g = 8 * Tt + 2 * j + m
                    dskl[q, Tt] = dsk[g, h]
                    for d in range(2):
                        sA_re[q, d, Tt] = a_re[d, g]; sA_im[q, d, Tt] = a_im[d, g]; sA_dt[q, d, Tt] = ldt[d, g]
                        sB_re[q, d, Tt] = b_re[d, g, :, h]; sB_im[q, d, Tt] = b_im[d, g, :, h]
    L["sA_re"] = sA_re.reshape(128, 512); L["sA_im"] = sA_im.reshape(128, 512); L["sA_dt"] = sA_dt.reshape(128, 512)
    L["sB_re"] = sB_re.reshape(128, 512); L["sB_im"] = sB_im.reshape(128, 512); L["sMask"] = sMask; L["dskip"] = dskl
    pA_re = np.zeros((128, 2, 16), f); pA_im = np.zeros_like(pA_re); pA_dt = np.zeros_like(pA_re)
    cC_re = np.zeros((128, 16, 16), f); cC_im = np.zeros_like(cC_re); cMask = np.zeros((128, 2), f)
    for m in range(2):
        for p in range(64):
            q = 64 * m + p
            cMask[q, m] = 1.0
            for Pp in range(16):
                g = 2 * Pp + m
                cC_re[q, Pp] = c_re[g, :, p]; cC_im[q, Pp] = c_im[g, :, p]
                for d in range(2):
                    pA_re[q, d, Pp] = a_re[d, g, p]; pA_im[q, d, Pp] = a_im[d, g, p]; pA_dt[q, d, Pp] = ldt[d, g]
    L["pA_re"] = pA_re.reshape(128, 32); L["pA_im"] = pA_im.reshape(128, 32); L["pA_dt"] = pA_dt.reshape(128, 32)
    L["cC_re"] = cC_re.reshape(128, 256); L["cC_im"] = cC_im.reshape(128, 256); L["cMask"] = cMask
    L["eye32"] = np.tile(np.eye(32, dtype=f), (4, 1)); L["iota1"] = np.tile(np.arange(1, 513, dtype=f)[None, :], (128, 1))
    return L


def kernel(**inp):
    inp = {k: np.asarray(v) for k, v in inp.items()}
    if "nc" not in _NC_CACHE:
        _NC_CACHE["nc"] = build_program()
    nc = _NC_CACHE["nc"]
    L = host_layouts(inp)
    in_maps = []
    for c in range(8):
        m = dict(L)
        m["x"] = np.ascontiguousarray(inp["x"][NB * c:NB * (c + 1)].reshape(NB * SEQ, D))
        m["ctx"] = np.ascontiguousarray(inp["ctx"][NB * c:NB * (c + 1)].reshape(NB * CTX, D))
        cv = np.concatenate([inp["c"][NB * c:NB * (c + 1)], inp["c_ctx"][None, :]], axis=0)
        m["cvT"] = np.ascontiguousarray(cv.reshape(3, 8, 128).transpose(2, 1, 0))
        in_maps.append(m)
    res = run_bass_kernel_spmd(nc, in_maps, core_ids=list(range(8)))
    out = np.concatenate([r["out"].reshape(NB, SEQ, D) for r in res.results], axis=0)
    return out.astype(np.float32)
```
